# Optimizing a Trainium2 kernel written in Bass

```python
import math
import jax, jax.numpy as jnp
from jax import lax
import numpy as np

D_MODEL = 1024
BATCH = 4
SEQ = 8192
DEPTH = 2

HEAD_DIM = 64
SWA_HEADS = 4
SWA_KV_HEADS = 2
SWA_WINDOW = 128
SWA_BLOCK = 128
MOBA_HEADS = 4
MOBA_BLOCK = 256
MOBA_TOPK = 3
MOBA_QCHUNK = 64
DN_HEADS = 4
DN_HEAD_DIM = 128
DN_CONV = 4
DN_CHUNK = 64
D_FF = 2816
FFN_CONV = 3

NORM_EPS = 1e-6
NEG_INF = -1e30

SWA_Q = SWA_HEADS * HEAD_DIM
SWA_KV = SWA_KV_HEADS * HEAD_DIM
MOBA_W = MOBA_HEADS * HEAD_DIM
DN_W = DN_HEADS * DN_HEAD_DIM
MIX_WIDTH = SWA_Q + MOBA_W + DN_W
IN_SPLIT_SIZES = (SWA_Q, SWA_KV, SWA_KV, MOBA_W, MOBA_W, MOBA_W, 3 * DN_W, DN_W, DN_HEADS, DN_HEADS)
IN_WIDTH = SWA_Q + 2 * SWA_KV + 3 * MOBA_W + 4 * DN_W + 2 * DN_HEADS

kernel_name = 'hybrid_swa_moba_gdn_block'


def rms_norm(x, w):
    xf = x.astype(jnp.float32)
    y = xf * lax.rsqrt(jnp.mean(xf * xf, axis=-1, keepdims=True) + NORM_EPS)
    return (y * w.astype(jnp.float32)).astype(x.dtype)


def l2_norm(x):
    return x * lax.rsqrt(jnp.sum(x * x, axis=-1, keepdims=True) + NORM_EPS)


def alibi_slopes():
    n = SWA_HEADS + MOBA_HEADS
    s = 2.0 ** (-8.0 * jnp.arange(1, n + 1, dtype=jnp.float32) / n)
    return s[0::2], s[1::2]


def causal_depthwise_conv(x, w):
    K = w.shape[0]
    S = x.shape[1]
    xp = jnp.pad(x, ((0, 0), (K - 1, 0), (0, 0)))
    y = xp[:, 0:S] * w[0]
    for i in range(1, K):
        y = y + xp[:, i:i + S] * w[i]
    return y


def swa_sink_attention(q, k, v, sinks, slopes):
    B, S, Hq, d = q.shape
    Hkv = k.shape[2]
    G = Hq // Hkv
    nb = S // SWA_BLOCK
    qb = q.reshape(B, nb, SWA_BLOCK, Hkv, G, d)
    kb = k.reshape(B, nb, SWA_BLOCK, Hkv, d)
    vb = v.reshape(B, nb, SWA_BLOCK, Hkv, d)

    def with_prev(t):
        prev = jnp.pad(t, ((0, 0), (1, 0), (0, 0), (0, 0), (0, 0)))[:, :-1]
        return jnp.concatenate([prev, t], axis=2)

    kk, vv = with_prev(kb), with_prev(vb)
    scores = jnp.einsum('bnqhgd,bnshd->bnhgqs', qb, kk).astype(jnp.float32) * (d ** -0.5)
    rel = jnp.arange(SWA_BLOCK)[:, None] + SWA_BLOCK - jnp.arange(2 * SWA_BLOCK)[None, :]
    kpos = (jnp.arange(nb) * SWA_BLOCK - SWA_BLOCK)[:, None] + jnp.arange(2 * SWA_BLOCK)[None, :]
    valid = ((rel >= 0) & (rel < SWA_WINDOW))[None] & (kpos >= 0)[:, None, :]
    m = slopes.astype(jnp.float32).reshape(Hkv, G)[:, :, None, None]
    scores = scores - m * rel.astype(jnp.float32)
    scores = jnp.where(valid[None, :, None, None], scores, NEG_INF)
    sink = sinks.astype(jnp.float32).reshape(Hkv, G)[:, :, None, None]
    mx = jnp.maximum(scores.max(axis=-1, keepdims=True), sink)
    p = jnp.exp(scores - mx)
    denom = p.sum(axis=-1, keepdims=True) + jnp.exp(sink - mx)
    probs = (p / denom).astype(v.dtype)
    out = jnp.einsum('bnhgqs,bnshd->bnqhgd', probs, vv)
    return out.reshape(B, S, Hq, d)


def moba_attention(q, k, v, slopes):
    B, S, H, d = q.shape
    S_pad = -(-S // MOBA_BLOCK) * MOBA_BLOCK
    pad = ((0, 0), (0, S_pad - S), (0, 0), (0, 0))
    q, k, v = jnp.pad(q, pad), jnp.pad(k, pad), jnp.pad(v, pad)
    nblk = S_pad // MOBA_BLOCK
    topk = min(MOBA_TOPK, nblk)
    kb = k.reshape(B, nblk, MOBA_BLOCK, H, d).transpose(0, 3, 1, 2, 4)
    vb = v.reshape(B, nblk, MOBA_BLOCK, H, d).transpose(0, 3, 1, 2, 4)
    k_mean = kb.astype(jnp.float32).mean(axis=3)
    nq = S_pad // MOBA_QCHUNK
    q_chunks = q.reshape(B, nq, MOBA_QCHUNK, H, d).transpose(1, 0, 3, 2, 4)
    bi = jnp.arange(B)[:, None, None, None]
    hi = jnp.arange(H)[None, :, None, None]
    m = slopes.astype(jnp.float32)[None, :, None, None]
    scale = d ** -0.5
    offs = jnp.arange(MOBA_BLOCK)

    def chunk_attend(args):
        qc, c = args
        t = c * MOBA_QCHUNK + jnp.arange(MOBA_QCHUNK)
        own = (c * MOBA_QCHUNK) // MOBA_BLOCK
        gate = jnp.einsum('bhqd,bhnd->bhqn', qc.astype(jnp.float32), k_mean)
        gate = jnp.where(jnp.arange(nblk) < own, gate, NEG_INF)
        _, idx = lax.top_k(gate, topk)
        sel_ok = idx < own
        k_sel = kb[bi, hi, idx]
        v_sel = vb[bi, hi, idx]
        s_sel = jnp.einsum('bhqd,bhqrsd->bhqrs', qc, k_sel).astype(jnp.float32) * scale
        kpos_sel = idx[..., None] * MOBA_BLOCK + offs
        s_sel = s_sel - m[..., None] * (t[:, None, None] - kpos_sel).astype(jnp.float32)
        s_sel = jnp.where(sel_ok[..., None], s_sel, NEG_INF).reshape(B, H, MOBA_QCHUNK, topk * MOBA_BLOCK)
        k_own = lax.dynamic_index_in_dim(kb, own, axis=2, keepdims=False)
        v_own = lax.dynamic_index_in_dim(vb, own, axis=2, keepdims=False)
        rel_own = t[:, None] - (own * MOBA_BLOCK + offs)[None, :]
        s_own = jnp.einsum('bhqd,bhsd->bhqs', qc, k_own).astype(jnp.float32) * scale - m * rel_own.astype(jnp.float32)
        s_own = jnp.where(rel_own >= 0, s_own, NEG_INF)
        probs = jax.nn.softmax(jnp.concatenate([s_sel, s_own], axis=-1), axis=-1).astype(v.dtype)
        p_sel = probs[..., :topk * MOBA_BLOCK].reshape(B, H, MOBA_QCHUNK, topk, MOBA_BLOCK)
        p_own = probs[..., topk * MOBA_BLOCK:]
        return (jnp.einsum('bhqrs,bhqrsd->bqhd', p_sel, v_sel)
                + jnp.einsum('bhqs,bhsd->bqhd', p_own, v_own))

    out = lax.map(chunk_attend, (q_chunks, jnp.arange(nq)))
    out = out.transpose(1, 0, 2, 3, 4).reshape(B, S_pad, H, d)
    return out[:, :S]


def gated_delta_rule(q, k, v, g, beta):
    B, S, H, dk = q.shape
    dv = v.shape[-1]
    C = DN_CHUNK
    nc = S // C

    def chunks(t):
        t = t.reshape((B, nc, C, H) + t.shape[3:])
        return jnp.moveaxis(t, 3, 1)

    q = chunks(q) * (dk ** -0.5)
    k = chunks(k)
    v = chunks(v)
    g = jnp.cumsum(chunks(g), axis=-1)
    beta = chunks(beta)
    causal = jnp.tril(jnp.ones((C, C), dtype=bool))
    strict = jnp.tril(jnp.ones((C, C), dtype=bool), -1)
    gdiff = g[..., :, None] - g[..., None, :]
    decay = jnp.where(causal, jnp.exp(jnp.where(causal, gdiff, 0.0)), 0.0)
    k_beta = k * beta[..., None]
    L = jnp.where(strict, jnp.einsum('bhnid,bhnjd->bhnij', k_beta, k) * decay, 0.0)
    rhs = jnp.concatenate([v * beta[..., None], k_beta * jnp.exp(g)[..., None]], axis=-1)
    sol = lax.linalg.triangular_solve(L + jnp.eye(C, dtype=L.dtype), rhs, left_side=True,
                                      lower=True, unit_diagonal=True)
    u, w = sol[..., :dv], sol[..., dv:]
    a_intra = jnp.einsum('bhnid,bhnjd->bhnij', q, k) * decay
    q_dec = q * jnp.exp(g)[..., None]
    g_last = g[..., -1]
    k_dec = k * jnp.exp(g_last[..., None] - g)[..., None]

    def step(state, xs):
        w_c, u_c, qd_c, a_c, kd_c, gl_c = xs
        v_new = u_c - jnp.einsum('bhcd,bhde->bhce', w_c, state)
        o = jnp.einsum('bhcd,bhde->bhce', qd_c, state) + jnp.einsum('bhij,bhje->bhie', a_c, v_new)
        state = state * jnp.exp(gl_c)[..., None, None] + jnp.einsum('bhcd,bhce->bhde', kd_c, v_new)
        return state, o

    xs = (jnp.moveaxis(w, 2, 0), jnp.moveaxis(u, 2, 0), jnp.moveaxis(q_dec, 2, 0),
          jnp.moveaxis(a_intra, 2, 0), jnp.moveaxis(k_dec, 2, 0), jnp.moveaxis(g_last, 2, 0))
    state0 = jnp.zeros((B, H, dk, dv), jnp.float32)
    _, o = lax.scan(step, state0, xs)
    return o.transpose(1, 0, 3, 2, 4).reshape(B, S, H, dv)


def hybrid_token_mixer(h, w_in, swa_sinks, dn_conv_w, dn_a_log, dn_dt_bias, dn_norm, w_out,
                       slopes_swa, slopes_moba):
    B, S, _ = h.shape
    proj = h @ w_in
    split_at = np.cumsum(IN_SPLIT_SIZES)[:-1].tolist()
    qa, ka, va, qb, kb, vb, qkv_c, z_c, b_c, a_c = jnp.split(proj, split_at, axis=-1)
    o_a = swa_sink_attention(qa.reshape(B, S, SWA_HEADS, HEAD_DIM),
                             ka.reshape(B, S, SWA_KV_HEADS, HEAD_DIM),
                             va.reshape(B, S, SWA_KV_HEADS, HEAD_DIM), swa_sinks, slopes_swa)
    o_b = moba_attention(qb.reshape(B, S, MOBA_HEADS, HEAD_DIM), kb.reshape(B, S, MOBA_HEADS, HEAD_DIM),
                         vb.reshape(B, S, MOBA_HEADS, HEAD_DIM), slopes_moba)
    qkv_c = jax.nn.silu(causal_depthwise_conv(qkv_c, dn_conv_w)).astype(jnp.float32)
    qc, kc, vc = jnp.split(qkv_c, 3, axis=-1)
    qc = l2_norm(qc.reshape(B, S, DN_HEADS, DN_HEAD_DIM))
    kc = l2_norm(kc.reshape(B, S, DN_HEADS, DN_HEAD_DIM))
    vc = vc.reshape(B, S, DN_HEADS, DN_HEAD_DIM)
    beta = jax.nn.sigmoid(b_c.astype(jnp.float32))
    g = -jnp.exp(dn_a_log.astype(jnp.float32)) * jax.nn.softplus(a_c.astype(jnp.float32) + dn_dt_bias.astype(jnp.float32))
    o_c = gated_delta_rule(qc, kc, vc, g, beta)
    o_c = rms_norm(o_c, dn_norm) * jax.nn.silu(z_c.astype(jnp.float32).reshape(B, S, DN_HEADS, DN_HEAD_DIM))
    mixed = jnp.concatenate([o_a.reshape(B, S, SWA_Q), o_b.reshape(B, S, MOBA_W),
                             o_c.reshape(B, S, DN_W).astype(h.dtype)], axis=-1)
    return mixed @ w_out


def conv_glu_ffn(h, w_up, conv_w, conv_b, w_down):
    u = causal_depthwise_conv(h @ w_up, conv_w) + conv_b
    gate, up = jnp.split(u, 2, axis=-1)
    return (jax.nn.gelu(gate, approximate=True) * up) @ w_down


def setup_inputs(seed: int = 0) -> dict:
    key = jax.random.key(seed)
    ks = jax.random.split(key, 16)
    f32 = jnp.float32

    def nrm(k, shape, scale):
        return jax.random.normal(k, shape, f32) * scale

    dt = jnp.exp(jax.random.uniform(ks[6], (DEPTH, DN_HEADS), f32)
                 * (math.log(0.1) - math.log(0.001)) + math.log(0.001))
    return {
        'x': nrm(ks[0], (BATCH, SEQ, D_MODEL), 1.0),
        'mix_pre_norm': 1.0 + nrm(ks[1], (DEPTH, D_MODEL), 0.05),
        'w_in': nrm(ks[2], (DEPTH, D_MODEL, IN_WIDTH), D_MODEL ** -0.5),
        'swa_sinks': nrm(ks[3], (DEPTH, SWA_HEADS), 0.5),
        'dn_conv_w': nrm(ks[4], (DEPTH, DN_CONV, 3 * DN_W), DN_CONV ** -0.5),
        'dn_a_log': jnp.log(jax.random.uniform(ks[5], (DEPTH, DN_HEADS), f32, 1.0, 16.0)),
        'dn_dt_bias': dt + jnp.log(-jnp.expm1(-dt)),
        'dn_norm': 1.0 + nrm(ks[7], (DEPTH, DN_HEAD_DIM), 0.05),
        'w_out': nrm(ks[8], (DEPTH, MIX_WIDTH, D_MODEL), MIX_WIDTH ** -0.5),
        'mix_post_norm': 1.0 + nrm(ks[9], (DEPTH, D_MODEL), 0.05),
        'ffn_pre_norm': 1.0 + nrm(ks[10], (DEPTH, D_MODEL), 0.05),
        'w_up': nrm(ks[11], (DEPTH, D_MODEL, 2 * D_FF), D_MODEL ** -0.5),
        'ffn_conv_w': nrm(ks[12], (DEPTH, FFN_CONV, 2 * D_FF), FFN_CONV ** -0.5),
        'ffn_conv_b': nrm(ks[13], (DEPTH, 2 * D_FF), 0.02),
        'w_down': nrm(ks[14], (DEPTH, D_FF, D_MODEL), D_FF ** -0.5),
        'ffn_post_norm': 1.0 + nrm(ks[15], (DEPTH, D_MODEL), 0.05),
    }


def reference(x, mix_pre_norm, w_in, swa_sinks, dn_conv_w, dn_a_log, dn_dt_bias, dn_norm, w_out,
              mix_post_norm, ffn_pre_norm, w_up, ffn_conv_w, ffn_conv_b, w_down, ffn_post_norm):
    slopes_swa, slopes_moba = alibi_slopes()
    for l in range(DEPTH):
        h = rms_norm(x, mix_pre_norm[l])
        mix = hybrid_token_mixer(h, w_in[l], swa_sinks[l], dn_conv_w[l], dn_a_log[l], dn_dt_bias[l],
                                 dn_norm[l], w_out[l], slopes_swa, slopes_moba)
        x = x + rms_norm(mix, mix_post_norm[l])
        h = rms_norm(x, ffn_pre_norm[l])
        x = x + rms_norm(conv_glu_ffn(h, w_up[l], ffn_conv_w[l], ffn_conv_b[l], w_down[l]), ffn_post_norm[l])
    return x
```

```python
import numpy as np
from contextlib import ExitStack
import concourse.bass as bass
import concourse.mybir as mybir
from concourse.bass_utils import run_bass_kernel_spmd

F32 = mybir.dt.float32
BF16 = mybir.dt.bfloat16
I32 = mybir.dt.int32
ALU = mybir.AluOpType
AF = mybir.ActivationFunctionType
AX = mybir.AxisListType

PE, ACT, DVE, POOL, SP = "tensor", "scalar", "vector", "gpsimd", "sync"
COMPUTE = (PE, ACT, DVE, POOL)
DMA_POOL = 12

D = 1024
DFF = 2816
INW = 3336
NFM = 2432
NTM = 904
EPS = 1e-6
NEG = -30000.0
STGW = 1408


_UID = [0]


def U(name):
    _UID[0] += 1
    return "%s_%d" % (name, _UID[0])


class Buf:
    __slots__ = ("name", "w", "r")

    def __init__(self, name=""):
        self.name = name
        self.w = None
        self.r = {}


class Sched:
    def __init__(self):
        self.ops = {e: [] for e in (PE, ACT, DVE, POOL, SP)}
        self.cnt = {}
        self.waited = {e: {} for e in self.ops}
        self.dma_rr = {e: 0 for e in self.ops}
        self.n_ops = 0

    def _deps(self, eng, reads, writes, sew):
        deps = {}

        def add(t):
            if t is None:
                return
            sk, v = t
            if deps.get(sk, 0) < v:
                deps[sk] = v
        for b in reads:
            add(b.w)
        for b in writes:
            add(b.w)
            for sk, v in b.r.items():
                add((sk, v))
        out = []
        for sk, v in deps.items():
            if sk == eng and not sew:
                continue
            if self.waited[eng].get(sk, 0) >= v:
                continue
            self.waited[eng][sk] = v
            out.append((sk, v))
        return out

    def _mark(self, ticket, reads, writes):
        sk, v = ticket
        for b in reads:
            if b.r.get(sk, 0) < v:
                b.r[sk] = v
        for b in writes:
            b.w = ticket
            b.r = {}

    def op(self, eng, fn, reads=(), writes=(), inc=True, sew=None):
        if sew is None:
            sew = eng != PE
        waits = self._deps(eng, reads, writes, sew)
        c = self.cnt.get(eng, 0)
        ticket = (eng, c + 1)
        if inc:
            self.cnt[eng] = c + 1
        self._mark(ticket, reads, writes)
        self.ops[eng].append((waits, fn, eng if inc else None, 1))
        self.n_ops += 1

    def dma(self, q, out_ap, in_ap, reads=(), writes=(), **kw):
        i = self.dma_rr[q]
        self.dma_rr[q] = (i + 1) % DMA_POOL
        sk = "dma_%s_%d" % (q, i)
        c = self.cnt.get(sk, 0)
        waits = self._deps(q, reads, writes, True)
        if c > 0 and self.waited[q].get(sk, 0) < c:
            self.waited[q][sk] = c
            waits.append((sk, c))
        self.cnt[sk] = c + 16
        self._mark((sk, c + 16), reads, writes)
        self.ops[q].append((waits, lambda e: e.dma_start(out=out_ap, in_=in_ap, **kw), sk, 16))
        self.n_ops += 1

    def barrier(self):
        for e in self.ops:
            waits = []
            for sk, v in self.cnt.items():
                if sk == e:
                    continue
                if self.waited[e].get(sk, 0) < v:
                    self.waited[e][sk] = v
                    waits.append((sk, v))
            if waits:
                self.ops[e].append((waits, None, None, 0))

    def flush(self, nc, sems):
        ops = self.ops
        self.ops = {e: [] for e in ops}

        def run(e, name):
            for waits, fn, sk, inc in ops[name]:
                for wsk, v in waits:
                    e.wait_ge(sems[wsk], v)
                if fn is None:
                    continue
                ins = fn(e)
                if sk is not None:
                    ins.then_inc(sems[sk], inc)
        with nc.Block() as block:
            block.sync(lambda e: run(e, SP))
            block.tensor(lambda e: run(e, PE))
            block.scalar(lambda e: run(e, ACT))
            block.vector(lambda e: run(e, DVE))
            block.gpsimd(lambda e: run(e, POOL))

    @staticmethod
    def semkeys():
        keys = list(COMPUTE)
        for q in (PE, ACT, DVE, POOL, SP):
            for i in range(DMA_POOL):
                keys.append("dma_%s_%d" % (q, i))
        return keys


class Ring:
    def __init__(self, tiles):
        self.items = [(t, Buf()) for t in tiles]
        self.i = 0

    def next(self):
        it = self.items[self.i % len(self.items)]
        self.i += 1
        return it


class Ctx:
    pass


def make_ident(S, es, nc, dt):
    identf = es.enter_context(nc.sbuf_tensor(U("identf"), [128, 128], F32))
    b = Buf()
    S.op(POOL, lambda e: e.memset(identf[:], 1.0), writes=[b])
    S.op(POOL, lambda e: e.affine_select(out=identf[:], in_=identf[:], pattern=[[-1, 128]],
                                         compare_op=ALU.is_equal, fill=0.0, base=0, channel_multiplier=1),
         reads=[b], writes=[b])
    if dt == F32:
        return identf, b
    ident = es.enter_context(nc.sbuf_tensor(U("identb"), [128, 128], BF16))
    b2 = Buf()
    S.op(POOL, lambda e: e.tensor_copy(out=ident[:], in_=identf[:]), reads=[b], writes=[b2])
    return ident, b2


def rstd_from_ss(S, ss, bss, n):
    S.op(DVE, lambda e: e.tensor_scalar(out=ss, in0=ss, scalar1=1.0 / n, scalar2=EPS, op0=ALU.mult, op1=ALU.add),
         reads=[bss], writes=[bss])
    S.op(ACT, lambda e: e.activation(out=ss, in_=ss, func=AF.Ln), reads=[bss], writes=[bss])
    S.op(ACT, lambda e: e.activation(out=ss, in_=ss, func=AF.Exp, scale=-0.5), reads=[bss], writes=[bss])


def load_weight_bf16(S, w_dram, dst, bdst, stg_ring, colmap, nk):
    for k in range(nk):
        for (s0, s1, d0) in colmap:
            n = s1 - s0
            for o in range(0, n, STGW):
                m = min(STGW, n - o)
                stg, bs = stg_ring.next()
                S.dma(SP, stg[:, 0:m], w_dram[k * 128:(k + 1) * 128, s0 + o:s0 + o + m], writes=[bs])
                S.op(POOL, lambda e, stg=stg, m=m, k=k, dd=d0 + o: e.tensor_copy(out=dst[:, k, dd:dd + m], in_=stg[:, 0:m]),
                     reads=[bs], writes=[bdst])


def norm_rows_to_bf16(S, C, xt, bx, gbc, bg, h, bh):
    junk, bj = C.junk.next()
    ss, bss = C.ss.next()
    S.op(DVE, lambda e: e.scalar_tensor_tensor(out=junk[:], in0=xt, scalar=1.0, in1=xt, op0=ALU.mult, op1=ALU.mult,
                                               accum_out=ss[:]), reads=[bx], writes=[bj, bss])
    rstd_from_ss(S, ss[:], bss, D)
    S.op(DVE, lambda e: e.scalar_tensor_tensor(out=h, in0=xt, scalar=ss[:], in1=gbc, op0=ALU.mult, op1=ALU.mult),
         reads=[bx, bss, bg], writes=[bh])


def transpose8(S, C, h, bh, dstT, bdT, col0):
    pT, bpT = C.pT.next()
    for c in range(8):
        S.op(PE, lambda e, c=c: e.transpose(out=pT[:, c, :], in_=h[:, c * 128:(c + 1) * 128], identity=C.ident[:]),
             reads=[bh, C.bident], writes=[bpT], inc=(c == 7))
    S.op(ACT, lambda e: e.copy(out=dstT[:, :, col0:col0 + 128], in_=pT[:]), reads=[bpT], writes=[bdT])


def phase_A(S, nc, sems, T, l, x_src):
    SL = T.SL
    with ExitStack() as es:
        def sb(name, shape, dt):
            return es.enter_context(nc.sbuf_tensor(U(name), shape, dt))

        def ps(name, shape, dt):
            return es.enter_context(nc.psum_tensor(U(name), shape, dt))
        C = Ctx()
        C.ident, C.bident = make_ident(S, es, nc, BF16)
        wfm = sb("wfm", [128, 8, NFM], BF16); bwfm = Buf()
        wtm = sb("wtm", [128, 8, NTM], BF16); bwtm = Buf()
        stg = Ring([sb("stg%d" % i, [128, STGW], F32) for i in range(2)])
        gbc = sb("gbc", [128, D], F32); bg = Buf()
        C.junk = Ring([sb("junk", [128, D], BF16)])
        C.ss = Ring([sb("ss%d" % i, [128, 1], F32) for i in range(4)])
        C.pT = Ring([ps("pT%d" % i, [128, 8, 128], BF16) for i in range(2)])
        xts = Ring([sb("xt%d" % i, [128, D], F32) for i in range(2)])
        hs = Ring([sb("h%d" % i, [128, D], BF16) for i in range(2)])
        hTs = Ring([sb("hT%d" % i, [128, 8, 512], BF16) for i in range(2)])
        pfm = Ring([ps("pfm%d" % i, [128, 512], F32) for i in range(2)])
        ptm = Ring([ps("ptm%d" % i, [128, 1024], F32) for i in range(2)])
        ofm = Ring([sb("ofm%d" % i, [128, 512], F32) for i in range(4)])
        otm = Ring([sb("otm%d" % i, [128, NTM], F32) for i in range(2)])

        S.dma(SP, gbc[:], T.mix_pre_norm[l].partition_broadcast(128), writes=[bg])
        w = T.w_in[l]
        load_weight_bf16(S, w, wfm, bwfm, stg, [(0, 384, 0), (512, 1024, 384), (1280, 2816, 896)], 8)
        load_weight_bf16(S, w, wtm, bwtm, stg, [(384, 512, 0), (1024, 1280, 128), (2816, 3336, 384)], 8)

        for g in range(SL // 512):
            hT, bhT = hTs.next()
            for i in range(4):
                t0 = g * 512 + i * 128
                xt, bx = xts.next()
                S.dma(SP, xt[:], x_src[t0:t0 + 128, :], writes=[bx])
                h, bh = hs.next()
                norm_rows_to_bf16(S, C, xt[:], bx, gbc[:], bg, h[:], bh)
                transpose8(S, C, h, bh, hT, bhT, i * 128)
            for i in range(4):
                t0 = g * 512 + i * 128
                p, bp = ptm.next()
                for (n0, n1) in ((0, 512), (512, NTM)):
                    for c in range(8):
                        S.op(PE, lambda e, c=c, n0=n0, n1=n1, p=p, i=i, hT=hT: e.matmul(
                            p[:, n0:n1], lhsT=hT[:, c, i * 128:(i + 1) * 128], rhs=wtm[:, c, n0:n1],
                            start=(c == 0), stop=(c == 7)), reads=[bhT, bwtm], writes=[bp], inc=(c == 7))
                o, bo = otm.next()
                S.op(ACT, lambda e, o=o, p=p: e.copy(out=o[:], in_=p[:, 0:NTM]), reads=[bp], writes=[bo])
                S.dma(SP, T.projTM[t0:t0 + 128, :], o[:], reads=[bo])
            for ch in range(NFM // 128):
                p, bp = pfm.next()
                for c in range(8):
                    S.op(PE, lambda e, c=c, ch=ch, p=p, hT=hT: e.matmul(
                        p[:], lhsT=wfm[:, c, ch * 128:(ch + 1) * 128], rhs=hT[:, c, :],
                        start=(c == 0), stop=(c == 7)), reads=[bhT, bwfm], writes=[bp], inc=(c == 7))
                o, bo = ofm.next()
                eng = ACT if ch % 2 == 0 else DVE
                if eng == ACT:
                    S.op(ACT, lambda e, o=o, p=p: e.copy(out=o[:], in_=p[:]), reads=[bp], writes=[bo])
                else:
                    S.op(DVE, lambda e, o=o, p=p: e.tensor_copy(out=o[:], in_=p[:]), reads=[bp], writes=[bo])
                S.dma(SP, T.projT[ch * 128:(ch + 1) * 128, g * 512:(g + 1) * 512], o[:], reads=[bo])
        S.barrier()
        S.flush(nc, sems)


def load_rows_T(S, C, nc, es, src2d, nrows, ncols, name):
    nch = ncols // 128
    rows = es.enter_context(nc.sbuf_tensor(U(name + "_rows"), [8, ncols], F32))
    br = Buf()
    S.dma(SP, rows[0:nrows, :], src2d, writes=[br])
    out = es.enter_context(nc.sbuf_tensor(U(name), [128, nch, nrows], F32))
    bo = Buf()
    for c0 in range(0, nch, 16):
        n = min(16, nch - c0)
        pt, bpt = C.pmisc.next()
        for j in range(n):
            c = c0 + j
            S.op(PE, lambda e, c=c, j=j, pt=pt: e.transpose(out=pt[:, j * nrows:(j + 1) * nrows],
                                                            in_=rows[0:nrows, c * 128:(c + 1) * 128],
                                                            identity=C.identf[0:nrows, 0:nrows]),
                 reads=[br, C.bidentf], writes=[bpt], inc=(j == n - 1))
        S.op(DVE, lambda e, c0=c0, n=n, pt=pt: e.tensor_copy(
            out=out[:, c0:c0 + n, :], in_=pt[:, 0:n * nrows].rearrange("p (c k) -> p c k", k=nrows)),
            reads=[bpt], writes=[bo])
    return out, bo


def phase_C1(S, nc, sems, T, l, x_src):
    SL = T.SL
    with ExitStack() as es:
        def sb(name, shape, dt):
            return es.enter_context(nc.sbuf_tensor(U(name), shape, dt))

        def ps(name, shape, dt):
            return es.enter_context(nc.psum_tensor(U(name), shape, dt))
        C = Ctx()
        C.ident, C.bident = make_ident(S, es, nc, BF16)
        identf = sb("identf2", [128, 128], F32); bidf = Buf()
        S.op(POOL, lambda e: e.memset(identf[:], 1.0), writes=[bidf])
        S.op(POOL, lambda e: e.affine_select(out=identf[:], in_=identf[:], pattern=[[-1, 128]],
                                             compare_op=ALU.is_equal, fill=0.0, base=0, channel_multiplier=1),
             reads=[bidf], writes=[bidf])
        C.identf, C.bidentf = identf, bidf
        C.pmisc = Ring([ps("pmisc", [128, 512], F32)])
        cw = sb("cw", [128, 44, 4], F32); bcw = Buf()
        with nc.sbuf_tensor(U("crow"), [8, 2 * DFF], F32) as crow:
            bcr = Buf()
            S.dma(SP, crow[0:3, :], T.ffn_conv_w[l], writes=[bcr])
            S.dma(SP, crow[3:4, :], T.ffn_conv_b[l:l + 1, :], writes=[bcr])
            for c0 in range(0, 44, 22):
                pt, bpt = C.pmisc.next()
                for j in range(22):
                    c = c0 + j
                    S.op(PE, lambda e, c=c, j=j, pt=pt: e.transpose(out=pt[:, j * 4:(j + 1) * 4], in_=crow[0:4, c * 128:(c + 1) * 128],
                                                                    identity=identf[0:4, 0:4]),
                         reads=[bcr, bidf], writes=[bpt], inc=(j == 21))
                S.op(DVE, lambda e, c0=c0, pt=pt: e.tensor_copy(out=cw[:, c0:c0 + 22, :],
                                                                in_=pt[:, 0:88].rearrange("p (c k) -> p c k", k=4)),
                     reads=[bpt], writes=[bcw])
            S.barrier()
            S.flush(nc, sems)
        wout = sb("wout", [128, 8, D], BF16); bwout = Buf()
        wup = sb("wup", [128, 8, 2 * DFF], BF16); bwup = Buf()
        stg = Ring([sb("stg%d" % i, [128, STGW], F32) for i in range(2)])
        gpost = sb("gpost", [128, D], F32); bgp = Buf()
        gpre = sb("gpre", [128, D], F32); bgq = Buf()
        C.junk = Ring([sb("junk", [128, D], BF16)])
        C.ss = Ring([sb("ss%d" % i, [128, 1], F32) for i in range(4)])
        C.pT = Ring([ps("pT%d" % i, [128, 8, 128], BF16) for i in range(2)])
        py = Ring([ps("py", [128, D], F32)])
        pup = Ring([ps("pup%d" % i, [128, 512], F32) for i in range(3)])
        mts = Ring([sb("mt%d" % i, [128, D], BF16) for i in range(2)])
        mTs = Ring([sb("mT%d" % i, [128, 8, 128], BF16) for i in range(2)])
        xts = Ring([sb("xt%d" % i, [128, D], F32) for i in range(2)])
        x1s = Ring([sb("x1%d" % i, [128, D], F32) for i in range(2)])
        hs = Ring([sb("h%d" % i, [128, D], BF16) for i in range(2)])
        hTs = Ring([sb("hT%d" % i, [128, 8, 512], BF16) for i in range(2)])
        ubs = Ring([sb("ub%d" % i, [128, 514], F32) for i in range(3)])
        accs = Ring([sb("acc%d" % i, [128, 512], F32) for i in range(3)])
        ggs = Ring([sb("gg%d" % i, [128, 512], F32) for i in range(2)])
        gos = Ring([sb("go%d" % i, [128, 512], BF16) for i in range(3)])
        halo = sb("halo", [128, 44, 2], F32); bhalo = [Buf() for _ in range(44)]

        S.dma(SP, gpost[:], T.mix_post_norm[l].partition_broadcast(128), writes=[bgp])
        S.dma(SP, gpre[:], T.ffn_pre_norm[l].partition_broadcast(128), writes=[bgq])
        S.op(POOL, lambda e: e.memset(halo[:], 0.0), writes=bhalo)
        load_weight_bf16(S, T.w_out[l], wout, bwout, stg, [(0, D, 0)], 8)
        load_weight_bf16(S, T.w_up[l], wup, bwup, stg, [(0, 2 * DFF, 0)], 8)

        for g in range(SL // 512):
            hT, bhT = hTs.next()
            for i in range(4):
                t0 = g * 512 + i * 128
                mt, bmt = mts.next()
                S.dma(SP, mt[:], T.mixed[t0:t0 + 128, :], writes=[bmt])
                mT, bmT = mTs.next()
                transpose8(S, C, mt, bmt, mT, bmT, 0)
                xt, bx = xts.next()
                S.dma(SP, xt[:], x_src[t0:t0 + 128, :], writes=[bx])
                p, bp = py.next()
                for nb in range(2):
                    for c in range(8):
                        S.op(PE, lambda e, c=c, nb=nb, p=p, mT=mT: e.matmul(
                            p[:, nb * 512:(nb + 1) * 512], lhsT=mT[:, c, :], rhs=wout[:, c, nb * 512:(nb + 1) * 512],
                            start=(c == 0), stop=(c == 7)), reads=[bmT, bwout], writes=[bp], inc=(c == 7))
                junk, bj = C.junk.next()
                ss, bss = C.ss.next()
                S.op(ACT, lambda e, junk=junk, p=p, ss=ss: e.activation(out=junk[:], in_=p[:], func=AF.Square, accum_out=ss[:]),
                     reads=[bp], writes=[bj, bss])
                rstd_from_ss(S, ss[:], bss, D)
                x1, bx1 = x1s.next()
                S.op(DVE, lambda e, x1=x1, p=p, ss=ss: e.scalar_tensor_tensor(out=x1[:], in0=p[:], scalar=ss[:], in1=gpost[:],
                                                                             op0=ALU.mult, op1=ALU.mult),
                     reads=[bp, bss, bgp], writes=[bx1])
                S.op(DVE, lambda e, x1=x1, xt=xt: e.tensor_tensor(out=x1[:], in0=x1[:], in1=xt[:], op=ALU.add),
                     reads=[bx1, bx], writes=[bx1])
                S.dma(SP, T.xres1[t0:t0 + 128, :], x1[:], reads=[bx1])
                h, bh = hs.next()
                norm_rows_to_bf16(S, C, x1[:], bx1, gpre[:], bgq, h[:], bh)
                transpose8(S, C, h, bh, hT, bhT, i * 128)
            for f in range(22):
                accp = []
                for part in range(2):
                    ch = part * 22 + f
                    p, bp = pup.next()
                    for c in range(8):
                        S.op(PE, lambda e, c=c, ch=ch, p=p, hT=hT: e.matmul(
                            p[:], lhsT=wup[:, c, ch * 128:(ch + 1) * 128], rhs=hT[:, c, :],
                            start=(c == 0), stop=(c == 7)), reads=[bhT, bwup], writes=[bp], inc=(c == 7))
                    ub, bub = ubs.next()
                    S.op(POOL, lambda e, ub=ub, ch=ch: e.tensor_copy(out=ub[:, 0:2], in_=halo[:, ch, :]),
                         reads=[bhalo[ch]], writes=[bub])
                    S.op(ACT, lambda e, ub=ub, p=p: e.copy(out=ub[:, 2:514], in_=p[:]), reads=[bp], writes=[bub])
                    S.op(POOL, lambda e, ub=ub, ch=ch: e.tensor_copy(out=halo[:, ch, :], in_=ub[:, 512:514]),
                         reads=[bub], writes=[bhalo[ch]])
                    acc, bacc = accs.next()
                    S.op(DVE, lambda e, acc=acc, ub=ub, ch=ch: e.tensor_scalar(
                        out=acc[:], in0=ub[:, 2:514], scalar1=cw[:, ch, 2:3], scalar2=cw[:, ch, 3:4], op0=ALU.mult, op1=ALU.add),
                        reads=[bub, bcw], writes=[bacc])
                    S.op(DVE, lambda e, acc=acc, ub=ub, ch=ch: e.scalar_tensor_tensor(
                        out=acc[:], in0=ub[:, 1:513], scalar=cw[:, ch, 1:2], in1=acc[:], op0=ALU.mult, op1=ALU.add),
                        reads=[bub, bcw, bacc], writes=[bacc])
                    S.op(DVE, lambda e, acc=acc, ub=ub, ch=ch: e.scalar_tensor_tensor(
                        out=acc[:], in0=ub[:, 0:512], scalar=cw[:, ch, 0:1], in1=acc[:], op0=ALU.mult, op1=ALU.add),
                        reads=[bub, bcw, bacc], writes=[bacc])
                    accp.append((acc, bacc))
                gg, bgg = ggs.next()
                S.op(ACT, lambda e, gg=gg, a=accp[0][0]: e.activation(out=gg[:], in_=a[:], func=AF.Gelu_apprx_tanh),
                     reads=[accp[0][1]], writes=[bgg])
                go, bgo = gos.next()
                S.op(POOL, lambda e, go=go, gg=gg, a=accp[1][0]: e.tensor_tensor(out=go[:], in0=gg[:], in1=a[:], op=ALU.mult),
                     reads=[bgg, accp[1][1]], writes=[bgo])
                S.dma(SP, T.gsc[f * 128:(f + 1) * 128, g * 512:(g + 1) * 512], go[:], reads=[bgo])
        S.barrier()
        S.flush(nc, sems)


def phase_C2(S, nc, sems, T, l, x_dst):
    SL = T.SL
    with ExitStack() as es:
        def sb(name, shape, dt):
            return es.enter_context(nc.sbuf_tensor(U(name), shape, dt))

        def ps(name, shape, dt):
            return es.enter_context(nc.psum_tensor(U(name), shape, dt))
        C = Ctx()
        wdn = sb("wdn", [128, 22, D], BF16); bwdn = Buf()
        stg = Ring([sb("stg%d" % i, [128, STGW], F32) for i in range(2)])
        gpost = sb("gpost", [128, D], F32); bgp = Buf()
        C.junk = Ring([sb("junk", [128, D], BF16)])
        C.ss = Ring([sb("ss%d" % i, [128, 1], F32) for i in range(4)])
        py = Ring([ps("py%d" % i, [128, D], F32) for i in range(2)])
        gTs = Ring([sb("gT%d" % i, [128, 22, 512], BF16) for i in range(2)])
        xts = Ring([sb("xt%d" % i, [128, D], F32) for i in range(2)])
        x2s = Ring([sb("x2%d" % i, [128, D], F32) for i in range(2)])
        S.dma(SP, gpost[:], T.ffn_post_norm[l].partition_broadcast(128), writes=[bgp])
        load_weight_bf16(S, T.w_down[l], wdn, bwdn, stg, [(0, D, 0)], 22)
        for g in range(SL // 512):
            gT, bgT = gTs.next()
            S.dma(SP, gT[:], T.gsc[:, g * 512:(g + 1) * 512].rearrange("(c p) t -> p c t", p=128), writes=[bgT])
            for i in range(4):
                t0 = g * 512 + i * 128
                xt, bx = xts.next()
                S.dma(SP, xt[:], T.xres1[t0:t0 + 128, :], writes=[bx])
                p, bp = py.next()
                for nb in range(2):
                    for f in range(22):
                        S.op(PE, lambda e, f=f, nb=nb, p=p, gT=gT, i=i: e.matmul(
                            p[:, nb * 512:(nb + 1) * 512], lhsT=gT[:, f, i * 128:(i + 1) * 128],
                            rhs=wdn[:, f, nb * 512:(nb + 1) * 512], start=(f == 0), stop=(f == 21)),
                            reads=[bgT, bwdn], writes=[bp], inc=(f == 21))
                junk, bj = C.junk.next()
                ss, bss = C.ss.next()
                S.op(ACT, lambda e, junk=junk, p=p, ss=ss: e.activation(out=junk[:], in_=p[:], func=AF.Square, accum_out=ss[:]),
                     reads=[bp], writes=[bj, bss])
                rstd_from_ss(S, ss[:], bss, D)
                x2, bx2 = x2s.next()
                S.op(DVE, lambda e, x2=x2, p=p, ss=ss: e.scalar_tensor_tensor(out=x2[:], in0=p[:], scalar=ss[:], in1=gpost[:],
                                                                             op0=ALU.mult, op1=ALU.mult),
                     reads=[bp, bss, bgp], writes=[bx2])
                S.op(DVE, lambda e, x2=x2, xt=xt: e.tensor_tensor(out=x2[:], in0=x2[:], in1=xt[:], op=ALU.add),
                     reads=[bx2, bx], writes=[bx2])
                S.dma(SP, x_dst[t0:t0 + 128, :], x2[:], reads=[bx2])
        S.barrier()
        S.flush(nc, sems)


SLOPES_SWA = [2.0 ** -1, 2.0 ** -3, 2.0 ** -5, 2.0 ** -7]
SLOPES_MOBA = [2.0 ** -2, 2.0 ** -4, 2.0 ** -6, 2.0 ** -8]


def make_rel(S, es, nc, ncols):
    ri = es.enter_context(nc.sbuf_tensor(U("reli"), [128, ncols], I32))
    rf = es.enter_context(nc.sbuf_tensor(U("relf"), [128, ncols], F32))
    b = Buf()
    S.op(POOL, lambda e: e.iota(ri[:], pattern=[[1, ncols]], base=0, channel_multiplier=-1), writes=[b])
    S.op(POOL, lambda e: e.tensor_copy(out=rf[:], in_=ri[:]), reads=[b], writes=[b])
    return rf, b


def phase_SWA(S, nc, sems, T, l):
    SL = T.SL
    scale = 64 ** -0.5
    with ExitStack() as es:
        def sb(name, shape, dt):
            return es.enter_context(nc.sbuf_tensor(U(name), shape, dt))

        def ps(name, shape, dt):
            return es.enter_context(nc.psum_tensor(U(name), shape, dt))
        rel, brel = make_rel(S, es, nc, 128)
        bias = [sb("bias%d" % j, [128, 2, 2, 128], F32) for j in range(2)]
        bbias = [Buf(), Buf()]
        for j in range(2):
            for g in range(2):
                m = SLOPES_SWA[2 * j + g]
                S.op(POOL, lambda e, j=j, g=g, m=m: e.tensor_scalar(out=bias[j][:, 1, g, :], in0=rel[:], scalar1=-m, scalar2=None,
                                                                    op0=ALU.mult), reads=[brel], writes=[bbias[j]])
                S.op(POOL, lambda e, j=j, g=g: e.affine_select(out=bias[j][:, 1, g, :], in_=bias[j][:, 1, g, :], pattern=[[1, 128]],
                                                               compare_op=ALU.is_ge, fill=NEG, base=0, channel_multiplier=-1),
                     reads=[bbias[j]], writes=[bbias[j]])
                S.op(POOL, lambda e, j=j, g=g, m=m: e.tensor_scalar(out=bias[j][:, 0, g, :], in0=rel[:], scalar1=-m, scalar2=-128.0 * m,
                                                                    op0=ALU.mult, op1=ALU.add), reads=[brel], writes=[bbias[j]])
                S.op(POOL, lambda e, j=j, g=g: e.affine_select(out=bias[j][:, 0, g, :], in_=bias[j][:, 0, g, :], pattern=[[-1, 128]],
                                                               compare_op=ALU.is_ge, fill=NEG, base=-1, channel_multiplier=1),
                     reads=[bbias[j]], writes=[bbias[j]])
        biasz = [sb("biasz%d" % j, [128, 2, 2, 128], F32) for j in range(2)]
        for j in range(2):
            S.op(POOL, lambda e, j=j: e.tensor_copy(out=biasz[j][:, 1], in_=bias[j][:, 1]), reads=[bbias[j]], writes=[bbias[j]])
            S.op(POOL, lambda e, j=j: e.memset(biasz[j][:, 0], NEG), writes=[bbias[j]])
        esink = sb("esink", [128, 4], F32); bes = Buf()
        S.dma(SP, esink[:], T.swa_sinks[l].partition_broadcast(128), writes=[bes])
        S.op(ACT, lambda e: e.activation(out=esink[:], in_=esink[:], func=AF.Exp), reads=[bes], writes=[bes])
        qfs = Ring([sb("qf%d" % i, [64, 4, 512], F32) for i in range(2)])
        kfs = Ring([sb("kf%d" % i, [64, 2, 640], F32) for i in range(2)])
        vfs = Ring([sb("vf%d" % i, [128, 5, 128], F32) for i in range(2)])
        qbs = Ring([sb("qb%d" % i, [64, 4, 512], BF16) for i in range(2)])
        kbs = Ring([sb("kb%d" % i, [64, 2, 640], BF16) for i in range(2)])
        vbs = Ring([sb("vb%d" % i, [128, 5, 2, 65], BF16) for i in range(2)])
        scs = Ring([ps("sc%d" % i, [128, 2, 2, 128], F32) for i in range(2)])
        pos = Ring([ps("po%d" % i, [128, 2, 65], F32) for i in range(2)])
        s2s = Ring([sb("s2%d" % i, [128, 2, 2, 128], F32) for i in range(2)])
        pbs = Ring([sb("pb%d" % i, [128, 2, 2, 128], BF16) for i in range(2)])
        dens = Ring([sb("den%d" % i, [128, 2], F32) for i in range(2)])
        oms = Ring([sb("om%d" % i, [128, 256], BF16) for i in range(2)])
        for g in range(SL // 512):
            c0 = g * 512
            qf, bqf = qfs.next(); kf, bkf = kfs.next(); vf, bvf = vfs.next()
            qb, bqb = qbs.next(); kb, bkb = kbs.next(); vb, bvb = vbs.next()
            S.dma(SP, qf[:], T.projT[0:256, c0:c0 + 512].rearrange("(h d) t -> d h t", d=64), writes=[bqf])
            if g == 0:
                S.op(POOL, lambda e, kf=kf: e.memset(kf[:, :, 0:128], 0.0), writes=[bkf])
                S.op(POOL, lambda e, vf=vf: e.memset(vf[:, 0, :], 0.0), writes=[bvf])
                S.dma(SP, kf[:, :, 128:640], T.projT[256:384, c0:c0 + 512].rearrange("(h d) t -> d h t", d=64), writes=[bkf])
                S.dma(SP, vf[:, 1:5, :], T.projTM[c0:c0 + 512, 0:128].rearrange("(n p) c -> p n c", p=128), writes=[bvf])
            else:
                S.dma(SP, kf[:], T.projT[256:384, c0 - 128:c0 + 512].rearrange("(h d) t -> d h t", d=64), writes=[bkf])
                S.dma(SP, vf[:], T.projTM[c0 - 128:c0 + 512, 0:128].rearrange("(n p) c -> p n c", p=128), writes=[bvf])
            S.op(POOL, lambda e, qb=qb, qf=qf: e.tensor_copy(out=qb[:], in_=qf[:]), reads=[bqf], writes=[bqb])
            S.op(POOL, lambda e, kb=kb, kf=kf: e.tensor_copy(out=kb[:], in_=kf[:]), reads=[bkf], writes=[bkb])
            S.op(POOL, lambda e, vb=vb: e.memset(vb[:], 1.0), writes=[bvb])
            S.op(POOL, lambda e, vb=vb, vf=vf: e.tensor_copy(out=vb[:, :, :, 0:64], in_=vf[:].rearrange("p n (j d) -> p n j d", d=64)),
                 reads=[bvf], writes=[bvb])
            for i in range(4):
                t = g * 4 + i
                cks = [0, 1]
                bsel = biasz if t == 0 else bias
                om, bom = oms.next()
                for j in range(2):
                    sc, bsc = scs.next()
                    for ck in cks:
                        S.op(PE, lambda e, sc=sc, ck=ck, j=j, i=i, kb=kb, qb=qb: e.matmul(
                            sc[:, ck, :, :], lhsT=kb[:, j, (i + ck) * 128:(i + ck + 1) * 128],
                            rhs=qb[:, 2 * j:2 * j + 2, i * 128:(i + 1) * 128], start=True, stop=True),
                            reads=[bkb, bqb], writes=[bsc], inc=(ck == 1))
                    k0 = cks[0]
                    s2, bs2 = s2s.next()
                    S.op(DVE, lambda e, s2=s2, sc=sc, j=j, k0=k0, bsel=bsel: e.scalar_tensor_tensor(
                        out=s2[:, k0:2], in0=sc[:, k0:2], scalar=scale, in1=bsel[j][:, k0:2], op0=ALU.mult, op1=ALU.add),
                        reads=[bsc, bbias[j]], writes=[bs2])
                    pb, bpb = pbs.next()
                    S.op(ACT, lambda e, pb=pb, s2=s2, k0=k0: e.activation(out=pb[:, k0:2], in_=s2[:, k0:2], func=AF.Exp),
                         reads=[bs2], writes=[bpb])
                    po, bpo = pos.next()
                    for gg in range(2):
                        for ck in cks:
                            S.op(PE, lambda e, po=po, pb=pb, gg=gg, ck=ck, i=i, j=j, vb=vb: e.matmul(
                                po[:, gg, :], lhsT=pb[:, ck, gg, :], rhs=vb[:, i + ck, j, :], start=(ck == cks[0]), stop=(ck == 1)),
                                reads=[bpb, bvb], writes=[bpo], inc=(ck == 1 and gg == 1))
                    den, bden = dens.next()
                    S.op(DVE, lambda e, den=den, po=po, j=j: e.tensor_tensor(out=den[:], in0=po[:, :, 64], in1=esink[:, 2 * j:2 * j + 2],
                                                                             op=ALU.add), reads=[bpo, bes], writes=[bden])
                    S.op(DVE, lambda e, den=den: e.reciprocal(out=den[:], in_=den[:]), reads=[bden], writes=[bden])
                    for gg in range(2):
                        h = 2 * j + gg
                        S.op(DVE, lambda e, om=om, po=po, den=den, gg=gg, h=h: e.tensor_scalar(
                            out=om[:, h * 64:(h + 1) * 64], in0=po[:, gg, 0:64], scalar1=den[:, gg:gg + 1], scalar2=None, op0=ALU.mult),
                            reads=[bpo, bden], writes=[bom])
                S.dma(SP, T.mixed[c0 + i * 128:c0 + (i + 1) * 128, 0:256], om[:], reads=[bom])
        S.barrier()
        S.flush(nc, sems)


PRUNE = 60.0


def phase_MOBA(S, nc, sems, T, l):
    SL = T.SL
    NT = SL // 128
    NB = SL // 256
    PIECE = min(2048, SL)
    with ExitStack() as es:
        def sb(name, shape, dt):
            return es.enter_context(nc.sbuf_tensor(U(name), shape, dt))

        def ps(name, shape, dt):
            return es.enter_context(nc.psum_tensor(U(name), shape, dt))
        ji = sb("ji", [33, SL], I32); jrow = sb("jrow", [33, SL], F32); bj = Buf()
        S.op(POOL, lambda e: e.iota(ji[:], pattern=[[0, NB], [1, 256]], base=0, channel_multiplier=0), writes=[bj])
        S.op(POOL, lambda e: e.tensor_copy(out=jrow[:], in_=ji[:]), reads=[bj], writes=[bj])
        cmask = sb("cmask", [128, 2, 256], F32); bcm = Buf()
        S.op(POOL, lambda e: e.memset(cmask[:], 0.0), writes=[bcm])
        for kc in range(2):
            S.op(POOL, lambda e, kc=kc: e.affine_select(out=cmask[:, kc, :], in_=cmask[:, kc, :], pattern=[[1, 256]],
                                                        compare_op=ALU.is_ge, fill=NEG, base=-128 * kc, channel_multiplier=-1),
                 reads=[bcm], writes=[bcm])
        qaug = sb("qaug", [128, SL], BF16); bqa = Buf()
        kaug = sb("kaug", [128, SL], BF16); bka = Buf()
        vaug = sb("vaug", [128, NT, 65], BF16); bva = Buf()
        selall = sb("selall", [128, NT, 32], F32); bsel = Buf()
        kmean = sb("kmean", [128, 32], F32); bkm = Buf()
        qfs = Ring([sb("qf%d" % i, [128, PIECE], F32) for i in range(2)])
        kfs = Ring([sb("kf%d" % i, [128, PIECE], F32) for i in range(2)])
        vfs = Ring([sb("vf%d" % i, [128, PIECE // 128, 64], F32) for i in range(2)])
        gsbs = Ring([sb("gsb%d" % i, [128, 32], F32) for i in range(2)])
        top8s = Ring([sb("top8%d" % i, [128, 8], F32) for i in range(2)])
        pgate = Ring([ps("pgate%d" % i, [128, 32], F32) for i in range(2)])
        pst = Ring([ps("pst%d" % i, [128, 2, 256], F32) for i in range(3)])
        pov = Ring([ps("pov%d" % i, [128, 2, 65], F32) for i in range(3)])
        sms = Ring([sb("sm%d" % i, [128, 2, 256], F32) for i in range(2)])
        pts = Ring([sb("pt%d" % i, [128, 2, 256], BF16) for i in range(3)])
        accs = Ring([sb("acc%d" % i, [128, 2, 65], F32) for i in range(2)])
        rcs = Ring([sb("rc%d" % i, [128, 2], F32) for i in range(2)])
        oms = Ring([sb("om%d" % i, [128, 2, 64], BF16) for i in range(2)])

        S.op(POOL, lambda e: e.memset(qaug[:], 0.0), writes=[bqa])
        S.op(POOL, lambda e: e.memset(kaug[:], 0.0), writes=[bka])
        S.op(POOL, lambda e: e.memset(qaug[0:1, :], 1.0), writes=[bqa])
        S.op(POOL, lambda e: e.memset(kaug[32:33, :], 1.0), writes=[bka])
        for h in range(4):
            m = SLOPES_MOBA[h]
            S.op(POOL, lambda e, m=m: e.tensor_scalar(out=qaug[32:33, :], in0=jrow[32:33, :], scalar1=-8.0 * m, scalar2=None,
                                                      op0=ALU.mult), reads=[bj], writes=[bqa])
            S.op(POOL, lambda e, m=m: e.tensor_scalar(out=kaug[0:1, :], in0=jrow[0:1, :], scalar1=8.0 * m, scalar2=None,
                                                      op0=ALU.mult), reads=[bj], writes=[bka])
            S.op(POOL, lambda e: e.memset(vaug[:], 1.0), writes=[bva])
            S.op(POOL, lambda e: e.memset(selall[:], 0.0), writes=[bsel])
            for pc in range(SL // PIECE):
                p0 = pc * PIECE
                qf, bqf = qfs.next(); kf, bkf = kfs.next(); vf, bvf = vfs.next()
                S.dma(SP, qf[64:128, :], T.projT[384 + h * 64:384 + (h + 1) * 64, p0:p0 + PIECE], writes=[bqf])
                S.dma(SP, kf[64:128, :], T.projT[640 + h * 64:640 + (h + 1) * 64, p0:p0 + PIECE], writes=[bkf])
                S.dma(SP, vf[:], T.projTM[p0:p0 + PIECE, 128 + h * 64:128 + (h + 1) * 64].rearrange("(n p) c -> p n c", p=128),
                      writes=[bvf])
                S.op(POOL, lambda e, qf=qf, p0=p0: e.tensor_copy(out=qaug[64:128, p0:p0 + PIECE], in_=qf[64:128, :]),
                     reads=[bqf], writes=[bqa])
                S.op(ACT, lambda e, kf=kf, p0=p0: e.copy(out=kaug[64:128, p0:p0 + PIECE], in_=kf[64:128, :]),
                     reads=[bkf], writes=[bka])
                S.op(POOL, lambda e, vf=vf, p0=p0: e.tensor_copy(out=vaug[:, p0 // 128:(p0 + PIECE) // 128, 0:64], in_=vf[:]),
                     reads=[bvf], writes=[bva])
                S.op(DVE, lambda e, kf=kf, p0=p0: e.tensor_reduce(
                    out=kmean[64:128, p0 // 256:(p0 + PIECE) // 256], in_=kf[64:128, :].rearrange("p (n j) -> p n j", j=256),
                    axis=AX.X, op=ALU.add), reads=[bkf], writes=[bkm])
                for tt in range(PIECE // 128):
                    t = p0 // 128 + tt
                    own = t // 2
                    if own == 0:
                        continue
                    pg, bpg = pgate.next()
                    S.op(PE, lambda e, pg=pg, qf=qf, tt=tt, own=own: e.matmul(
                        pg[:, 0:own], lhsT=qf[64:128, tt * 128:(tt + 1) * 128], rhs=kmean[64:128, 0:own], start=True, stop=True),
                        reads=[bqf, bkm], writes=[bpg])
                    gsb, bgsb = gsbs.next()
                    S.op(POOL, lambda e, gsb=gsb: e.memset(gsb[:], -1e30), writes=[bgsb])
                    S.op(DVE, lambda e, gsb=gsb, pg=pg, own=own: e.tensor_copy(out=gsb[:, 0:own], in_=pg[:, 0:own]),
                         reads=[bpg], writes=[bgsb])
                    t8, bt8 = top8s.next()
                    S.op(DVE, lambda e, t8=t8, gsb=gsb: e.max(out=t8[:], in_=gsb[:]), reads=[bgsb], writes=[bt8])
                    S.op(DVE, lambda e, t8=t8, gsb=gsb, t=t, own=own: e.tensor_scalar(
                        out=selall[:, t, 0:own], in0=gsb[:, 0:own], scalar1=t8[:, 2:3], scalar2=None, op0=ALU.is_ge),
                        reads=[bgsb, bt8], writes=[bsel])
            for c in range(NB):
                acc, bacc = accs.next()
                blocks = [c] + [n for n in range(c - 1, -1, -1) if m * 256.0 * (c - n - 1) <= PRUNE]
                for n in blocks:
                    st, bst = pst.next()
                    for kc in range(2):
                        S.op(PE, lambda e, st=st, kc=kc, n=n, c=c: e.matmul(
                            st[:, kc, :], lhsT=kaug[:, n * 256 + kc * 128:n * 256 + (kc + 1) * 128],
                            rhs=qaug[:, c * 256:(c + 1) * 256], start=True, stop=True),
                            reads=[bka, bqa], writes=[bst], inc=(kc == 1))
                    pt, bpt = pts.next()
                    if n == c:
                        sm, bsm = sms.next()
                        S.op(DVE, lambda e, sm=sm, st=st: e.scalar_tensor_tensor(
                            out=sm[:], in0=st[:], scalar=0.125, in1=cmask[:], op0=ALU.mult, op1=ALU.add),
                            reads=[bst, bcm], writes=[bsm])
                        S.op(ACT, lambda e, pt=pt, sm=sm: e.activation(out=pt[:], in_=sm[:], func=AF.Exp), reads=[bsm], writes=[bpt])
                    else:
                        cst = -m * 256.0 * (c - n)
                        S.op(ACT, lambda e, pt=pt, st=st, cst=cst: e.activation(out=pt[:], in_=st[:], func=AF.Exp, scale=0.125, bias=cst),
                             reads=[bst], writes=[bpt])
                    ov, bov = pov.next()
                    for half in range(2):
                        for kc in range(2):
                            S.op(PE, lambda e, ov=ov, pt=pt, half=half, kc=kc, n=n: e.matmul(
                                ov[:, half, :], lhsT=pt[:, kc, half * 128:(half + 1) * 128], rhs=vaug[:, 2 * n + kc, :],
                                start=(kc == 0), stop=(kc == 1)), reads=[bpt, bva], writes=[bov], inc=(kc == 1 and half == 1))
                    if n == c:
                        S.op(DVE, lambda e, acc=acc, ov=ov: e.tensor_copy(out=acc[:], in_=ov[:]), reads=[bov], writes=[bacc])
                    else:
                        for half in range(2):
                            S.op(DVE, lambda e, acc=acc, ov=ov, half=half, c=c, n=n: e.scalar_tensor_tensor(
                                out=acc[:, half, :], in0=ov[:, half, :], scalar=selall[:, 2 * c + half, n:n + 1], in1=acc[:, half, :],
                                op0=ALU.mult, op1=ALU.add), reads=[bov, bsel, bacc], writes=[bacc])
                rc, brc = rcs.next()
                S.op(DVE, lambda e, rc=rc, acc=acc: e.reciprocal(out=rc[:], in_=acc[:, :, 64]), reads=[bacc], writes=[brc])
                om, bom = oms.next()
                for half in range(2):
                    S.op(DVE, lambda e, om=om, acc=acc, rc=rc, half=half: e.tensor_scalar(
                        out=om[:, half, :], in0=acc[:, half, 0:64], scalar1=rc[:, half:half + 1], scalar2=None, op0=ALU.mult),
                        reads=[bacc, brc], writes=[bom])
                S.dma(SP, T.mixed[c * 256:(c + 1) * 256, 256 + h * 64:256 + (h + 1) * 64].rearrange("(a p) d -> p a d", p=128),
                      om[:], reads=[bom])
        S.barrier()
        S.flush(nc, sems)


def phase_GDN(S, nc, sems, T, l):
    SL = T.SL
    DK = 128
    STOP = getattr(T, "gdn_stop", 4)
    with ExitStack() as es:
        def sb(name, shape, dt):
            return es.enter_context(nc.sbuf_tensor(U(name), shape, dt))

        def ps(name, shape, dt):
            return es.enter_context(nc.psum_tensor(U(name), shape, dt))
        C = Ctx()
        banks = [ps("bank%d" % i, [128, 512], F32) for i in range(8)]
        C.pmisc = Ring([banks[7]])
        identf, bidf = make_ident(S, es, nc, F32)
        C.identf, C.bidentf = identf, bidf
        ones = sb("ones", [128, 128], F32); bones = Buf()
        S.op(POOL, lambda e: e.memset(ones[:], 1.0), writes=[bones])
        masks = sb("masks", [128, 2, 128], F32); bmask = Buf()
        S.op(POOL, lambda e: e.memset(masks[:], 1.0), writes=[bmask])
        S.op(POOL, lambda e: e.affine_select(out=masks[:, 0, :], in_=masks[:, 0, :], pattern=[[1, 128]], compare_op=ALU.is_ge,
                                             fill=0.0, base=-1, channel_multiplier=-1), reads=[bmask], writes=[bmask])
        S.op(POOL, lambda e: e.affine_select(out=masks[:, 1, :], in_=masks[:, 1, :], pattern=[[1, 128]], compare_op=ALU.is_ge,
                                             fill=0.0, base=0, channel_multiplier=-1), reads=[bmask], writes=[bmask])
        S.op(POOL, lambda e: e.memset(masks[0:64, :, 64:128], 0.0), reads=[bmask], writes=[bmask])
        blkblk = sb("blkblk", [128, 128], F32); blk = sb("blk", [128, 2], F32); bblk = Buf()
        S.op(POOL, lambda e: e.memset(blkblk[:], 0.0), writes=[bblk])
        S.op(POOL, lambda e: e.memset(blkblk[0:64, 0:64], 1.0), writes=[bblk])
        S.op(POOL, lambda e: e.memset(blkblk[64:128, 64:128], 1.0), writes=[bblk])
        S.op(POOL, lambda e: e.memset(blk[:], 0.0), writes=[bblk])
        S.op(POOL, lambda e: e.memset(blk[0:64, 0:1], 1.0), writes=[bblk])
        S.op(POOL, lambda e: e.memset(blk[64:128, 1:2], 1.0), writes=[bblk])
        cw, bcw = load_rows_T(S, C, nc, es, T.dn_conv_w[l], 4, 1536, "dncw")
        expA = sb("expA", [128, 4], F32); bA = Buf()
        S.dma(SP, expA[:], T.dn_a_log[l].partition_broadcast(128), writes=[bA])
        S.op(ACT, lambda e: e.activation(out=expA[:], in_=expA[:], func=AF.Exp), reads=[bA], writes=[bA])
        S.op(DVE, lambda e: e.tensor_scalar(out=expA[:], in0=expA[:], scalar1=-1.0, scalar2=None, op0=ALU.mult), reads=[bA], writes=[bA])
        dtb = sb("dtb", [128, 4], F32); bdt = Buf()
        S.dma(SP, dtb[:], T.dn_dt_bias[l].partition_broadcast(128), writes=[bdt])
        wn = sb("wn", [128, 128], F32); bwn = Buf()
        S.dma(SP, wn[:], T.dn_norm[l].partition_broadcast(128), writes=[bwn])
        state = [sb("state%d" % h, [128, 128], F32) for h in range(4)]
        bstate = [Buf() for _ in range(4)]
        vn = [[sb("vn%d_%d" % (h, cc), [128, 128], F32) for cc in range(2)] for h in range(4)]
        bvn = [[Buf() for cc in range(2)] for h in range(4)]
        for h in range(4):
            S.op(POOL, lambda e, h=h: e.memset(state[h][:], 0.0), writes=[bstate[h]])
            for cc in range(2):
                S.op(POOL, lambda e, h=h, cc=cc: e.memset(vn[h][cc][:], 0.0), writes=[bvn[h][cc]])
        raws = Ring([sb("raw%d" % i, [128, 515], F32) for i in range(3)])
        caccs = Ring([sb("cacc%d" % i, [128, 512], F32) for i in range(3)])
        cts = [Ring([sb("ct%d_%d" % (i, k), [128, 512], F32) for k in range(2)]) for i in range(12)]
        sqts = [Ring([sb("sqt%d_%d" % (i, k), [128, 512], F32) for k in range(1)]) for i in range(8)]
        abs_ = Ring([sb("ab%d" % i, [128, 8], F32) for i in range(2)])
        zs = Ring([sb("z%d" % i, [128, 512], F32) for i in range(2)])
        scal = Ring([sb("scal%d" % i, [128, 96], F32) for i in range(2)])
        NSLOT = 3
        def slot_rings(k):
            R = Ctx()
            R.diags = Ring([sb("diag%d_%d" % (k, i), [128, 3, 128], F32) for i in range(1)])
            R.dmins = Ring([sb("dmin%d_%d" % (k, i), [128, 128], F32) for i in range(1)])
            R.Es = Ring([sb("E%d_%d" % (k, i), [128, 128], F32) for i in range(1)])
            R.F12s = Ring([sb("F12%d_%d" % (k, i), [128, 2, 128], F32) for i in range(1)])
            R.UAs = Ring([sb("UA%d_%d" % (k, i), [128, 2, 128], F32) for i in range(1)])
            R.Ls = Ring([sb("L%d_%d" % (k, i), [128, 128], F32) for i in range(1)])
            R.pws = Ring([sb("pw%d_%d" % (k, i), [128, 2, 128], F32) for i in range(2)])
            R.Xs = Ring([sb("X%d_%d" % (k, i), [128, 256], F32) for i in range(2)])
            R.kdecs = Ring([sb("kdec%d_%d" % (k, i), [128, 128], F32) for i in range(1)])
            R.wTs = Ring([sb("wT%d_%d" % (k, i), [128, 128], F32) for i in range(1)])
            R.oqs = Ring([sb("oq%d_%d" % (k, i), [128, 128], F32) for i in range(1)])
            R.vtmps = Ring([sb("vtmp%d_%d" % (k, i), [128, 128], F32) for i in range(1)])
            return R
        SLOTS = [slot_rings(k) for k in range(NSLOT)]
        oalls = Ring([sb("oall%d" % i, [128, 4, 128], F32) for i in range(2)])
        zsil = Ring([sb("zsil%d" % i, [128, 512], F32) for i in range(2)])
        oms = Ring([sb("om%d" % i, [128, 512], BF16) for i in range(2)])
        junkg = Ring([sb("junkg", [128, 128], F32)])
        BK = [Buf("psum_bank%d" % i) for i in range(8)]
        bA_ = banks[0]; bufA = BK[0]
        p_rb = banks[1][:, 0:384].rearrange("p (a i) -> p a i", i=128); buf_rb = BK[1]
        p_Lt = banks[1][:, 384:512]; buf_Lt = BK[1]
        p_g = banks[2][:, 0:256].rearrange("p (a i) -> p a i", i=128); buf_g = BK[2]
        p_tr = banks[2][:, 256:512].rearrange("p (a i) -> p a i", i=128); buf_tr = BK[2]
        for k_, bk_ in enumerate((3, 4, 7)):
            SLOTS[k_].p_pw = banks[bk_][:, 0:256].rearrange("p (a i) -> p a i", i=128)
            SLOTS[k_].p_app = banks[bk_][:, 256:512]
            SLOTS[k_].bchain = BK[bk_]
        p_wq = banks[5][:, 256:512].rearrange("p (a i) -> p a i", i=128); buf_wq = BK[5]
        p_wT = banks[5][:, 0:128]; buf_wT = BK[5]
        p_kv = banks[6][:, 0:128]; buf_kv = BK[6]
        p_av = banks[6][:, 128:256]; buf_av = BK[6]

        for g in range(SL // 512):
            p0 = g * 512
            ct = {}
            sq = {}
            for h in range(4):
                for part in range(3):
                    row0 = 896 + part * 512 + h * 128
                    ch = part * 4 + h
                    raw, braw = raws.next()
                    if g == 0:
                        S.op(POOL, lambda e, raw=raw: e.memset(raw[:, 0:3], 0.0), writes=[braw])
                        S.dma(SP, raw[:, 3:515], T.projT[row0:row0 + 128, 0:512], writes=[braw])
                    else:
                        S.dma(SP, raw[:], T.projT[row0:row0 + 128, p0 - 3:p0 + 512], writes=[braw])
                    ca, bca = caccs.next()
                    S.op(DVE, lambda e, ca=ca, raw=raw, ch=ch: e.tensor_scalar(out=ca[:], in0=raw[:, 3:515], scalar1=cw[:, ch, 3:4],
                                                                               scalar2=None, op0=ALU.mult), reads=[braw, bcw], writes=[bca])
                    for k in (2, 1, 0):
                        eng = DVE
                        S.op(eng, lambda e, ca=ca, raw=raw, ch=ch, k=k: e.scalar_tensor_tensor(
                            out=ca[:], in0=raw[:, k:k + 512], scalar=cw[:, ch, k:k + 1], in1=ca[:], op0=ALU.mult, op1=ALU.add),
                            reads=[braw, bcw, bca], writes=[bca])
                    c_, bc_ = cts[ch].next()
                    S.op(ACT, lambda e, c_=c_, ca=ca: e.activation(out=c_[:], in_=ca[:], func=AF.Silu), reads=[bca], writes=[bc_])
                    ct[(part, h)] = (c_, bc_)
                    if part < 2:
                        s_, bs_ = sqts[part * 4 + h].next()
                        S.op(POOL, lambda e, s_=s_, c_=c_: e.tensor_tensor(out=s_[:], in0=c_[:], in1=c_[:], op=ALU.mult),
                             reads=[bc_], writes=[bs_])
                        sq[(part, h)] = (s_, bs_)
            for i in range(4):
                t0 = p0 + i * 128
                cs = slice(i * 128, (i + 1) * 128)
                ab, bab = abs_.next()
                S.dma(SP, ab[:], T.projTM[t0:t0 + 128, 896:904], writes=[bab])
                z, bz = zs.next()
                S.dma(SP, z[:], T.projTM[t0:t0 + 128, 384:896], writes=[bz])
                sc, bsc = scal.next()
                for part in range(2):
                    for h in range(4):
                        s_, bs_ = sq[(part, h)]
                        idx = part * 4 + h
                        S.op(PE, lambda e, s_=s_, idx=idx, cs=cs: e.matmul(bA_[:, idx * 2:idx * 2 + 2], lhsT=s_[:, cs], rhs=ones[:, 0:2],
                                                                           start=True, stop=True),
                             reads=[bs_, bones], writes=[bufA], inc=(idx == 7))
                S.op(DVE, lambda e, sc=sc: e.tensor_scalar(out=sc[:, 0:8], in0=bA_[:, 0:16].rearrange("p (a b) -> p a b", b=2)[:, :, 0],
                                                           scalar1=EPS, scalar2=None, op0=ALU.add), reads=[], writes=[bsc, bufA])
                S.op(ACT, lambda e, sc=sc: e.activation(out=sc[:, 0:8], in_=sc[:, 0:8], func=AF.Ln), reads=[bsc], writes=[bsc])
                S.op(DVE, lambda e, sc=sc: e.tensor_scalar(out=sc[:, 52:56], in0=sc[:, 4:8], scalar1=-0.5, scalar2=None, op0=ALU.mult),
                     reads=[bsc], writes=[bsc])
                S.op(ACT, lambda e, sc=sc: e.activation(out=sc[:, 0:8], in_=sc[:, 0:8], func=AF.Exp, scale=-0.5), reads=[bsc], writes=[bsc])
                S.op(ACT, lambda e, sc=sc, ab=ab: e.activation(out=sc[:, 8:12], in_=ab[:, 0:4], func=AF.Sigmoid), reads=[bab], writes=[bsc])
                S.op(DVE, lambda e, sc=sc, ab=ab: e.tensor_tensor(out=sc[:, 20:24], in0=ab[:, 4:8], in1=dtb[:], op=ALU.add),
                     reads=[bab, bdt], writes=[bsc])
                S.op(DVE, lambda e, sc=sc: e.tensor_scalar(out=sc[:, 72:76], in0=sc[:, 20:24], scalar1=-1.0, scalar2=None, op0=ALU.mult),
                     reads=[bsc], writes=[bsc])
                S.op(DVE, lambda e, sc=sc: e.tensor_tensor(out=sc[:, 72:76], in0=sc[:, 72:76], in1=sc[:, 20:24], op=ALU.max),
                     reads=[bsc], writes=[bsc])
                S.op(ACT, lambda e, sc=sc: e.activation(out=sc[:, 72:76], in_=sc[:, 72:76], func=AF.Exp, scale=-1.0), reads=[bsc], writes=[bsc])
                S.op(ACT, lambda e, sc=sc: e.activation(out=sc[:, 72:76], in_=sc[:, 72:76], func=AF.Ln, bias=1.0), reads=[bsc], writes=[bsc])
                S.op(DVE, lambda e, sc=sc: e.scalar_tensor_tensor(out=sc[:, 76:80], in0=sc[:, 20:24], scalar=0.0, in1=sc[:, 72:76],
                                                                  op0=ALU.max, op1=ALU.add), reads=[bsc], writes=[bsc])
                S.op(DVE, lambda e, sc=sc: e.tensor_tensor(out=sc[:, 12:16], in0=sc[:, 76:80], in1=expA[:], op=ALU.mult),
                     reads=[bsc, bA], writes=[bsc])
                for cc in range(2):
                    S.op(DVE, lambda e, sc=sc, cc=cc: e.tensor_scalar(out=sc[:, 56 + cc * 4:60 + cc * 4], in0=sc[:, 12:16],
                                                                      scalar1=blk[:, cc:cc + 1], scalar2=None, op0=ALU.mult),
                         reads=[bsc, bblk], writes=[bsc])
                S.op(PE, lambda e, sc=sc: e.matmul(bA_[:, 16:20], lhsT=masks[:, 1, :], rhs=sc[:, 12:16], start=True, stop=True),
                     reads=[bsc, bmask], writes=[bufA], inc=False)
                S.op(PE, lambda e, sc=sc: e.matmul(bA_[:, 20:24], lhsT=blkblk[:], rhs=sc[:, 12:16], start=True, stop=True),
                     reads=[bsc, bblk], writes=[bufA], inc=False)
                S.op(PE, lambda e, sc=sc: e.matmul(bA_[:, 24:32], lhsT=ones[:], rhs=sc[:, 56:64], start=True, stop=True),
                     reads=[bsc, bones], writes=[bufA])
                S.op(DVE, lambda e, sc=sc: e.tensor_copy(out=sc[:, 16:20], in_=bA_[:, 16:20]), reads=[], writes=[bsc, bufA])
                S.op(DVE, lambda e, sc=sc: e.tensor_tensor(out=sc[:, 20:24], in0=bA_[:, 20:24], in1=sc[:, 16:20], op=ALU.subtract),
                     reads=[bsc], writes=[bsc, bufA])
                S.op(ACT, lambda e, sc=sc: e.activation(out=sc[:, 24:28], in_=sc[:, 16:20], func=AF.Exp), reads=[bsc], writes=[bsc])
                S.op(ACT, lambda e, sc=sc: e.activation(out=sc[:, 28:32], in_=sc[:, 20:24], func=AF.Exp), reads=[bsc], writes=[bsc])
                S.op(ACT, lambda e, sc=sc: e.activation(out=sc[:, 64:72], in_=bA_[:, 24:32], func=AF.Exp), reads=[], writes=[bsc, bufA])
                S.op(DVE, lambda e, sc=sc: e.tensor_tensor(out=sc[:, 40:44], in0=sc[:, 8:12], in1=sc[:, 4:8], op=ALU.mult), reads=[bsc], writes=[bsc])
                S.op(DVE, lambda e, sc=sc: e.tensor_tensor(out=sc[:, 32:36], in0=sc[:, 40:44], in1=sc[:, 24:28], op=ALU.mult), reads=[bsc], writes=[bsc])
                S.op(DVE, lambda e, sc=sc: e.tensor_tensor(out=sc[:, 36:40], in0=sc[:, 4:8], in1=sc[:, 28:32], op=ALU.mult), reads=[bsc], writes=[bsc])
                S.op(DVE, lambda e, sc=sc: e.tensor_scalar(out=sc[:, 44:48], in0=sc[:, 0:4], scalar1=DK ** -0.5, scalar2=None, op0=ALU.mult),
                     reads=[bsc], writes=[bsc])
                S.op(DVE, lambda e, sc=sc: e.tensor_tensor(out=sc[:, 48:52], in0=sc[:, 44:48], in1=sc[:, 24:28], op=ALU.mult), reads=[bsc], writes=[bsc])
                oall, boall = oalls.next()
                def head_gen(h, R):
                    if STOP <= 1:
                        return
                    yield
                    qT, bqT = ct[(0, h)]
                    kT, bkT = ct[(1, h)]
                    vT, bvT = ct[(2, h)]
                    dg, bdg = R.diags.next()
                    for a, col in enumerate((40 + h, 44 + h, 16 + h)):
                        S.op(POOL, lambda e, dg=dg, a=a, col=col, sc=sc: e.tensor_scalar(out=dg[:, a, :], in0=identf[:], scalar1=sc[:, col:col + 1],
                                                                                       scalar2=None, op0=ALU.mult), reads=[bsc, bidf], writes=[bdg])
                    yield
                    S.op(PE, lambda e, dg=dg: e.matmul(p_rb, lhsT=ones[:], rhs=dg[:], start=True, stop=True), reads=[bdg, bones], writes=[buf_rb])
                    S.op(PE, lambda e, kT=kT, cs=cs: e.matmul(p_g[:, 0, :], lhsT=kT[:, cs], rhs=kT[:, cs], start=True, stop=True),
                         reads=[bkT], writes=[buf_g], inc=False)
                    S.op(PE, lambda e, kT=kT, qT=qT, cs=cs: e.matmul(p_g[:, 1, :], lhsT=kT[:, cs], rhs=qT[:, cs], start=True, stop=True),
                         reads=[bkT, bqT], writes=[buf_g])
                    S.op(PE, lambda e, kT=kT, cs=cs: e.transpose(out=p_tr[:, 0, :], in_=kT[:, cs], identity=identf[:]),
                         reads=[bkT, bidf], writes=[buf_tr], inc=False)
                    S.op(PE, lambda e, vT=vT, cs=cs: e.transpose(out=p_tr[:, 1, :], in_=vT[:, cs], identity=identf[:]),
                         reads=[bvT, bidf], writes=[buf_tr])
                    dm, bdm = R.dmins.next()
                    S.op(DVE, lambda e, dm=dm, sc=sc, h=h: e.tensor_scalar(out=dm[:], in0=p_rb[:, 2, :], scalar1=sc[:, 16 + h:17 + h], scalar2=0.0,
                                                                           op0=ALU.subtract, op1=ALU.min), reads=[bsc], writes=[bdm, buf_rb])
                    E, bE = R.Es.next()
                    S.op(ACT, lambda e, E=E, dm=dm, sc=sc, h=h: e.activation(out=E[:], in_=dm[:], func=AF.Exp, bias=sc[:, 52 + h:53 + h]),
                         reads=[bdm, bsc], writes=[bE])
                    F12, bF = R.F12s.next()
                    S.op(DVE, lambda e, F12=F12: e.tensor_tensor(out=F12[:], in0=p_rb[:, 0:2, :], in1=masks[:], op=ALU.mult),
                         reads=[bmask], writes=[bF, buf_rb])
                    for a in range(2):
                        S.op(POOL, lambda e, F12=F12, E=E, a=a: e.tensor_tensor(out=F12[:, a, :], in0=F12[:, a, :], in1=E[:], op=ALU.mult),
                             reads=[bF, bE], writes=[bF])
                    UA, bUA = R.UAs.next()
                    S.op(DVE, lambda e, UA=UA, F12=F12: e.tensor_tensor(out=UA[:], in0=p_g, in1=F12[:], op=ALU.mult),
                         reads=[bF], writes=[bUA, buf_g])
                    X, bX = R.Xs.next()
                    kd, bkd = R.kdecs.next()
                    S.op(ACT, lambda e, X=X, sc=sc, h=h: e.activation(out=X[:, 0:128], in_=p_tr[:, 1, :], func=AF.Identity, scale=sc[:, 8 + h:9 + h]),
                         reads=[bsc], writes=[bX, buf_tr])
                    S.op(ACT, lambda e, X=X, sc=sc, h=h: e.activation(out=X[:, 128:256], in_=p_tr[:, 0, :], func=AF.Identity, scale=sc[:, 32 + h:33 + h]),
                         reads=[bsc], writes=[bX, buf_tr])
                    S.op(ACT, lambda e, kd=kd, sc=sc, h=h: e.activation(out=kd[:], in_=p_tr[:, 0, :], func=AF.Identity, scale=sc[:, 36 + h:37 + h]),
                         reads=[bsc], writes=[bkd, buf_tr])
                    yield
                    S.op(PE, lambda e, UA=UA: e.transpose(out=p_Lt, in_=UA[:, 0, :], identity=identf[:]), reads=[bUA, bidf], writes=[buf_Lt])
                    L, bL = R.Ls.next()
                    S.op(ACT, lambda e, L=L: e.copy(out=L[:], in_=p_Lt), reads=[], writes=[bL, buf_Lt])
                    if STOP <= 2:
                        return
                    Ucur, bUcur = UA[:, 0, :], bUA
                    Lcur, bLcur = L[:], bL
                    sign = ALU.subtract
                    for lev in range(6):
                        yield
                        pa, bpa = R.p_app, R.bchain
                        S.op(PE, lambda e, pa=pa, Ucur=Ucur, X=X: e.matmul(pa, lhsT=Ucur, rhs=X[:], start=True, stop=True),
                             reads=[bUcur, bX], writes=[bpa])
                        Xn, bXn = R.Xs.next()
                        S.op(DVE, lambda e, Xn=Xn, X=X, pa=pa, sign=sign: e.tensor_tensor(out=Xn[:], in0=X[:], in1=pa, op=sign),
                             reads=[bX], writes=[bXn, bpa])
                        X, bX = Xn, bXn
                        sign = ALU.add
                        if lev == 5:
                            break
                        yield
                        pp, bpp = R.p_pw, R.bchain
                        S.op(PE, lambda e, pp=pp, Ucur=Ucur, Lcur=Lcur: e.matmul(pp[:, 0, :], lhsT=Lcur, rhs=Ucur, start=True, stop=True),
                             reads=[bUcur, bLcur], writes=[bpp], inc=(lev == 4))
                        if lev < 4:
                            S.op(PE, lambda e, pp=pp, Ucur=Ucur, Lcur=Lcur: e.matmul(pp[:, 1, :], lhsT=Ucur, rhs=Lcur, start=True, stop=True),
                                 reads=[bUcur, bLcur], writes=[bpp])
                        pw, bpw = R.pws.next()
                        na = 2 if lev < 4 else 1
                        S.op(ACT, lambda e, pw=pw, pp=pp, na=na: e.copy(out=pw[:, 0:na, :], in_=pp[:, 0:na, :]),
                             reads=[], writes=[bpw, bpp])
                        Ucur, bUcur = pw[:, 0, :], bpw
                        Lcur, bLcur = pw[:, 1, :], bpw
                    yield
                    S.op(PE, lambda e, X=X: e.transpose(out=p_wT, in_=X[:, 128:256], identity=identf[:]), reads=[bX, bidf], writes=[buf_wT])
                    wT, bwT = R.wTs.next()
                    S.op(ACT, lambda e, wT=wT: e.copy(out=wT[:], in_=p_wT), reads=[], writes=[bwT, buf_wT])
                    if STOP <= 3 or STOP == 6:
                        return
                    RS = {7: 1, 8: 2, 9: 3, 10: 4}.get(STOP, 99)
                    st, bst = state[h], bstate[h]
                    for cc in range(2):
                        yield
                        v_, bv_ = vn[h][cc], bvn[h][cc]
                        S.op(PE, lambda e, wT=wT, st=st: e.matmul(p_wq[:, 0, :], lhsT=wT[:], rhs=st[:], start=True, stop=True),
                             reads=[bwT, bst], writes=[buf_wq], inc=False)
                        S.op(PE, lambda e, qT=qT, st=st, cs=cs: e.matmul(p_wq[:, 1, :], lhsT=qT[:, cs], rhs=st[:], start=True, stop=True),
                             reads=[bqT, bst], writes=[buf_wq])
                        if RS <= 1:
                            continue
                        vt, bvt = R.vtmps.next()
                        S.op(DVE, lambda e, vt=vt, X=X: e.tensor_tensor(out=vt[:], in0=X[:, 0:128], in1=p_wq[:, 0, :], op=ALU.subtract),
                             reads=[bX], writes=[bvt, buf_wq])
                        S.op(DVE, lambda e, v_=v_, vt=vt, cc=cc: e.tensor_scalar(out=v_[:], in0=vt[:], scalar1=blk[:, cc:cc + 1], scalar2=None,
                                                                                op0=ALU.mult), reads=[bvt, bblk], writes=[bv_])
                        oq, boq = R.oqs.next()
                        S.op(ACT, lambda e, oq=oq, sc=sc, h=h: e.activation(out=oq[:], in_=p_wq[:, 1, :], func=AF.Identity,
                                                                           scale=sc[:, 48 + h:49 + h]), reads=[bsc], writes=[boq, buf_wq])
                        if RS <= 2:
                            continue
                        yield
                        S.op(PE, lambda e, UA=UA, v_=v_: e.matmul(p_av, lhsT=UA[:, 1, :], rhs=v_[:], start=True, stop=True),
                             reads=[bUA, bv_], writes=[buf_av])
                        S.op(PE, lambda e, kd=kd, v_=v_: e.matmul(p_kv, lhsT=kd[:], rhs=v_[:], start=True, stop=True),
                             reads=[bkd, bv_], writes=[buf_kv])
                        if RS <= 3:
                            continue
                        S.op(DVE, lambda e, st=st, sc=sc, cc=cc, h=h: e.scalar_tensor_tensor(
                            out=st[:], in0=st[:], scalar=sc[:, 64 + cc * 4 + h:65 + cc * 4 + h], in1=p_kv, op0=ALU.mult, op1=ALU.add),
                            reads=[bst, bsc], writes=[bst, buf_kv])
                        if RS <= 4:
                            continue
                        S.op(DVE, lambda e, oq=oq: e.tensor_tensor(out=oq[:], in0=oq[:], in1=p_av, op=ALU.add),
                             reads=[boq], writes=[boq, buf_av])
                        if cc == 0:
                            S.op(DVE, lambda e, oall=oall, oq=oq, h=h: e.tensor_scalar(out=oall[:, h, :], in0=oq[:], scalar1=blk[:, 0:1],
                                                                                     scalar2=None, op0=ALU.mult), reads=[boq, bblk], writes=[boall])
                        else:
                            S.op(DVE, lambda e, oall=oall, oq=oq, h=h: e.scalar_tensor_tensor(
                                out=oall[:, h, :], in0=oq[:], scalar=blk[:, 1:2], in1=oall[:, h, :], op0=ALU.mult, op1=ALU.add),
                                reads=[boq, bblk, boall], writes=[boall])

                pending = list(range(4))
                active = []
                free = list(range(NSLOT))
                while pending or active:
                    while pending and free:
                        k_ = free.pop(0)
                        active.append((head_gen(pending.pop(0), SLOTS[k_]), k_))
                    nxt = []
                    for gen_, k_ in active:
                        try:
                            next(gen_)
                            nxt.append((gen_, k_))
                        except StopIteration:
                            free.append(k_)
                    active = nxt
                if STOP <= 3 or STOP == 5 or STOP >= 7:
                    continue
                if STOP == 6:
                    S.op(POOL, lambda e, oall=oall: e.memset(oall[:], 0.5), writes=[boall])
                zl, bzl = zsil.next()
                S.op(ACT, lambda e, zl=zl, z=z: e.activation(out=zl[:], in_=z[:], func=AF.Silu), reads=[bz], writes=[bzl])
                for h in range(4):
                    jk, bjk = junkg.next()
                    S.op(DVE, lambda e, jk=jk, oall=oall, h=h, sc=sc: e.scalar_tensor_tensor(
                        out=jk[:], in0=oall[:, h, :], scalar=1.0, in1=oall[:, h, :], op0=ALU.mult, op1=ALU.mult, accum_out=sc[:, 80 + h:81 + h]),
                        reads=[boall], writes=[bjk, bsc])
                S.op(DVE, lambda e, sc=sc: e.tensor_scalar(out=sc[:, 80:84], in0=sc[:, 80:84], scalar1=1.0 / 128, scalar2=EPS, op0=ALU.mult, op1=ALU.add),
                     reads=[bsc], writes=[bsc])
                S.op(ACT, lambda e, sc=sc: e.activation(out=sc[:, 80:84], in_=sc[:, 80:84], func=AF.Ln), reads=[bsc], writes=[bsc])
                S.op(ACT, lambda e, sc=sc: e.activation(out=sc[:, 80:84], in_=sc[:, 80:84], func=AF.Exp, scale=-0.5), reads=[bsc], writes=[bsc])
                om, bom = oms.next()
                for h in range(4):
                    S.op(DVE, lambda e, oall=oall, h=h, sc=sc: e.scalar_tensor_tensor(
                        out=oall[:, h, :], in0=oall[:, h, :], scalar=sc[:, 80 + h:81 + h], in1=wn[:], op0=ALU.mult, op1=ALU.mult),
                        reads=[boall, bsc, bwn], writes=[boall])
                S.op(POOL, lambda e, om=om, oall=oall, zl=zl: e.tensor_tensor(out=om[:], in0=oall[:].rearrange("p h d -> p (h d)"), in1=zl[:], op=ALU.mult),
                     reads=[boall, bzl], writes=[bom])
                S.dma(SP, T.mixed[t0:t0 + 128, 512:1024], om[:], reads=[bom])
        S.barrier()
        S.flush(nc, sems)


WEIGHT_SPECS = [
    ("mix_pre_norm", [2, D]), ("w_in", [2, D, INW]), ("swa_sinks", [2, 4]), ("dn_conv_w", [2, 4, 1536]),
    ("dn_a_log", [2, 4]), ("dn_dt_bias", [2, 4]), ("dn_norm", [2, 128]), ("w_out", [2, D, D]),
    ("mix_post_norm", [2, D]), ("ffn_pre_norm", [2, D]), ("w_up", [2, D, 2 * DFF]), ("ffn_conv_w", [2, 3, 2 * DFF]),
    ("ffn_conv_b", [2, 2 * DFF]), ("w_down", [2, DFF, D]), ("ffn_post_norm", [2, D]),
]


def build(SL=8192, n_layers=2, phases="ABC", dump=(), mixed_in=False, gdn_stop=4):
    nc = bass.Bass("TRN2", target_bir_lowering=False)
    T = Ctx()
    T.SL = SL
    T.gdn_stop = gdn_stop
    T.x = nc.dram_tensor("x", [SL, D], F32, kind="ExternalInput").ap()
    for name, shape in WEIGHT_SPECS:
        setattr(T, name, nc.dram_tensor(name, shape, F32, kind="ExternalInput").ap())
    T.out = nc.dram_tensor("out", [SL, D], F32, kind="ExternalOutput").ap()

    def scratch(name, shape, dt):
        kind = "ExternalOutput" if name in dump else "Internal"
        if name == "mixed" and mixed_in:
            kind = "ExternalInput"
        return nc.dram_tensor(name, shape, dt, kind=kind).ap()
    T.projT = scratch("projT", [NFM, SL], F32)
    T.projTM = scratch("projTM", [SL, NTM], F32)
    T.mixed = scratch("mixed", [SL, D], BF16)
    T.xres1 = scratch("xres1", [SL, D], F32)
    T.gsc = scratch("gsc", [DFF, SL], BF16)
    T.xmid = scratch("xmid", [SL, D], F32)
    S = Sched()
    with ExitStack() as es:
        sems = {k: es.enter_context(nc.semaphore(k.replace("_", ""))) for k in S.semkeys()}
        for l in range(n_layers):
            x_src = T.x if l == 0 else T.xmid
            x_dst = T.out if l == n_layers - 1 else T.xmid
            if "A" in phases:
                phase_A(S, nc, sems, T, l, x_src)
            if "B" in phases or "S" in phases:
                phase_SWA(S, nc, sems, T, l)
            if "B" in phases or "M" in phases:
                phase_MOBA(S, nc, sems, T, l)
            if "B" in phases or "G" in phases:
                phase_GDN(S, nc, sems, T, l)
            if "C" in phases:
                phase_C1(S, nc, sems, T, l, x_src)
                phase_C2(S, nc, sems, T, l, x_dst)
    return nc, S


def kernel(**inputs):
    x = np.ascontiguousarray(inputs["x"], dtype=np.float32)
    B, SL, _ = x.shape
    nc, _ = build(SL, 2)
    w = {name: np.ascontiguousarray(inputs[name], dtype=np.float32) for name, _ in WEIGHT_SPECS}
    in_maps = [dict(w, x=x[b]) for b in range(B)]
    res = run_bass_kernel_spmd(nc, in_maps, core_ids=list(range(B)))
    return np.stack([r["out"] for r in res.results], axis=0).astype(np.float32)
```

```python
import numpy as np
from contextlib import ExitStack
import concourse.bass as bass
import concourse.mybir as mybir
from concourse.bass_utils import run_bass_kernel_spmd

F32 = mybir.dt.float32
BF16 = mybir.dt.bfloat16
I32 = mybir.dt.int32
ALU = mybir.AluOpType
AF = mybir.ActivationFunctionType
AX = mybir.AxisListType

PE, ACT, DVE, POOL, SP = "tensor", "scalar", "vector", "gpsimd", "sync"
COMPUTE = (PE, ACT, DVE, POOL)
DMA_POOL = 12

D = 1024
DFF = 2816
INW = 3336
NFM = 2432
NTM = 904
EPS = 1e-6
NEG = -30000.0
STGW = 1408


_UID = [0]


def U(name):
    _UID[0] += 1
    return "%s_%d" % (name, _UID[0])


class Buf:
    __slots__ = ("name", "w", "r")

    def __init__(self, name=""):
        self.name = name
        self.w = None
        self.r = {}


class Sched:
    def __init__(self):
        self.ops = {e: [] for e in (PE, ACT, DVE, POOL, SP)}
        self.cnt = {}
        self.waited = {e: {} for e in self.ops}
        self.dma_rr = {e: 0 for e in self.ops}
        self.n_ops = 0

    def _deps(self, eng, reads, writes, sew):
        deps = {}

        def add(t):
            if t is None:
                return
            sk, v = t
            if deps.get(sk, 0) < v:
                deps[sk] = v
        for b in reads:
            add(b.w)
        for b in writes:
            add(b.w)
            for sk, v in b.r.items():
                add((sk, v))
        out = []
        for sk, v in deps.items():
            if sk == eng and not sew:
                continue
            if self.waited[eng].get(sk, 0) >= v:
                continue
            self.waited[eng][sk] = v
            out.append((sk, v))
        return out

    def _mark(self, ticket, reads, writes):
        sk, v = ticket
        for b in reads:
            if b.r.get(sk, 0) < v:
                b.r[sk] = v
        for b in writes:
            b.w = ticket
            b.r = {}

    def op(self, eng, fn, reads=(), writes=(), inc=True, sew=None):
        if sew is None:
            sew = eng != PE
        waits = self._deps(eng, reads, writes, sew)
        c = self.cnt.get(eng, 0)
        ticket = (eng, c + 1)
        if inc:
            self.cnt[eng] = c + 1
        self._mark(ticket, reads, writes)
        self.ops[eng].append((waits, fn, eng if inc else None, 1))
        self.n_ops += 1

    def dma(self, q, out_ap, in_ap, reads=(), writes=(), **kw):
        i = self.dma_rr[q]
        self.dma_rr[q] = (i + 1) % DMA_POOL
        sk = "dma_%s_%d" % (q, i)
        c = self.cnt.get(sk, 0)
        waits = self._deps(q, reads, writes, True)
        if c > 0 and self.waited[q].get(sk, 0) < c:
            self.waited[q][sk] = c
            waits.append((sk, c))
        self.cnt[sk] = c + 16
        self._mark((sk, c + 16), reads, writes)
        self.ops[q].append((waits, lambda e: e.dma_start(out=out_ap, in_=in_ap, **kw), sk, 16))
        self.n_ops += 1

    def barrier(self):
        for e in self.ops:
            waits = []
            for sk, v in self.cnt.items():
                if sk == e:
                    continue
                if self.waited[e].get(sk, 0) < v:
                    self.waited[e][sk] = v
                    waits.append((sk, v))
            if waits:
                self.ops[e].append((waits, None, None, 0))

    def flush(self, nc, sems):
        ops = self.ops
        self.ops = {e: [] for e in ops}

        def run(e, name):
            for waits, fn, sk, inc in ops[name]:
                for wsk, v in waits:
                    e.wait_ge(sems[wsk], v)
                if fn is None:
                    continue
                ins = fn(e)
                if sk is not None:
                    ins.then_inc(sems[sk], inc)
        with nc.Block() as block:
            block.sync(lambda e: run(e, SP))
            block.tensor(lambda e: run(e, PE))
            block.scalar(lambda e: run(e, ACT))
            block.vector(lambda e: run(e, DVE))
            block.gpsimd(lambda e: run(e, POOL))

    @staticmethod
    def semkeys():
        keys = list(COMPUTE)
        for q in (PE, ACT, DVE, POOL, SP):
            for i in range(DMA_POOL):
                keys.append("dma_%s_%d" % (q, i))
        return keys


class Ring:
    def __init__(self, tiles):
        self.items = [(t, Buf()) for t in tiles]
        self.i = 0

    def next(self):
        it = self.items[self.i % len(self.items)]
        self.i += 1
        return it


class Ctx:
    pass


def make_ident(S, es, nc, dt):
    identf = es.enter_context(nc.sbuf_tensor(U("identf"), [128, 128], F32))
    b = Buf()
    S.op(POOL, lambda e: e.memset(identf[:], 1.0), writes=[b])
    S.op(POOL, lambda e: e.affine_select(out=identf[:], in_=identf[:], pattern=[[-1, 128]],
                                         compare_op=ALU.is_equal, fill=0.0, base=0, channel_multiplier=1),
         reads=[b], writes=[b])
    if dt == F32:
        return identf, b
    ident = es.enter_context(nc.sbuf_tensor(U("identb"), [128, 128], BF16))
    b2 = Buf()
    S.op(POOL, lambda e: e.tensor_copy(out=ident[:], in_=identf[:]), reads=[b], writes=[b2])
    return ident, b2


def rstd_from_ss(S, ss, bss, n):
    S.op(DVE, lambda e: e.tensor_scalar(out=ss, in0=ss, scalar1=1.0 / n, scalar2=EPS, op0=ALU.mult, op1=ALU.add),
         reads=[bss], writes=[bss])
    S.op(ACT, lambda e: e.activation(out=ss, in_=ss, func=AF.Ln), reads=[bss], writes=[bss])
    S.op(ACT, lambda e: e.activation(out=ss, in_=ss, func=AF.Exp, scale=-0.5), reads=[bss], writes=[bss])


def load_weight_bf16(S, w_dram, dst, bdst, stg_ring, colmap, nk):
    for k in range(nk):
        for (s0, s1, d0) in colmap:
            n = s1 - s0
            for o in range(0, n, STGW):
                m = min(STGW, n - o)
                stg, bs = stg_ring.next()
                S.dma(SP, stg[:, 0:m], w_dram[k * 128:(k + 1) * 128, s0 + o:s0 + o + m], writes=[bs])
                S.op(POOL, lambda e, stg=stg, m=m, k=k, dd=d0 + o: e.tensor_copy(out=dst[:, k, dd:dd + m], in_=stg[:, 0:m]),
                     reads=[bs], writes=[bdst])


def norm_rows_to_bf16(S, C, xt, bx, gbc, bg, h, bh):
    junk, bj = C.junk.next()
    ss, bss = C.ss.next()
    S.op(DVE, lambda e: e.scalar_tensor_tensor(out=junk[:], in0=xt, scalar=1.0, in1=xt, op0=ALU.mult, op1=ALU.mult,
                                               accum_out=ss[:]), reads=[bx], writes=[bj, bss])
    rstd_from_ss(S, ss[:], bss, D)
    S.op(DVE, lambda e: e.scalar_tensor_tensor(out=h, in0=xt, scalar=ss[:], in1=gbc, op0=ALU.mult, op1=ALU.mult),
         reads=[bx, bss, bg], writes=[bh])


def transpose8(S, C, h, bh, dstT, bdT, col0):
    pT, bpT = C.pT.next()
    for c in range(8):
        S.op(PE, lambda e, c=c: e.transpose(out=pT[:, c, :], in_=h[:, c * 128:(c + 1) * 128], identity=C.ident[:]),
             reads=[bh, C.bident], writes=[bpT], inc=(c == 7))
    S.op(ACT, lambda e: e.copy(out=dstT[:, :, col0:col0 + 128], in_=pT[:]), reads=[bpT], writes=[bdT])


def phase_A(S, nc, sems, T, l, x_src):
    SL = T.SL
    with ExitStack() as es:
        def sb(name, shape, dt):
            return es.enter_context(nc.sbuf_tensor(U(name), shape, dt))

        def ps(name, shape, dt):
            return es.enter_context(nc.psum_tensor(U(name), shape, dt))
        C = Ctx()
        C.ident, C.bident = make_ident(S, es, nc, BF16)
        wfm = sb("wfm", [128, 8, NFM], BF16); bwfm = Buf()
        wtm = sb("wtm", [128, 8, NTM], BF16); bwtm = Buf()
        stg = Ring([sb("stg%d" % i, [128, STGW], F32) for i in range(2)])
        gbc = sb("gbc", [128, D], F32); bg = Buf()
        C.junk = Ring([sb("junk", [128, D], BF16)])
        C.ss = Ring([sb("ss%d" % i, [128, 1], F32) for i in range(4)])
        C.pT = Ring([ps("pT%d" % i, [128, 8, 128], BF16) for i in range(2)])
        xts = Ring([sb("xt%d" % i, [128, D], F32) for i in range(2)])
        hs = Ring([sb("h%d" % i, [128, D], BF16) for i in range(2)])
        hTs = Ring([sb("hT%d" % i, [128, 8, 512], BF16) for i in range(2)])
        pfm = Ring([ps("pfm%d" % i, [128, 512], F32) for i in range(2)])
        ptm = Ring([ps("ptm%d" % i, [128, 1024], F32) for i in range(2)])
        ofm = Ring([sb("ofm%d" % i, [128, 512], F32) for i in range(4)])
        otm = Ring([sb("otm%d" % i, [128, NTM], F32) for i in range(2)])

        S.dma(SP, gbc[:], T.mix_pre_norm[l].partition_broadcast(128), writes=[bg])
        w = T.w_in[l]
        load_weight_bf16(S, w, wfm, bwfm, stg, [(0, 384, 0), (512, 1024, 384), (1280, 2816, 896)], 8)
        load_weight_bf16(S, w, wtm, bwtm, stg, [(384, 512, 0), (1024, 1280, 128), (2816, 3336, 384)], 8)

        for g in range(SL // 512):
            hT, bhT = hTs.next()
            for i in range(4):
                t0 = g * 512 + i * 128
                xt, bx = xts.next()
                S.dma(SP, xt[:], x_src[t0:t0 + 128, :], writes=[bx])
                h, bh = hs.next()
                norm_rows_to_bf16(S, C, xt[:], bx, gbc[:], bg, h[:], bh)
                transpose8(S, C, h, bh, hT, bhT, i * 128)
            for i in range(4):
                t0 = g * 512 + i * 128
                p, bp = ptm.next()
                for (n0, n1) in ((0, 512), (512, NTM)):
                    for c in range(8):
                        S.op(PE, lambda e, c=c, n0=n0, n1=n1, p=p, i=i, hT=hT: e.matmul(
                            p[:, n0:n1], lhsT=hT[:, c, i * 128:(i + 1) * 128], rhs=wtm[:, c, n0:n1],
                            start=(c == 0), stop=(c == 7)), reads=[bhT, bwtm], writes=[bp], inc=(c == 7))
                o, bo = otm.next()
                S.op(ACT, lambda e, o=o, p=p: e.copy(out=o[:], in_=p[:, 0:NTM]), reads=[bp], writes=[bo])
                S.dma(SP, T.projTM[t0:t0 + 128, :], o[:], reads=[bo])
            for ch in range(NFM // 128):
                p, bp = pfm.next()
                for c in range(8):
                    S.op(PE, lambda e, c=c, ch=ch, p=p, hT=hT: e.matmul(
                        p[:], lhsT=wfm[:, c, ch * 128:(ch + 1) * 128], rhs=hT[:, c, :],
                        start=(c == 0), stop=(c == 7)), reads=[bhT, bwfm], writes=[bp], inc=(c == 7))
                o, bo = ofm.next()
                eng = ACT if ch % 2 == 0 else DVE
                if eng == ACT:
                    S.op(ACT, lambda e, o=o, p=p: e.copy(out=o[:], in_=p[:]), reads=[bp], writes=[bo])
                else:
                    S.op(DVE, lambda e, o=o, p=p: e.tensor_copy(out=o[:], in_=p[:]), reads=[bp], writes=[bo])
                S.dma(SP, T.projT[ch * 128:(ch + 1) * 128, g * 512:(g + 1) * 512], o[:], reads=[bo])
        S.barrier()
        S.flush(nc, sems)


def load_rows_T(S, C, nc, es, src2d, nrows, ncols, name):
    nch = ncols // 128
    rows = es.enter_context(nc.sbuf_tensor(U(name + "_rows"), [8, ncols], F32))
    br = Buf()
    S.dma(SP, rows[0:nrows, :], src2d, writes=[br])
    out = es.enter_context(nc.sbuf_tensor(U(name), [128, nch, nrows], F32))
    bo = Buf()
    for c0 in range(0, nch, 16):
        n = min(16, nch - c0)
        pt, bpt = C.pmisc.next()
        for j in range(n):
            c = c0 + j
            S.op(PE, lambda e, c=c, j=j, pt=pt: e.transpose(out=pt[:, j * nrows:(j + 1) * nrows],
                                                            in_=rows[0:nrows, c * 128:(c + 1) * 128],
                                                            identity=C.identf[0:nrows, 0:nrows]),
                 reads=[br, C.bidentf], writes=[bpt], inc=(j == n - 1))
        S.op(DVE, lambda e, c0=c0, n=n, pt=pt: e.tensor_copy(
            out=out[:, c0:c0 + n, :], in_=pt[:, 0:n * nrows].rearrange("p (c k) -> p c k", k=nrows)),
            reads=[bpt], writes=[bo])
    return out, bo


def phase_C1(S, nc, sems, T, l, x_src):
    SL = T.SL
    with ExitStack() as es:
        def sb(name, shape, dt):
            return es.enter_context(nc.sbuf_tensor(U(name), shape, dt))

        def ps(name, shape, dt):
            return es.enter_context(nc.psum_tensor(U(name), shape, dt))
        C = Ctx()
        C.ident, C.bident = make_ident(S, es, nc, BF16)
        identf = sb("identf2", [128, 128], F32); bidf = Buf()
        S.op(POOL, lambda e: e.memset(identf[:], 1.0), writes=[bidf])
        S.op(POOL, lambda e: e.affine_select(out=identf[:], in_=identf[:], pattern=[[-1, 128]],
                                             compare_op=ALU.is_equal, fill=0.0, base=0, channel_multiplier=1),
             reads=[bidf], writes=[bidf])
        C.identf, C.bidentf = identf, bidf
        C.pmisc = Ring([ps("pmisc", [128, 512], F32)])
        cw = sb("cw", [128, 44, 4], F32); bcw = Buf()
        with nc.sbuf_tensor(U("crow"), [8, 2 * DFF], F32) as crow:
            bcr = Buf()
            S.dma(SP, crow[0:3, :], T.ffn_conv_w[l], writes=[bcr])
            S.dma(SP, crow[3:4, :], T.ffn_conv_b[l:l + 1, :], writes=[bcr])
            for c0 in range(0, 44, 22):
                pt, bpt = C.pmisc.next()
                for j in range(22):
                    c = c0 + j
                    S.op(PE, lambda e, c=c, j=j, pt=pt: e.transpose(out=pt[:, j * 4:(j + 1) * 4], in_=crow[0:4, c * 128:(c + 1) * 128],
                                                                    identity=identf[0:4, 0:4]),
                         reads=[bcr, bidf], writes=[bpt], inc=(j == 21))
                S.op(DVE, lambda e, c0=c0, pt=pt: e.tensor_copy(out=cw[:, c0:c0 + 22, :],
                                                                in_=pt[:, 0:88].rearrange("p (c k) -> p c k", k=4)),
                     reads=[bpt], writes=[bcw])
            S.barrier()
            S.flush(nc, sems)
        wout = sb("wout", [128, 8, D], BF16); bwout = Buf()
        wup = sb("wup", [128, 8, 2 * DFF], BF16); bwup = Buf()
        stg = Ring([sb("stg%d" % i, [128, STGW], F32) for i in range(2)])
        gpost = sb("gpost", [128, D], F32); bgp = Buf()
        gpre = sb("gpre", [128, D], F32); bgq = Buf()
        C.junk = Ring([sb("junk", [128, D], BF16)])
        C.ss = Ring([sb("ss%d" % i, [128, 1], F32) for i in range(4)])
        C.pT = Ring([ps("pT%d" % i, [128, 8, 128], BF16) for i in range(2)])
        py = Ring([ps("py", [128, D], F32)])
        pup = Ring([ps("pup%d" % i, [128, 512], F32) for i in range(3)])
        mts = Ring([sb("mt%d" % i, [128, D], BF16) for i in range(2)])
        mTs = Ring([sb("mT%d" % i, [128, 8, 128], BF16) for i in range(2)])
        xts = Ring([sb("xt%d" % i, [128, D], F32) for i in range(2)])
        x1s = Ring([sb("x1%d" % i, [128, D], F32) for i in range(2)])
        hs = Ring([sb("h%d" % i, [128, D], BF16) for i in range(2)])
        hTs = Ring([sb("hT%d" % i, [128, 8, 512], BF16) for i in range(2)])
        ubs = Ring([sb("ub%d" % i, [128, 514], F32) for i in range(3)])
        accs = Ring([sb("acc%d" % i, [128, 512], F32) for i in range(3)])
        ggs = Ring([sb("gg%d" % i, [128, 512], F32) for i in range(2)])
        gos = Ring([sb("go%d" % i, [128, 512], BF16) for i in range(3)])
        halo = sb("halo", [128, 44, 2], F32); bhalo = [Buf() for _ in range(44)]

        S.dma(SP, gpost[:], T.mix_post_norm[l].partition_broadcast(128), writes=[bgp])
        S.dma(SP, gpre[:], T.ffn_pre_norm[l].partition_broadcast(128), writes=[bgq])
        S.op(POOL, lambda e: e.memset(halo[:], 0.0), writes=bhalo)
        load_weight_bf16(S, T.w_out[l], wout, bwout, stg, [(0, D, 0)], 8)
        load_weight_bf16(S, T.w_up[l], wup, bwup, stg, [(0, 2 * DFF, 0)], 8)

        for g in range(SL // 512):
            hT, bhT = hTs.next()
            for i in range(4):
                t0 = g * 512 + i * 128
                mt, bmt = mts.next()
                S.dma(SP, mt[:], T.mixed[t0:t0 + 128, :], writes=[bmt])
                mT, bmT = mTs.next()
                transpose8(S, C, mt, bmt, mT, bmT, 0)
                xt, bx = xts.next()
                S.dma(SP, xt[:], x_src[t0:t0 + 128, :], writes=[bx])
                p, bp = py.next()
                for nb in range(2):
                    for c in range(8):
                        S.op(PE, lambda e, c=c, nb=nb, p=p, mT=mT: e.matmul(
                            p[:, nb * 512:(nb + 1) * 512], lhsT=mT[:, c, :], rhs=wout[:, c, nb * 512:(nb + 1) * 512],
                            start=(c == 0), stop=(c == 7)), reads=[bmT, bwout], writes=[bp], inc=(c == 7))
                junk, bj = C.junk.next()
                ss, bss = C.ss.next()
                S.op(ACT, lambda e, junk=junk, p=p, ss=ss: e.activation(out=junk[:], in_=p[:], func=AF.Square, accum_out=ss[:]),
                     reads=[bp], writes=[bj, bss])
                rstd_from_ss(S, ss[:], bss, D)
                x1, bx1 = x1s.next()
                S.op(DVE, lambda e, x1=x1, p=p, ss=ss: e.scalar_tensor_tensor(out=x1[:], in0=p[:], scalar=ss[:], in1=gpost[:],
                                                                             op0=ALU.mult, op1=ALU.mult),
                     reads=[bp, bss, bgp], writes=[bx1])
                S.op(DVE, lambda e, x1=x1, xt=xt: e.tensor_tensor(out=x1[:], in0=x1[:], in1=xt[:], op=ALU.add),
                     reads=[bx1, bx], writes=[bx1])
                S.dma(SP, T.xres1[t0:t0 + 128, :], x1[:], reads=[bx1])
                h, bh = hs.next()
                norm_rows_to_bf16(S, C, x1[:], bx1, gpre[:], bgq, h[:], bh)
                transpose8(S, C, h, bh, hT, bhT, i * 128)
            for f in range(22):
                accp = []
                for part in range(2):
                    ch = part * 22 + f
                    p, bp = pup.next()
                    for c in range(8):
                        S.op(PE, lambda e, c=c, ch=ch, p=p, hT=hT: e.matmul(
                            p[:], lhsT=wup[:, c, ch * 128:(ch + 1) * 128], rhs=hT[:, c, :],
                            start=(c == 0), stop=(c == 7)), reads=[bhT, bwup], writes=[bp], inc=(c == 7))
                    ub, bub = ubs.next()
                    S.op(POOL, lambda e, ub=ub, ch=ch: e.tensor_copy(out=ub[:, 0:2], in_=halo[:, ch, :]),
                         reads=[bhalo[ch]], writes=[bub])
                    S.op(ACT, lambda e, ub=ub, p=p: e.copy(out=ub[:, 2:514], in_=p[:]), reads=[bp], writes=[bub])
                    S.op(POOL, lambda e, ub=ub, ch=ch: e.tensor_copy(out=halo[:, ch, :], in_=ub[:, 512:514]),
                         reads=[bub], writes=[bhalo[ch]])
                    acc, bacc = accs.next()
                    S.op(DVE, lambda e, acc=acc, ub=ub, ch=ch: e.tensor_scalar(
                        out=acc[:], in0=ub[:, 2:514], scalar1=cw[:, ch, 2:3], scalar2=cw[:, ch, 3:4], op0=ALU.mult, op1=ALU.add),
                        reads=[bub, bcw], writes=[bacc])
                    S.op(DVE, lambda e, acc=acc, ub=ub, ch=ch: e.scalar_tensor_tensor(
                        out=acc[:], in0=ub[:, 1:513], scalar=cw[:, ch, 1:2], in1=acc[:], op0=ALU.mult, op1=ALU.add),
                        reads=[bub, bcw, bacc], writes=[bacc])
                    S.op(DVE, lambda e, acc=acc, ub=ub, ch=ch: e.scalar_tensor_tensor(
                        out=acc[:], in0=ub[:, 0:512], scalar=cw[:, ch, 0:1], in1=acc[:], op0=ALU.mult, op1=ALU.add),
                        reads=[bub, bcw, bacc], writes=[bacc])
                    accp.append((acc, bacc))
                gg, bgg = ggs.next()
                S.op(ACT, lambda e, gg=gg, a=accp[0][0]: e.activation(out=gg[:], in_=a[:], func=AF.Gelu_apprx_tanh),
                     reads=[accp[0][1]], writes=[bgg])
                go, bgo = gos.next()
                S.op(POOL, lambda e, go=go, gg=gg, a=accp[1][0]: e.tensor_tensor(out=go[:], in0=gg[:], in1=a[:], op=ALU.mult),
                     reads=[bgg, accp[1][1]], writes=[bgo])
                S.dma(SP, T.gsc[f * 128:(f + 1) * 128, g * 512:(g + 1) * 512], go[:], reads=[bgo])
        S.barrier()
        S.flush(nc, sems)


def phase_C2(S, nc, sems, T, l, x_dst):
    SL = T.SL
    with ExitStack() as es:
        def sb(name, shape, dt):
            return es.enter_context(nc.sbuf_tensor(U(name), shape, dt))

        def ps(name, shape, dt):
            return es.enter_context(nc.psum_tensor(U(name), shape, dt))
        C = Ctx()
        wdn = sb("wdn", [128, 22, D], BF16); bwdn = Buf()
        stg = Ring([sb("stg%d" % i, [128, STGW], F32) for i in range(2)])
        gpost = sb("gpost", [128, D], F32); bgp = Buf()
        C.junk = Ring([sb("junk", [128, D], BF16)])
        C.ss = Ring([sb("ss%d" % i, [128, 1], F32) for i in range(4)])
        py = Ring([ps("py%d" % i, [128, D], F32) for i in range(2)])
        gTs = Ring([sb("gT%d" % i, [128, 22, 512], BF16) for i in range(2)])
        xts = Ring([sb("xt%d" % i, [128, D], F32) for i in range(2)])
        x2s = Ring([sb("x2%d" % i, [128, D], F32) for i in range(2)])
        S.dma(SP, gpost[:], T.ffn_post_norm[l].partition_broadcast(128), writes=[bgp])
        load_weight_bf16(S, T.w_down[l], wdn, bwdn, stg, [(0, D, 0)], 22)
        for g in range(SL // 512):
            gT, bgT = gTs.next()
            S.dma(SP, gT[:], T.gsc[:, g * 512:(g + 1) * 512].rearrange("(c p) t -> p c t", p=128), writes=[bgT])
            for i in range(4):
                t0 = g * 512 + i * 128
                xt, bx = xts.next()
                S.dma(SP, xt[:], T.xres1[t0:t0 + 128, :], writes=[bx])
                p, bp = py.next()
                for nb in range(2):
                    for f in range(22):
                        S.op(PE, lambda e, f=f, nb=nb, p=p, gT=gT, i=i: e.matmul(
                            p[:, nb * 512:(nb + 1) * 512], lhsT=gT[:, f, i * 128:(i + 1) * 128],
                            rhs=wdn[:, f, nb * 512:(nb + 1) * 512], start=(f == 0), stop=(f == 21)),
                            reads=[bgT, bwdn], writes=[bp], inc=(f == 21))
                junk, bj = C.junk.next()
                ss, bss = C.ss.next()
                S.op(ACT, lambda e, junk=junk, p=p, ss=ss: e.activation(out=junk[:], in_=p[:], func=AF.Square, accum_out=ss[:]),
                     reads=[bp], writes=[bj, bss])
                rstd_from_ss(S, ss[:], bss, D)
                x2, bx2 = x2s.next()
                S.op(DVE, lambda e, x2=x2, p=p, ss=ss: e.scalar_tensor_tensor(out=x2[:], in0=p[:], scalar=ss[:], in1=gpost[:],
                                                                             op0=ALU.mult, op1=ALU.mult),
                     reads=[bp, bss, bgp], writes=[bx2])
                S.op(DVE, lambda e, x2=x2, xt=xt: e.tensor_tensor(out=x2[:], in0=x2[:], in1=xt[:], op=ALU.add),
                     reads=[bx2, bx], writes=[bx2])
                S.dma(SP, x_dst[t0:t0 + 128, :], x2[:], reads=[bx2])
        S.barrier()
        S.flush(nc, sems)


SLOPES_SWA = [2.0 ** -1, 2.0 ** -3, 2.0 ** -5, 2.0 ** -7]
SLOPES_MOBA = [2.0 ** -2, 2.0 ** -4, 2.0 ** -6, 2.0 ** -8]


def make_rel(S, es, nc, ncols):
    ri = es.enter_context(nc.sbuf_tensor(U("reli"), [128, ncols], I32))
    rf = es.enter_context(nc.sbuf_tensor(U("relf"), [128, ncols], F32))
    b = Buf()
    S.op(POOL, lambda e: e.iota(ri[:], pattern=[[1, ncols]], base=0, channel_multiplier=-1), writes=[b])
    S.op(POOL, lambda e: e.tensor_copy(out=rf[:], in_=ri[:]), reads=[b], writes=[b])
    return rf, b


def phase_SWA(S, nc, sems, T, l):
    SL = T.SL
    scale = 64 ** -0.5
    with ExitStack() as es:
        def sb(name, shape, dt):
            return es.enter_context(nc.sbuf_tensor(U(name), shape, dt))

        def ps(name, shape, dt):
            return es.enter_context(nc.psum_tensor(U(name), shape, dt))
        rel, brel = make_rel(S, es, nc, 128)
        bias = [sb("bias%d" % j, [128, 2, 2, 128], F32) for j in range(2)]
        bbias = [Buf(), Buf()]
        for j in range(2):
            for g in range(2):
                m = SLOPES_SWA[2 * j + g]
                S.op(POOL, lambda e, j=j, g=g, m=m: e.tensor_scalar(out=bias[j][:, 1, g, :], in0=rel[:], scalar1=-m, scalar2=None,
                                                                    op0=ALU.mult), reads=[brel], writes=[bbias[j]])
                S.op(POOL, lambda e, j=j, g=g: e.affine_select(out=bias[j][:, 1, g, :], in_=bias[j][:, 1, g, :], pattern=[[1, 128]],
                                                               compare_op=ALU.is_ge, fill=NEG, base=0, channel_multiplier=-1),
                     reads=[bbias[j]], writes=[bbias[j]])
                S.op(POOL, lambda e, j=j, g=g, m=m: e.tensor_scalar(out=bias[j][:, 0, g, :], in0=rel[:], scalar1=-m, scalar2=-128.0 * m,
                                                                    op0=ALU.mult, op1=ALU.add), reads=[brel], writes=[bbias[j]])
                S.op(POOL, lambda e, j=j, g=g: e.affine_select(out=bias[j][:, 0, g, :], in_=bias[j][:, 0, g, :], pattern=[[-1, 128]],
                                                               compare_op=ALU.is_ge, fill=NEG, base=-1, channel_multiplier=1),
                     reads=[bbias[j]], writes=[bbias[j]])
        biasz = [sb("biasz%d" % j, [128, 2, 2, 128], F32) for j in range(2)]
        for j in range(2):
            S.op(POOL, lambda e, j=j: e.tensor_copy(out=biasz[j][:, 1], in_=bias[j][:, 1]), reads=[bbias[j]], writes=[bbias[j]])
            S.op(POOL, lambda e, j=j: e.memset(biasz[j][:, 0], NEG), writes=[bbias[j]])
        esink = sb("esink", [128, 4], F32); bes = Buf()
        S.dma(SP, esink[:], T.swa_sinks[l].partition_broadcast(128), writes=[bes])
        S.op(ACT, lambda e: e.activation(out=esink[:], in_=esink[:], func=AF.Exp), reads=[bes], writes=[bes])
        qfs = Ring([sb("qf%d" % i, [64, 4, 512], F32) for i in range(2)])
        kfs = Ring([sb("kf%d" % i, [64, 2, 640], F32) for i in range(2)])
        vfs = Ring([sb("vf%d" % i, [128, 5, 128], F32) for i in range(2)])
        qbs = Ring([sb("qb%d" % i, [64, 4, 512], BF16) for i in range(2)])
        kbs = Ring([sb("kb%d" % i, [64, 2, 640], BF16) for i in range(2)])
        vbs = Ring([sb("vb%d" % i, [128, 5, 2, 65], BF16) for i in range(2)])
        scs = Ring([ps("sc%d" % i, [128, 2, 2, 128], F32) for i in range(2)])
        pos = Ring([ps("po%d" % i, [128, 2, 65], F32) for i in range(2)])
        s2s = Ring([sb("s2%d" % i, [128, 2, 2, 128], F32) for i in range(2)])
        pbs = Ring([sb("pb%d" % i, [128, 2, 2, 128], BF16) for i in range(2)])
        dens = Ring([sb("den%d" % i, [128, 2], F32) for i in range(2)])
        oms = Ring([sb("om%d" % i, [128, 256], BF16) for i in range(2)])
        for g in range(SL // 512):
            c0 = g * 512
            qf, bqf = qfs.next(); kf, bkf = kfs.next(); vf, bvf = vfs.next()
            qb, bqb = qbs.next(); kb, bkb = kbs.next(); vb, bvb = vbs.next()
            S.dma(SP, qf[:], T.projT[0:256, c0:c0 + 512].rearrange("(h d) t -> d h t", d=64), writes=[bqf])
            if g == 0:
                S.op(POOL, lambda e, kf=kf: e.memset(kf[:, :, 0:128], 0.0), writes=[bkf])
                S.op(POOL, lambda e, vf=vf: e.memset(vf[:, 0, :], 0.0), writes=[bvf])
                S.dma(SP, kf[:, :, 128:640], T.projT[256:384, c0:c0 + 512].rearrange("(h d) t -> d h t", d=64), writes=[bkf])
                S.dma(SP, vf[:, 1:5, :], T.projTM[c0:c0 + 512, 0:128].rearrange("(n p) c -> p n c", p=128), writes=[bvf])
            else:
                S.dma(SP, kf[:], T.projT[256:384, c0 - 128:c0 + 512].rearrange("(h d) t -> d h t", d=64), writes=[bkf])
                S.dma(SP, vf[:], T.projTM[c0 - 128:c0 + 512, 0:128].rearrange("(n p) c -> p n c", p=128), writes=[bvf])
            S.op(POOL, lambda e, qb=qb, qf=qf: e.tensor_copy(out=qb[:], in_=qf[:]), reads=[bqf], writes=[bqb])
            S.op(POOL, lambda e, kb=kb, kf=kf: e.tensor_copy(out=kb[:], in_=kf[:]), reads=[bkf], writes=[bkb])
            S.op(POOL, lambda e, vb=vb: e.memset(vb[:], 1.0), writes=[bvb])
            S.op(POOL, lambda e, vb=vb, vf=vf: e.tensor_copy(out=vb[:, :, :, 0:64], in_=vf[:].rearrange("p n (j d) -> p n j d", d=64)),
                 reads=[bvf], writes=[bvb])
            for i in range(4):
                t = g * 4 + i
                cks = [0, 1]
                bsel = biasz if t == 0 else bias
                om, bom = oms.next()
                for j in range(2):
                    sc, bsc = scs.next()
                    for ck in cks:
                        S.op(PE, lambda e, sc=sc, ck=ck, j=j, i=i, kb=kb, qb=qb: e.matmul(
                            sc[:, ck, :, :], lhsT=kb[:, j, (i + ck) * 128:(i + ck + 1) * 128],
                            rhs=qb[:, 2 * j:2 * j + 2, i * 128:(i + 1) * 128], start=True, stop=True),
                            reads=[bkb, bqb], writes=[bsc], inc=(ck == 1))
                    k0 = cks[0]
                    s2, bs2 = s2s.next()
                    S.op(DVE, lambda e, s2=s2, sc=sc, j=j, k0=k0, bsel=bsel: e.scalar_tensor_tensor(
                        out=s2[:, k0:2], in0=sc[:, k0:2], scalar=scale, in1=bsel[j][:, k0:2], op0=ALU.mult, op1=ALU.add),
                        reads=[bsc, bbias[j]], writes=[bs2])
                    pb, bpb = pbs.next()
                    S.op(ACT, lambda e, pb=pb, s2=s2, k0=k0: e.activation(out=pb[:, k0:2], in_=s2[:, k0:2], func=AF.Exp),
                         reads=[bs2], writes=[bpb])
                    po, bpo = pos.next()
                    for gg in range(2):
                        for ck in cks:
                            S.op(PE, lambda e, po=po, pb=pb, gg=gg, ck=ck, i=i, j=j, vb=vb: e.matmul(
                                po[:, gg, :], lhsT=pb[:, ck, gg, :], rhs=vb[:, i + ck, j, :], start=(ck == cks[0]), stop=(ck == 1)),
                                reads=[bpb, bvb], writes=[bpo], inc=(ck == 1 and gg == 1))
                    den, bden = dens.next()
                    S.op(DVE, lambda e, den=den, po=po, j=j: e.tensor_tensor(out=den[:], in0=po[:, :, 64], in1=esink[:, 2 * j:2 * j + 2],
                                                                             op=ALU.add), reads=[bpo, bes], writes=[bden])
                    S.op(DVE, lambda e, den=den: e.reciprocal(out=den[:], in_=den[:]), reads=[bden], writes=[bden])
                    for gg in range(2):
                        h = 2 * j + gg
                        S.op(DVE, lambda e, om=om, po=po, den=den, gg=gg, h=h: e.tensor_scalar(
                            out=om[:, h * 64:(h + 1) * 64], in0=po[:, gg, 0:64], scalar1=den[:, gg:gg + 1], scalar2=None, op0=ALU.mult),
                            reads=[bpo, bden], writes=[bom])
                S.dma(SP, T.mixed[c0 + i * 128:c0 + (i + 1) * 128, 0:256], om[:], reads=[bom])
        S.barrier()
        S.flush(nc, sems)


PRUNE = 60.0


def phase_MOBA(S, nc, sems, T, l):
    SL = T.SL
    NT = SL // 128
    NB = SL // 256
    PIECE = min(2048, SL)
    with ExitStack() as es:
        def sb(name, shape, dt):
            return es.enter_context(nc.sbuf_tensor(U(name), shape, dt))

        def ps(name, shape, dt):
            return es.enter_context(nc.psum_tensor(U(name), shape, dt))
        ji = sb("ji", [33, SL], I32); jrow = sb("jrow", [33, SL], F32); bj = Buf()
        S.op(POOL, lambda e: e.iota(ji[:], pattern=[[0, NB], [1, 256]], base=0, channel_multiplier=0), writes=[bj])
        S.op(POOL, lambda e: e.tensor_copy(out=jrow[:], in_=ji[:]), reads=[bj], writes=[bj])
        cmask = sb("cmask", [128, 2, 256], F32); bcm = Buf()
        S.op(POOL, lambda e: e.memset(cmask[:], 0.0), writes=[bcm])
        for kc in range(2):
            S.op(POOL, lambda e, kc=kc: e.affine_select(out=cmask[:, kc, :], in_=cmask[:, kc, :], pattern=[[1, 256]],
                                                        compare_op=ALU.is_ge, fill=NEG, base=-128 * kc, channel_multiplier=-1),
                 reads=[bcm], writes=[bcm])
        qaug = sb("qaug", [128, SL], BF16); bqa = Buf()
        kaug = sb("kaug", [128, SL], BF16); bka = Buf()
        vaug = sb("vaug", [128, NT, 65], BF16); bva = Buf()
        selall = sb("selall", [128, NT, 32], F32); bsel = Buf()
        kmean = sb("kmean", [128, 32], F32); bkm = Buf()
        qfs = Ring([sb("qf%d" % i, [128, PIECE], F32) for i in range(2)])
        kfs = Ring([sb("kf%d" % i, [128, PIECE], F32) for i in range(2)])
        vfs = Ring([sb("vf%d" % i, [128, PIECE // 128, 64], F32) for i in range(2)])
        gsbs = Ring([sb("gsb%d" % i, [128, 32], F32) for i in range(2)])
        top8s = Ring([sb("top8%d" % i, [128, 8], F32) for i in range(2)])
        pgate = Ring([ps("pgate%d" % i, [128, 32], F32) for i in range(2)])
        pst = Ring([ps("pst%d" % i, [128, 2, 256], F32) for i in range(3)])
        pov = Ring([ps("pov%d" % i, [128, 2, 65], F32) for i in range(3)])
        sms = Ring([sb("sm%d" % i, [128, 2, 256], F32) for i in range(2)])
        pts = Ring([sb("pt%d" % i, [128, 2, 256], BF16) for i in range(3)])
        accs = Ring([sb("acc%d" % i, [128, 2, 65], F32) for i in range(2)])
        rcs = Ring([sb("rc%d" % i, [128, 2], F32) for i in range(2)])
        oms = Ring([sb("om%d" % i, [128, 2, 64], BF16) for i in range(2)])

        S.op(POOL, lambda e: e.memset(qaug[:], 0.0), writes=[bqa])
        S.op(POOL, lambda e: e.memset(kaug[:], 0.0), writes=[bka])
        S.op(POOL, lambda e: e.memset(qaug[0:1, :], 1.0), writes=[bqa])
        S.op(POOL, lambda e: e.memset(kaug[32:33, :], 1.0), writes=[bka])
        for h in range(4):
            m = SLOPES_MOBA[h]
            S.op(POOL, lambda e, m=m: e.tensor_scalar(out=qaug[32:33, :], in0=jrow[32:33, :], scalar1=-8.0 * m, scalar2=None,
                                                      op0=ALU.mult), reads=[bj], writes=[bqa])
            S.op(POOL, lambda e, m=m: e.tensor_scalar(out=kaug[0:1, :], in0=jrow[0:1, :], scalar1=8.0 * m, scalar2=None,
                                                      op0=ALU.mult), reads=[bj], writes=[bka])
            S.op(POOL, lambda e: e.memset(vaug[:], 1.0), writes=[bva])
            S.op(POOL, lambda e: e.memset(selall[:], 0.0), writes=[bsel])
            for pc in range(SL // PIECE):
                p0 = pc * PIECE
                qf, bqf = qfs.next(); kf, bkf = kfs.next(); vf, bvf = vfs.next()
                S.dma(SP, qf[64:128, :], T.projT[384 + h * 64:384 + (h + 1) * 64, p0:p0 + PIECE], writes=[bqf])
                S.dma(SP, kf[64:128, :], T.projT[640 + h * 64:640 + (h + 1) * 64, p0:p0 + PIECE], writes=[bkf])
                S.dma(SP, vf[:], T.projTM[p0:p0 + PIECE, 128 + h * 64:128 + (h + 1) * 64].rearrange("(n p) c -> p n c", p=128),
                      writes=[bvf])
                S.op(POOL, lambda e, qf=qf, p0=p0: e.tensor_copy(out=qaug[64:128, p0:p0 + PIECE], in_=qf[64:128, :]),
                     reads=[bqf], writes=[bqa])
                S.op(ACT, lambda e, kf=kf, p0=p0: e.copy(out=kaug[64:128, p0:p0 + PIECE], in_=kf[64:128, :]),
                     reads=[bkf], writes=[bka])
                S.op(POOL, lambda e, vf=vf, p0=p0: e.tensor_copy(out=vaug[:, p0 // 128:(p0 + PIECE) // 128, 0:64], in_=vf[:]),
                     reads=[bvf], writes=[bva])
                S.op(DVE, lambda e, kf=kf, p0=p0: e.tensor_reduce(
                    out=kmean[64:128, p0 // 256:(p0 + PIECE) // 256], in_=kf[64:128, :].rearrange("p (n j) -> p n j", j=256),
                    axis=AX.X, op=ALU.add), reads=[bkf], writes=[bkm])
                for tt in range(PIECE // 128):
                    t = p0 // 128 + tt
                    own = t // 2
                    if own == 0:
                        continue
                    pg, bpg = pgate.next()
                    S.op(PE, lambda e, pg=pg, qf=qf, tt=tt, own=own: e.matmul(
                        pg[:, 0:own], lhsT=qf[64:128, tt * 128:(tt + 1) * 128], rhs=kmean[64:128, 0:own], start=True, stop=True),
                        reads=[bqf, bkm], writes=[bpg])
                    gsb, bgsb = gsbs.next()
                    S.op(POOL, lambda e, gsb=gsb: e.memset(gsb[:], -1e30), writes=[bgsb])
                    S.op(DVE, lambda e, gsb=gsb, pg=pg, own=own: e.tensor_copy(out=gsb[:, 0:own], in_=pg[:, 0:own]),
                         reads=[bpg], writes=[bgsb])
                    t8, bt8 = top8s.next()
                    S.op(DVE, lambda e, t8=t8, gsb=gsb: e.max(out=t8[:], in_=gsb[:]), reads=[bgsb], writes=[bt8])
                    S.op(DVE, lambda e, t8=t8, gsb=gsb, t=t, own=own: e.tensor_scalar(
                        out=selall[:, t, 0:own], in0=gsb[:, 0:own], scalar1=t8[:, 2:3], scalar2=None, op0=ALU.is_ge),
                        reads=[bgsb, bt8], writes=[bsel])
            for c in range(NB):
                acc, bacc = accs.next()
                blocks = [c] + [n for n in range(c - 1, -1, -1) if m * 256.0 * (c - n - 1) <= PRUNE]
                for n in blocks:
                    st, bst = pst.next()
                    for kc in range(2):
                        S.op(PE, lambda e, st=st, kc=kc, n=n, c=c: e.matmul(
                            st[:, kc, :], lhsT=kaug[:, n * 256 + kc * 128:n * 256 + (kc + 1) * 128],
                            rhs=qaug[:, c * 256:(c + 1) * 256], start=True, stop=True),
                            reads=[bka, bqa], writes=[bst], inc=(kc == 1))
                    pt, bpt = pts.next()
                    if n == c:
                        sm, bsm = sms.next()
                        S.op(DVE, lambda e, sm=sm, st=st: e.scalar_tensor_tensor(
                            out=sm[:], in0=st[:], scalar=0.125, in1=cmask[:], op0=ALU.mult, op1=ALU.add),
                            reads=[bst, bcm], writes=[bsm])
                        S.op(ACT, lambda e, pt=pt, sm=sm: e.activation(out=pt[:], in_=sm[:], func=AF.Exp), reads=[bsm], writes=[bpt])
                    else:
                        cst = -m * 256.0 * (c - n)
                        S.op(ACT, lambda e, pt=pt, st=st, cst=cst: e.activation(out=pt[:], in_=st[:], func=AF.Exp, scale=0.125, bias=cst),
                             reads=[bst], writes=[bpt])
                    ov, bov = pov.next()
                    for half in range(2):
                        for kc in range(2):
                            S.op(PE, lambda e, ov=ov, pt=pt, half=half, kc=kc, n=n: e.matmul(
                                ov[:, half, :], lhsT=pt[:, kc, half * 128:(half + 1) * 128], rhs=vaug[:, 2 * n + kc, :],
                                start=(kc == 0), stop=(kc == 1)), reads=[bpt, bva], writes=[bov], inc=(kc == 1 and half == 1))
                    if n == c:
                        S.op(DVE, lambda e, acc=acc, ov=ov: e.tensor_copy(out=acc[:], in_=ov[:]), reads=[bov], writes=[bacc])
                    else:
                        for half in range(2):
                            S.op(DVE, lambda e, acc=acc, ov=ov, half=half, c=c, n=n: e.scalar_tensor_tensor(
                                out=acc[:, half, :], in0=ov[:, half, :], scalar=selall[:, 2 * c + half, n:n + 1], in1=acc[:, half, :],
                                op0=ALU.mult, op1=ALU.add), reads=[bov, bsel, bacc], writes=[bacc])
                rc, brc = rcs.next()
                S.op(DVE, lambda e, rc=rc, acc=acc: e.reciprocal(out=rc[:], in_=acc[:, :, 64]), reads=[bacc], writes=[brc])
                om, bom = oms.next()
                for half in range(2):
                    S.op(DVE, lambda e, om=om, acc=acc, rc=rc, half=half: e.tensor_scalar(
                        out=om[:, half, :], in0=acc[:, half, 0:64], scalar1=rc[:, half:half + 1], scalar2=None, op0=ALU.mult),
                        reads=[bacc, brc], writes=[bom])
                S.dma(SP, T.mixed[c * 256:(c + 1) * 256, 256 + h * 64:256 + (h + 1) * 64].rearrange("(a p) d -> p a d", p=128),
                      om[:], reads=[bom])
        S.barrier()
        S.flush(nc, sems)


def phase_GDN(S, nc, sems, T, l):
    SL = T.SL
    DK = 128
    STOP = getattr(T, "gdn_stop", 4)
    with ExitStack() as es:
        def sb(name, shape, dt):
            return es.enter_context(nc.sbuf_tensor(U(name), shape, dt))

        def ps(name, shape, dt):
            return es.enter_context(nc.psum_tensor(U(name), shape, dt))
        C = Ctx()
        banks = [ps("bank%d" % i, [128, 512], F32) for i in range(8)]
        C.pmisc = Ring([banks[7]])
        identf, bidf = make_ident(S, es, nc, F32)
        C.identf, C.bidentf = identf, bidf
        ones = sb("ones", [128, 128], F32); bones = Buf()
        S.op(POOL, lambda e: e.memset(ones[:], 1.0), writes=[bones])
        masks = sb("masks", [128, 2, 128], F32); bmask = Buf()
        S.op(POOL, lambda e: e.memset(masks[:], 1.0), writes=[bmask])
        S.op(POOL, lambda e: e.affine_select(out=masks[:, 0, :], in_=masks[:, 0, :], pattern=[[1, 128]], compare_op=ALU.is_ge,
                                             fill=0.0, base=-1, channel_multiplier=-1), reads=[bmask], writes=[bmask])
        S.op(POOL, lambda e: e.affine_select(out=masks[:, 1, :], in_=masks[:, 1, :], pattern=[[1, 128]], compare_op=ALU.is_ge,
                                             fill=0.0, base=0, channel_multiplier=-1), reads=[bmask], writes=[bmask])
        S.op(POOL, lambda e: e.memset(masks[0:64, :, 64:128], 0.0), reads=[bmask], writes=[bmask])
        blkblk = sb("blkblk", [128, 128], F32); blk = sb("blk", [128, 2], F32); bblk = Buf()
        S.op(POOL, lambda e: e.memset(blkblk[:], 0.0), writes=[bblk])
        S.op(POOL, lambda e: e.memset(blkblk[0:64, 0:64], 1.0), writes=[bblk])
        S.op(POOL, lambda e: e.memset(blkblk[64:128, 64:128], 1.0), writes=[bblk])
        S.op(POOL, lambda e: e.memset(blk[:], 0.0), writes=[bblk])
        S.op(POOL, lambda e: e.memset(blk[0:64, 0:1], 1.0), writes=[bblk])
        S.op(POOL, lambda e: e.memset(blk[64:128, 1:2], 1.0), writes=[bblk])
        cw, bcw = load_rows_T(S, C, nc, es, T.dn_conv_w[l], 4, 1536, "dncw")
        expA = sb("expA", [128, 4], F32); bA = Buf()
        S.dma(SP, expA[:], T.dn_a_log[l].partition_broadcast(128), writes=[bA])
        S.op(ACT, lambda e: e.activation(out=expA[:], in_=expA[:], func=AF.Exp), reads=[bA], writes=[bA])
        S.op(DVE, lambda e: e.tensor_scalar(out=expA[:], in0=expA[:], scalar1=-1.0, scalar2=None, op0=ALU.mult), reads=[bA], writes=[bA])
        dtb = sb("dtb", [128, 4], F32); bdt = Buf()
        S.dma(SP, dtb[:], T.dn_dt_bias[l].partition_broadcast(128), writes=[bdt])
        wn = sb("wn", [128, 128], F32); bwn = Buf()
        S.dma(SP, wn[:], T.dn_norm[l].partition_broadcast(128), writes=[bwn])
        state = [sb("state%d" % h, [128, 128], F32) for h in range(4)]
        bstate = [Buf() for _ in range(4)]
        vn = [[sb("vn%d_%d" % (h, cc), [128, 128], F32) for cc in range(2)] for h in range(4)]
        bvn = [[Buf() for cc in range(2)] for h in range(4)]
        for h in range(4):
            S.op(POOL, lambda e, h=h: e.memset(state[h][:], 0.0), writes=[bstate[h]])
            for cc in range(2):
                S.op(POOL, lambda e, h=h, cc=cc: e.memset(vn[h][cc][:], 0.0), writes=[bvn[h][cc]])
        raws = Ring([sb("raw%d" % i, [128, 515], F32) for i in range(3)])
        caccs = Ring([sb("cacc%d" % i, [128, 512], F32) for i in range(3)])
        cts = [Ring([sb("ct%d_%d" % (i, k), [128, 512], F32) for k in range(2)]) for i in range(12)]
        sqts = [Ring([sb("sqt%d_%d" % (i, k), [128, 512], F32) for k in range(1)]) for i in range(8)]
        abs_ = Ring([sb("ab%d" % i, [128, 8], F32) for i in range(2)])
        zs = Ring([sb("z%d" % i, [128, 512], F32) for i in range(2)])
        scal = Ring([sb("scal%d" % i, [128, 96], F32) for i in range(2)])
        NSLOT = 3
        def slot_rings(k):
            R = Ctx()
            R.diags = Ring([sb("diag%d_%d" % (k, i), [128, 3, 128], F32) for i in range(1)])
            R.dmins = Ring([sb("dmin%d_%d" % (k, i), [128, 128], F32) for i in range(1)])
            R.Es = Ring([sb("E%d_%d" % (k, i), [128, 128], F32) for i in range(1)])
            R.F12s = Ring([sb("F12%d_%d" % (k, i), [128, 2, 128], F32) for i in range(1)])
            R.UAs = Ring([sb("UA%d_%d" % (k, i), [128, 2, 128], F32) for i in range(1)])
            R.Ls = Ring([sb("L%d_%d" % (k, i), [128, 128], F32) for i in range(1)])
            R.pws = Ring([sb("pw%d_%d" % (k, i), [128, 2, 128], F32) for i in range(2)])
            R.Xs = Ring([sb("X%d_%d" % (k, i), [128, 256], F32) for i in range(2)])
            R.kdecs = Ring([sb("kdec%d_%d" % (k, i), [128, 128], F32) for i in range(1)])
            R.wTs = Ring([sb("wT%d_%d" % (k, i), [128, 128], F32) for i in range(1)])
            R.oqs = Ring([sb("oq%d_%d" % (k, i), [128, 128], F32) for i in range(1)])
            R.vtmps = Ring([sb("vtmp%d_%d" % (k, i), [128, 128], F32) for i in range(1)])
            return R
        SLOTS = [slot_rings(k) for k in range(NSLOT)]
        oalls = Ring([sb("oall%d" % i, [128, 4, 128], F32) for i in range(2)])
        zsil = Ring([sb("zsil%d" % i, [128, 512], F32) for i in range(2)])
        oms = Ring([sb("om%d" % i, [128, 512], BF16) for i in range(2)])
        junkg = Ring([sb("junkg", [128, 128], F32)])
        BK = [Buf("psum_bank%d" % i) for i in range(8)]
        bA_ = banks[0]; bufA = BK[0]
        for k_ in range(NSLOT):
            R = SLOTS[k_]
            XA, XB = banks[1 + 2 * k_], banks[2 + 2 * k_]
            R.bXA, R.bXB = BK[1 + 2 * k_], BK[2 + 2 * k_]
            R.p_rb = XA[:, 0:384].rearrange("p (a i) -> p a i", i=128)
            R.p_Lt = XA[:, 384:512]
            R.p_pw = XA[:, 0:256].rearrange("p (a i) -> p a i", i=128)
            R.p_app = XA[:, 256:512]
            R.p_wT = XA[:, 0:128]
            R.p_g = XB[:, 0:256].rearrange("p (a i) -> p a i", i=128)
            R.p_tr = XB[:, 256:512].rearrange("p (a i) -> p a i", i=128)
            R.p_wq = XB[:, 0:256].rearrange("p (a i) -> p a i", i=128)
            R.p_av = XB[:, 256:384]
            R.p_kv = XB[:, 384:512]

        for g in range(SL // 512):
            p0 = g * 512
            ct = {}
            sq = {}
            for h in range(4):
                for part in range(3):
                    row0 = 896 + part * 512 + h * 128
                    ch = part * 4 + h
                    raw, braw = raws.next()
                    if g == 0:
                        S.op(POOL, lambda e, raw=raw: e.memset(raw[:, 0:3], 0.0), writes=[braw])
                        S.dma(SP, raw[:, 3:515], T.projT[row0:row0 + 128, 0:512], writes=[braw])
                    else:
                        S.dma(SP, raw[:], T.projT[row0:row0 + 128, p0 - 3:p0 + 512], writes=[braw])
                    ca, bca = caccs.next()
                    S.op(DVE, lambda e, ca=ca, raw=raw, ch=ch: e.tensor_scalar(out=ca[:], in0=raw[:, 3:515], scalar1=cw[:, ch, 3:4],
                                                                               scalar2=None, op0=ALU.mult), reads=[braw, bcw], writes=[bca])
                    for k in (2, 1, 0):
                        eng = DVE
                        S.op(eng, lambda e, ca=ca, raw=raw, ch=ch, k=k: e.scalar_tensor_tensor(
                            out=ca[:], in0=raw[:, k:k + 512], scalar=cw[:, ch, k:k + 1], in1=ca[:], op0=ALU.mult, op1=ALU.add),
                            reads=[braw, bcw, bca], writes=[bca])
                    c_, bc_ = cts[ch].next()
                    S.op(ACT, lambda e, c_=c_, ca=ca: e.activation(out=c_[:], in_=ca[:], func=AF.Silu), reads=[bca], writes=[bc_])
                    ct[(part, h)] = (c_, bc_)
                    if part < 2:
                        s_, bs_ = sqts[part * 4 + h].next()
                        S.op(POOL, lambda e, s_=s_, c_=c_: e.tensor_tensor(out=s_[:], in0=c_[:], in1=c_[:], op=ALU.mult),
                             reads=[bc_], writes=[bs_])
                        sq[(part, h)] = (s_, bs_)
            for i in range(4):
                t0 = p0 + i * 128
                cs = slice(i * 128, (i + 1) * 128)
                ab, bab = abs_.next()
                S.dma(SP, ab[:], T.projTM[t0:t0 + 128, 896:904], writes=[bab])
                z, bz = zs.next()
                S.dma(SP, z[:], T.projTM[t0:t0 + 128, 384:896], writes=[bz])
                sc, bsc = scal.next()
                for part in range(2):
                    for h in range(4):
                        s_, bs_ = sq[(part, h)]
                        idx = part * 4 + h
                        S.op(PE, lambda e, s_=s_, idx=idx, cs=cs: e.matmul(bA_[:, idx * 2:idx * 2 + 2], lhsT=s_[:, cs], rhs=ones[:, 0:2],
                                                                           start=True, stop=True),
                             reads=[bs_, bones], writes=[bufA], inc=(idx == 7))
                S.op(DVE, lambda e, sc=sc: e.tensor_scalar(out=sc[:, 0:8], in0=bA_[:, 0:16].rearrange("p (a b) -> p a b", b=2)[:, :, 0],
                                                           scalar1=EPS, scalar2=None, op0=ALU.add), reads=[], writes=[bsc, bufA])
                S.op(ACT, lambda e, sc=sc: e.activation(out=sc[:, 0:8], in_=sc[:, 0:8], func=AF.Ln), reads=[bsc], writes=[bsc])
                S.op(DVE, lambda e, sc=sc: e.tensor_scalar(out=sc[:, 52:56], in0=sc[:, 4:8], scalar1=-0.5, scalar2=None, op0=ALU.mult),
                     reads=[bsc], writes=[bsc])
                S.op(ACT, lambda e, sc=sc: e.activation(out=sc[:, 0:8], in_=sc[:, 0:8], func=AF.Exp, scale=-0.5), reads=[bsc], writes=[bsc])
                S.op(ACT, lambda e, sc=sc, ab=ab: e.activation(out=sc[:, 8:12], in_=ab[:, 0:4], func=AF.Sigmoid), reads=[bab], writes=[bsc])
                S.op(DVE, lambda e, sc=sc, ab=ab: e.tensor_tensor(out=sc[:, 20:24], in0=ab[:, 4:8], in1=dtb[:], op=ALU.add),
                     reads=[bab, bdt], writes=[bsc])
                S.op(DVE, lambda e, sc=sc: e.tensor_scalar(out=sc[:, 72:76], in0=sc[:, 20:24], scalar1=-1.0, scalar2=None, op0=ALU.mult),
                     reads=[bsc], writes=[bsc])
                S.op(DVE, lambda e, sc=sc: e.tensor_tensor(out=sc[:, 72:76], in0=sc[:, 72:76], in1=sc[:, 20:24], op=ALU.max),
                     reads=[bsc], writes=[bsc])
                S.op(ACT, lambda e, sc=sc: e.activation(out=sc[:, 72:76], in_=sc[:, 72:76], func=AF.Exp, scale=-1.0), reads=[bsc], writes=[bsc])
                S.op(ACT, lambda e, sc=sc: e.activation(out=sc[:, 72:76], in_=sc[:, 72:76], func=AF.Ln, bias=1.0), reads=[bsc], writes=[bsc])
                S.op(DVE, lambda e, sc=sc: e.scalar_tensor_tensor(out=sc[:, 76:80], in0=sc[:, 20:24], scalar=0.0, in1=sc[:, 72:76],
                                                                  op0=ALU.max, op1=ALU.add), reads=[bsc], writes=[bsc])
                S.op(DVE, lambda e, sc=sc: e.tensor_tensor(out=sc[:, 12:16], in0=sc[:, 76:80], in1=expA[:], op=ALU.mult),
                     reads=[bsc, bA], writes=[bsc])
                for cc in range(2):
                    S.op(DVE, lambda e, sc=sc, cc=cc: e.tensor_scalar(out=sc[:, 56 + cc * 4:60 + cc * 4], in0=sc[:, 12:16],
                                                                      scalar1=blk[:, cc:cc + 1], scalar2=None, op0=ALU.mult),
                         reads=[bsc, bblk], writes=[bsc])
                S.op(PE, lambda e, sc=sc: e.matmul(bA_[:, 16:20], lhsT=masks[:, 1, :], rhs=sc[:, 12:16], start=True, stop=True),
                     reads=[bsc, bmask], writes=[bufA], inc=False)
                S.op(PE, lambda e, sc=sc: e.matmul(bA_[:, 20:24], lhsT=blkblk[:], rhs=sc[:, 12:16], start=True, stop=True),
                     reads=[bsc, bblk], writes=[bufA], inc=False)
                S.op(PE, lambda e, sc=sc: e.matmul(bA_[:, 24:32], lhsT=ones[:], rhs=sc[:, 56:64], start=True, stop=True),
                     reads=[bsc, bones], writes=[bufA])
                S.op(DVE, lambda e, sc=sc: e.tensor_copy(out=sc[:, 16:20], in_=bA_[:, 16:20]), reads=[], writes=[bsc, bufA])
                S.op(DVE, lambda e, sc=sc: e.tensor_tensor(out=sc[:, 20:24], in0=bA_[:, 20:24], in1=sc[:, 16:20], op=ALU.subtract),
                     reads=[bsc], writes=[bsc, bufA])
                S.op(ACT, lambda e, sc=sc: e.activation(out=sc[:, 24:28], in_=sc[:, 16:20], func=AF.Exp), reads=[bsc], writes=[bsc])
                S.op(ACT, lambda e, sc=sc: e.activation(out=sc[:, 28:32], in_=sc[:, 20:24], func=AF.Exp), reads=[bsc], writes=[bsc])
                S.op(ACT, lambda e, sc=sc: e.activation(out=sc[:, 64:72], in_=bA_[:, 24:32], func=AF.Exp), reads=[], writes=[bsc, bufA])
                S.op(DVE, lambda e, sc=sc: e.tensor_tensor(out=sc[:, 40:44], in0=sc[:, 8:12], in1=sc[:, 4:8], op=ALU.mult), reads=[bsc], writes=[bsc])
                S.op(DVE, lambda e, sc=sc: e.tensor_tensor(out=sc[:, 32:36], in0=sc[:, 40:44], in1=sc[:, 24:28], op=ALU.mult), reads=[bsc], writes=[bsc])
                S.op(DVE, lambda e, sc=sc: e.tensor_tensor(out=sc[:, 36:40], in0=sc[:, 4:8], in1=sc[:, 28:32], op=ALU.mult), reads=[bsc], writes=[bsc])
                S.op(DVE, lambda e, sc=sc: e.tensor_scalar(out=sc[:, 44:48], in0=sc[:, 0:4], scalar1=DK ** -0.5, scalar2=None, op0=ALU.mult),
                     reads=[bsc], writes=[bsc])
                S.op(DVE, lambda e, sc=sc: e.tensor_tensor(out=sc[:, 48:52], in0=sc[:, 44:48], in1=sc[:, 24:28], op=ALU.mult), reads=[bsc], writes=[bsc])
                oall, boall = oalls.next()
                def head_gen(h, R):
                    if STOP <= 1:
                        return
                    yield
                    qT, bqT = ct[(0, h)]
                    kT, bkT = ct[(1, h)]
                    vT, bvT = ct[(2, h)]
                    dg, bdg = R.diags.next()
                    for a, col in enumerate((40 + h, 44 + h, 16 + h)):
                        S.op(POOL, lambda e, dg=dg, a=a, col=col, sc=sc: e.tensor_scalar(out=dg[:, a, :], in0=identf[:], scalar1=sc[:, col:col + 1],
                                                                                       scalar2=None, op0=ALU.mult), reads=[bsc, bidf], writes=[bdg])
                    yield
                    S.op(PE, lambda e, dg=dg: e.matmul(R.p_rb, lhsT=ones[:], rhs=dg[:], start=True, stop=True), reads=[bdg, bones], writes=[R.bXA])
                    S.op(PE, lambda e, kT=kT, cs=cs: e.matmul(R.p_g[:, 0, :], lhsT=kT[:, cs], rhs=kT[:, cs], start=True, stop=True),
                         reads=[bkT], writes=[R.bXB], inc=False)
                    S.op(PE, lambda e, kT=kT, qT=qT, cs=cs: e.matmul(R.p_g[:, 1, :], lhsT=kT[:, cs], rhs=qT[:, cs], start=True, stop=True),
                         reads=[bkT, bqT], writes=[R.bXB])
                    S.op(PE, lambda e, kT=kT, cs=cs: e.transpose(out=R.p_tr[:, 0, :], in_=kT[:, cs], identity=identf[:]),
                         reads=[bkT, bidf], writes=[R.bXB], inc=False)
                    S.op(PE, lambda e, vT=vT, cs=cs: e.transpose(out=R.p_tr[:, 1, :], in_=vT[:, cs], identity=identf[:]),
                         reads=[bvT, bidf], writes=[R.bXB])
                    dm, bdm = R.dmins.next()
                    S.op(DVE, lambda e, dm=dm, sc=sc, h=h: e.tensor_scalar(out=dm[:], in0=R.p_rb[:, 2, :], scalar1=sc[:, 16 + h:17 + h], scalar2=0.0,
                                                                           op0=ALU.subtract, op1=ALU.min), reads=[bsc], writes=[bdm, R.bXA])
                    E, bE = R.Es.next()
                    S.op(ACT, lambda e, E=E, dm=dm, sc=sc, h=h: e.activation(out=E[:], in_=dm[:], func=AF.Exp, bias=sc[:, 52 + h:53 + h]),
                         reads=[bdm, bsc], writes=[bE])
                    F12, bF = R.F12s.next()
                    S.op(DVE, lambda e, F12=F12: e.tensor_tensor(out=F12[:], in0=R.p_rb[:, 0:2, :], in1=masks[:], op=ALU.mult),
                         reads=[bmask], writes=[bF, R.bXA])
                    for a in range(2):
                        S.op(POOL, lambda e, F12=F12, E=E, a=a: e.tensor_tensor(out=F12[:, a, :], in0=F12[:, a, :], in1=E[:], op=ALU.mult),
                             reads=[bF, bE], writes=[bF])
                    UA, bUA = R.UAs.next()
                    S.op(DVE, lambda e, UA=UA, F12=F12: e.tensor_tensor(out=UA[:], in0=R.p_g, in1=F12[:], op=ALU.mult),
                         reads=[bF], writes=[bUA, R.bXB])
                    X, bX = R.Xs.next()
                    kd, bkd = R.kdecs.next()
                    S.op(ACT, lambda e, X=X, sc=sc, h=h: e.activation(out=X[:, 0:128], in_=R.p_tr[:, 1, :], func=AF.Identity, scale=sc[:, 8 + h:9 + h]),
                         reads=[bsc], writes=[bX, R.bXB])
                    S.op(ACT, lambda e, X=X, sc=sc, h=h: e.activation(out=X[:, 128:256], in_=R.p_tr[:, 0, :], func=AF.Identity, scale=sc[:, 32 + h:33 + h]),
                         reads=[bsc], writes=[bX, R.bXB])
                    S.op(ACT, lambda e, kd=kd, sc=sc, h=h: e.activation(out=kd[:], in_=R.p_tr[:, 0, :], func=AF.Identity, scale=sc[:, 36 + h:37 + h]),
                         reads=[bsc], writes=[bkd, R.bXB])
                    yield
                    S.op(PE, lambda e, UA=UA: e.transpose(out=R.p_Lt, in_=UA[:, 0, :], identity=identf[:]), reads=[bUA, bidf], writes=[R.bXA])
                    L, bL = R.Ls.next()
                    S.op(ACT, lambda e, L=L: e.copy(out=L[:], in_=R.p_Lt), reads=[], writes=[bL, R.bXA])
                    if STOP <= 2:
                        return
                    Ucur, bUcur = UA[:, 0, :], bUA
                    Lcur, bLcur = L[:], bL
                    sign = ALU.subtract
                    for lev in range(6):
                        yield
                        pa, bpa = R.p_app, R.bXA
                        S.op(PE, lambda e, pa=pa, Ucur=Ucur, X=X: e.matmul(pa, lhsT=Ucur, rhs=X[:], start=True, stop=True),
                             reads=[bUcur, bX], writes=[bpa])
                        Xn, bXn = R.Xs.next()
                        S.op(DVE, lambda e, Xn=Xn, X=X, pa=pa, sign=sign: e.tensor_tensor(out=Xn[:], in0=X[:], in1=pa, op=sign),
                             reads=[bX], writes=[bXn, bpa])
                        X, bX = Xn, bXn
                        sign = ALU.add
                        if lev == 5:
                            break
                        yield
                        pp, bpp = R.p_pw, R.bXA
                        S.op(PE, lambda e, pp=pp, Ucur=Ucur, Lcur=Lcur: e.matmul(pp[:, 0, :], lhsT=Lcur, rhs=Ucur, start=True, stop=True),
                             reads=[bUcur, bLcur], writes=[bpp], inc=(lev == 4))
                        if lev < 4:
                            S.op(PE, lambda e, pp=pp, Ucur=Ucur, Lcur=Lcur: e.matmul(pp[:, 1, :], lhsT=Ucur, rhs=Lcur, start=True, stop=True),
                                 reads=[bUcur, bLcur], writes=[bpp])
                        pw, bpw = R.pws.next()
                        na = 2 if lev < 4 else 1
                        S.op(ACT, lambda e, pw=pw, pp=pp, na=na: e.copy(out=pw[:, 0:na, :], in_=pp[:, 0:na, :]),
                             reads=[], writes=[bpw, bpp])
                        Ucur, bUcur = pw[:, 0, :], bpw
                        Lcur, bLcur = pw[:, 1, :], bpw
                    yield
                    S.op(PE, lambda e, X=X: e.transpose(out=R.p_wT, in_=X[:, 128:256], identity=identf[:]), reads=[bX, bidf], writes=[R.bXA])
                    wT, bwT = R.wTs.next()
                    S.op(ACT, lambda e, wT=wT: e.copy(out=wT[:], in_=R.p_wT), reads=[], writes=[bwT, R.bXA])
                    if STOP <= 3 or STOP == 6:
                        return
                    RS = {7: 1, 8: 2, 9: 3, 10: 4}.get(STOP, 99)
                    st, bst = state[h], bstate[h]
                    for cc in range(2):
                        yield
                        v_, bv_ = vn[h][cc], bvn[h][cc]
                        S.op(PE, lambda e, wT=wT, st=st: e.matmul(R.p_wq[:, 0, :], lhsT=wT[:], rhs=st[:], start=True, stop=True),
                             reads=[bwT, bst], writes=[R.bXB], inc=False)
                        S.op(PE, lambda e, qT=qT, st=st, cs=cs: e.matmul(R.p_wq[:, 1, :], lhsT=qT[:, cs], rhs=st[:], start=True, stop=True),
                             reads=[bqT, bst], writes=[R.bXB])
                        if RS <= 1:
                            continue
                        vt, bvt = R.vtmps.next()
                        S.op(DVE, lambda e, vt=vt, X=X: e.tensor_tensor(out=vt[:], in0=X[:, 0:128], in1=R.p_wq[:, 0, :], op=ALU.subtract),
                             reads=[bX], writes=[bvt, R.bXB])
                        S.op(DVE, lambda e, v_=v_, vt=vt, cc=cc: e.tensor_scalar(out=v_[:], in0=vt[:], scalar1=blk[:, cc:cc + 1], scalar2=None,
                                                                                op0=ALU.mult), reads=[bvt, bblk], writes=[bv_])
                        oq, boq = R.oqs.next()
                        S.op(ACT, lambda e, oq=oq, sc=sc, h=h: e.activation(out=oq[:], in_=R.p_wq[:, 1, :], func=AF.Identity,
                                                                           scale=sc[:, 48 + h:49 + h]), reads=[bsc], writes=[boq, R.bXB])
                        if RS <= 2:
                            continue
                        yield
                        S.op(PE, lambda e, UA=UA, v_=v_: e.matmul(R.p_av, lhsT=UA[:, 1, :], rhs=v_[:], start=True, stop=True),
                             reads=[bUA, bv_], writes=[R.bXB])
                        S.op(PE, lambda e, kd=kd, v_=v_: e.matmul(R.p_kv, lhsT=kd[:], rhs=v_[:], start=True, stop=True),
                             reads=[bkd, bv_], writes=[R.bXB])
                        if RS <= 3:
                            continue
                        S.op(DVE, lambda e, st=st, sc=sc, cc=cc, h=h: e.scalar_tensor_tensor(
                            out=st[:], in0=st[:], scalar=sc[:, 64 + cc * 4 + h:65 + cc * 4 + h], in1=R.p_kv, op0=ALU.mult, op1=ALU.add),
                            reads=[bst, bsc], writes=[bst, R.bXB])
                        if RS <= 4:
                            continue
                        S.op(DVE, lambda e, oq=oq: e.tensor_tensor(out=oq[:], in0=oq[:], in1=R.p_av, op=ALU.add),
                             reads=[boq], writes=[boq, R.bXB])
                        if cc == 0:
                            S.op(DVE, lambda e, oall=oall, oq=oq, h=h: e.tensor_scalar(out=oall[:, h, :], in0=oq[:], scalar1=blk[:, 0:1],
                                                                                     scalar2=None, op0=ALU.mult), reads=[boq, bblk], writes=[boall])
                        else:
                            S.op(DVE, lambda e, oall=oall, oq=oq, h=h: e.scalar_tensor_tensor(
                                out=oall[:, h, :], in0=oq[:], scalar=blk[:, 1:2], in1=oall[:, h, :], op0=ALU.mult, op1=ALU.add),
                                reads=[boq, bblk, boall], writes=[boall])

                pending = list(range(4))
                active = []
                free = list(range(NSLOT))
                while pending or active:
                    while pending and free:
                        k_ = free.pop(0)
                        active.append((head_gen(pending.pop(0), SLOTS[k_]), k_))
                    nxt = []
                    for gen_, k_ in active:
                        try:
                            next(gen_)
                            nxt.append((gen_, k_))
                        except StopIteration:
                            free.append(k_)
                    active = nxt
                if STOP <= 3 or STOP == 5 or STOP >= 7:
                    continue
                if STOP == 6:
                    S.op(POOL, lambda e, oall=oall: e.memset(oall[:], 0.5), writes=[boall])
                zl, bzl = zsil.next()
                S.op(ACT, lambda e, zl=zl, z=z: e.activation(out=zl[:], in_=z[:], func=AF.Silu), reads=[bz], writes=[bzl])
                for h in range(4):
                    jk, bjk = junkg.next()
                    S.op(DVE, lambda e, jk=jk, oall=oall, h=h, sc=sc: e.scalar_tensor_tensor(
                        out=jk[:], in0=oall[:, h, :], scalar=1.0, in1=oall[:, h, :], op0=ALU.mult, op1=ALU.mult, accum_out=sc[:, 80 + h:81 + h]),
                        reads=[boall], writes=[bjk, bsc])
                S.op(DVE, lambda e, sc=sc: e.tensor_scalar(out=sc[:, 80:84], in0=sc[:, 80:84], scalar1=1.0 / 128, scalar2=EPS, op0=ALU.mult, op1=ALU.add),
                     reads=[bsc], writes=[bsc])
                S.op(ACT, lambda e, sc=sc: e.activation(out=sc[:, 80:84], in_=sc[:, 80:84], func=AF.Ln), reads=[bsc], writes=[bsc])
                S.op(ACT, lambda e, sc=sc: e.activation(out=sc[:, 80:84], in_=sc[:, 80:84], func=AF.Exp, scale=-0.5), reads=[bsc], writes=[bsc])
                om, bom = oms.next()
                for h in range(4):
                    S.op(DVE, lambda e, oall=oall, h=h, sc=sc: e.scalar_tensor_tensor(
                        out=oall[:, h, :], in0=oall[:, h, :], scalar=sc[:, 80 + h:81 + h], in1=wn[:], op0=ALU.mult, op1=ALU.mult),
                        reads=[boall, bsc, bwn], writes=[boall])
                S.op(POOL, lambda e, om=om, oall=oall, zl=zl: e.tensor_tensor(out=om[:], in0=oall[:].rearrange("p h d -> p (h d)"), in1=zl[:], op=ALU.mult),
                     reads=[boall, bzl], writes=[bom])
                S.dma(SP, T.mixed[t0:t0 + 128, 512:1024], om[:], reads=[bom])
        S.barrier()
        S.flush(nc, sems)


WEIGHT_SPECS = [
    ("mix_pre_norm", [2, D]), ("w_in", [2, D, INW]), ("swa_sinks", [2, 4]), ("dn_conv_w", [2, 4, 1536]),
    ("dn_a_log", [2, 4]), ("dn_dt_bias", [2, 4]), ("dn_norm", [2, 128]), ("w_out", [2, D, D]),
    ("mix_post_norm", [2, D]), ("ffn_pre_norm", [2, D]), ("w_up", [2, D, 2 * DFF]), ("ffn_conv_w", [2, 3, 2 * DFF]),
    ("ffn_conv_b", [2, 2 * DFF]), ("w_down", [2, DFF, D]), ("ffn_post_norm", [2, D]),
]


def build(SL=8192, n_layers=2, phases="ABC", dump=(), mixed_in=False, gdn_stop=4):
    nc = bass.Bass("TRN2", target_bir_lowering=False)
    T = Ctx()
    T.SL = SL
    T.gdn_stop = gdn_stop
    T.x = nc.dram_tensor("x", [SL, D], F32, kind="ExternalInput").ap()
    for name, shape in WEIGHT_SPECS:
        setattr(T, name, nc.dram_tensor(name, shape, F32, kind="ExternalInput").ap())
    T.out = nc.dram_tensor("out", [SL, D], F32, kind="ExternalOutput").ap()

    def scratch(name, shape, dt):
        kind = "ExternalOutput" if name in dump else "Internal"
        if name == "mixed" and mixed_in:
            kind = "ExternalInput"
        return nc.dram_tensor(name, shape, dt, kind=kind).ap()
    T.projT = scratch("projT", [NFM, SL], F32)
    T.projTM = scratch("projTM", [SL, NTM], F32)
    T.mixed = scratch("mixed", [SL, D], BF16)
    T.xres1 = scratch("xres1", [SL, D], F32)
    T.gsc = scratch("gsc", [DFF, SL], BF16)
    T.xmid = scratch("xmid", [SL, D], F32)
    S = Sched()
    with ExitStack() as es:
        sems = {k: es.enter_context(nc.semaphore(k.replace("_", ""))) for k in S.semkeys()}
        for l in range(n_layers):
            x_src = T.x if l == 0 else T.xmid
            x_dst = T.out if l == n_layers - 1 else T.xmid
            if "A" in phases:
                phase_A(S, nc, sems, T, l, x_src)
            if "B" in phases or "S" in phases:
                phase_SWA(S, nc, sems, T, l)
            if "B" in phases or "M" in phases:
                phase_MOBA(S, nc, sems, T, l)
            if "B" in phases or "G" in phases:
                phase_GDN(S, nc, sems, T, l)
            if "C" in phases:
                phase_C1(S, nc, sems, T, l, x_src)
                phase_C2(S, nc, sems, T, l, x_dst)
    return nc, S


def kernel(**inputs):
    x = np.ascontiguousarray(inputs["x"], dtype=np.float32)
    B, SL, _ = x.shape
    nc, _ = build(SL, 2)
    w = {name: np.ascontiguousarray(inputs[name], dtype=np.float32) for name, _ in WEIGHT_SPECS}
    in_maps = [dict(w, x=x[b]) for b in range(B)]
    res = run_bass_kernel_spmd(nc, in_maps, core_ids=list(range(B)))
    return np.stack([r["out"] for r in res.results], axis=0).astype(np.float32)
```

```python
import numpy as np
from contextlib import ExitStack
import concourse.bass as bass
import concourse.mybir as mybir
from concourse.bass_utils import run_bass_kernel_spmd

F32 = mybir.dt.float32
BF16 = mybir.dt.bfloat16
I32 = mybir.dt.int32
ALU = mybir.AluOpType
AF = mybir.ActivationFunctionType
AX = mybir.AxisListType

PE, ACT, DVE, POOL, SP = "tensor", "scalar", "vector", "gpsimd", "sync"
COMPUTE = (PE, ACT, DVE, POOL)
DMA_POOL = 12

D = 1024
DFF = 2816
INW = 3336
NFM = 2432
NTM = 904
EPS = 1e-6
NEG = -30000.0
STGW = 1408


_UID = [0]


def U(name):
    _UID[0] += 1
    return "%s_%d" % (name, _UID[0])


class Buf:
    __slots__ = ("name", "w", "r")

    def __init__(self, name=""):
        self.name = name
        self.w = None
        self.r = {}


class Sched:
    def __init__(self):
        self.ops = {e: [] for e in (PE, ACT, DVE, POOL, SP)}
        self.cnt = {}
        self.waited = {e: {} for e in self.ops}
        self.dma_rr = {e: 0 for e in self.ops}
        self.n_ops = 0

    def _deps(self, eng, reads, writes, sew):
        deps = {}

        def add(t):
            if t is None:
                return
            sk, v = t
            if deps.get(sk, 0) < v:
                deps[sk] = v
        for b in reads:
            add(b.w)
        for b in writes:
            add(b.w)
            for sk, v in b.r.items():
                add((sk, v))
        out = []
        for sk, v in deps.items():
            if sk == eng and not sew:
                continue
            if self.waited[eng].get(sk, 0) >= v:
                continue
            self.waited[eng][sk] = v
            out.append((sk, v))
        return out

    def _mark(self, ticket, reads, writes):
        sk, v = ticket
        for b in reads:
            if b.r.get(sk, 0) < v:
                b.r[sk] = v
        for b in writes:
            b.w = ticket
            b.r = {}

    def op(self, eng, fn, reads=(), writes=(), inc=True, sew=None):
        if sew is None:
            sew = eng != PE
        waits = self._deps(eng, reads, writes, sew)
        c = self.cnt.get(eng, 0)
        ticket = (eng, c + 1)
        if inc:
            self.cnt[eng] = c + 1
        self._mark(ticket, reads, writes)
        self.ops[eng].append((waits, fn, eng if inc else None, 1))
        self.n_ops += 1

    def dma(self, q, out_ap, in_ap, reads=(), writes=(), **kw):
        i = self.dma_rr[q]
        self.dma_rr[q] = (i + 1) % DMA_POOL
        sk = "dma_%s_%d" % (q, i)
        c = self.cnt.get(sk, 0)
        waits = self._deps(q, reads, writes, True)
        if c > 0 and self.waited[q].get(sk, 0) < c:
            self.waited[q][sk] = c
            waits.append((sk, c))
        self.cnt[sk] = c + 16
        self._mark((sk, c + 16), reads, writes)
        self.ops[q].append((waits, lambda e: e.dma_start(out=out_ap, in_=in_ap, **kw), sk, 16))
        self.n_ops += 1

    def barrier(self):
        for e in self.ops:
            waits = []
            for sk, v in self.cnt.items():
                if sk == e:
                    continue
                if self.waited[e].get(sk, 0) < v:
                    self.waited[e][sk] = v
                    waits.append((sk, v))
            if waits:
                self.ops[e].append((waits, None, None, 0))

    def flush(self, nc, sems):
        ops = self.ops
        self.ops = {e: [] for e in ops}

        def run(e, name):
            for waits, fn, sk, inc in ops[name]:
                for wsk, v in waits:
                    e.wait_ge(sems[wsk], v)
                if fn is None:
                    continue
                ins = fn(e)
                if sk is not None:
                    ins.then_inc(sems[sk], inc)
        with nc.Block() as block:
            block.sync(lambda e: run(e, SP))
            block.tensor(lambda e: run(e, PE))
            block.scalar(lambda e: run(e, ACT))
            block.vector(lambda e: run(e, DVE))
            block.gpsimd(lambda e: run(e, POOL))

    @staticmethod
    def semkeys():
        keys = list(COMPUTE)
        for q in (PE, ACT, DVE, POOL, SP):
            for i in range(DMA_POOL):
                keys.append("dma_%s_%d" % (q, i))
        return keys


class Ring:
    def __init__(self, tiles):
        self.items = [(t, Buf()) for t in tiles]
        self.i = 0

    def next(self):
        it = self.items[self.i % len(self.items)]
        self.i += 1
        return it


class Ctx:
    pass


def make_ident(S, es, nc, dt):
    identf = es.enter_context(nc.sbuf_tensor(U("identf"), [128, 128], F32))
    b = Buf()
    S.op(POOL, lambda e: e.memset(identf[:], 1.0), writes=[b])
    S.op(POOL, lambda e: e.affine_select(out=identf[:], in_=identf[:], pattern=[[-1, 128]],
                                         compare_op=ALU.is_equal, fill=0.0, base=0, channel_multiplier=1),
         reads=[b], writes=[b])
    if dt == F32:
        return identf, b
    ident = es.enter_context(nc.sbuf_tensor(U("identb"), [128, 128], BF16))
    b2 = Buf()
    S.op(POOL, lambda e: e.tensor_copy(out=ident[:], in_=identf[:]), reads=[b], writes=[b2])
    return ident, b2


def rstd_from_ss(S, ss, bss, n):
    S.op(DVE, lambda e: e.tensor_scalar(out=ss, in0=ss, scalar1=1.0 / n, scalar2=EPS, op0=ALU.mult, op1=ALU.add),
         reads=[bss], writes=[bss])
    S.op(ACT, lambda e: e.activation(out=ss, in_=ss, func=AF.Ln), reads=[bss], writes=[bss])
    S.op(ACT, lambda e: e.activation(out=ss, in_=ss, func=AF.Exp, scale=-0.5), reads=[bss], writes=[bss])


def load_weight_bf16(S, w_dram, dst, bdst, stg_ring, colmap, nk):
    for k in range(nk):
        for (s0, s1, d0) in colmap:
            n = s1 - s0
            for o in range(0, n, STGW):
                m = min(STGW, n - o)
                stg, bs = stg_ring.next()
                S.dma(SP, stg[:, 0:m], w_dram[k * 128:(k + 1) * 128, s0 + o:s0 + o + m], writes=[bs])
                S.op(POOL, lambda e, stg=stg, m=m, k=k, dd=d0 + o: e.tensor_copy(out=dst[:, k, dd:dd + m], in_=stg[:, 0:m]),
                     reads=[bs], writes=[bdst])


def norm_rows_to_bf16(S, C, xt, bx, gbc, bg, h, bh):
    junk, bj = C.junk.next()
    ss, bss = C.ss.next()
    S.op(DVE, lambda e: e.scalar_tensor_tensor(out=junk[:], in0=xt, scalar=1.0, in1=xt, op0=ALU.mult, op1=ALU.mult,
                                               accum_out=ss[:]), reads=[bx], writes=[bj, bss])
    rstd_from_ss(S, ss[:], bss, D)
    S.op(DVE, lambda e: e.scalar_tensor_tensor(out=h, in0=xt, scalar=ss[:], in1=gbc, op0=ALU.mult, op1=ALU.mult),
         reads=[bx, bss, bg], writes=[bh])


def transpose8(S, C, h, bh, dstT, bdT, col0):
    pT, bpT = C.pT.next()
    for c in range(8):
        S.op(PE, lambda e, c=c: e.transpose(out=pT[:, c, :], in_=h[:, c * 128:(c + 1) * 128], identity=C.ident[:]),
             reads=[bh, C.bident], writes=[bpT], inc=(c == 7))
    S.op(ACT, lambda e: e.copy(out=dstT[:, :, col0:col0 + 128], in_=pT[:]), reads=[bpT], writes=[bdT])


def phase_A(S, nc, sems, T, l, x_src):
    SL = T.SL
    with ExitStack() as es:
        def sb(name, shape, dt):
            return es.enter_context(nc.sbuf_tensor(U(name), shape, dt))

        def ps(name, shape, dt):
            return es.enter_context(nc.psum_tensor(U(name), shape, dt))
        C = Ctx()
        C.ident, C.bident = make_ident(S, es, nc, BF16)
        wfm = sb("wfm", [128, 8, NFM], BF16); bwfm = Buf()
        wtm = sb("wtm", [128, 8, NTM], BF16); bwtm = Buf()
        stg = Ring([sb("stg%d" % i, [128, STGW], F32) for i in range(2)])
        gbc = sb("gbc", [128, D], F32); bg = Buf()
        C.junk = Ring([sb("junk", [128, D], BF16)])
        C.ss = Ring([sb("ss%d" % i, [128, 1], F32) for i in range(4)])
        C.pT = Ring([ps("pT%d" % i, [128, 8, 128], BF16) for i in range(2)])
        xts = Ring([sb("xt%d" % i, [128, D], F32) for i in range(2)])
        hs = Ring([sb("h%d" % i, [128, D], BF16) for i in range(2)])
        hTs = Ring([sb("hT%d" % i, [128, 8, 512], BF16) for i in range(2)])
        pfm = Ring([ps("pfm%d" % i, [128, 512], F32) for i in range(2)])
        ptm = Ring([ps("ptm%d" % i, [128, 1024], F32) for i in range(2)])
        ofm = Ring([sb("ofm%d" % i, [128, 512], F32) for i in range(4)])
        otm = Ring([sb("otm%d" % i, [128, NTM], F32) for i in range(2)])

        S.dma(SP, gbc[:], T.mix_pre_norm[l].partition_broadcast(128), writes=[bg])
        w = T.w_in[l]
        load_weight_bf16(S, w, wfm, bwfm, stg, [(0, 384, 0), (512, 1024, 384), (1280, 2816, 896)], 8)
        load_weight_bf16(S, w, wtm, bwtm, stg, [(384, 512, 0), (1024, 1280, 128), (2816, 3336, 384)], 8)

        for g in range(SL // 512):
            hT, bhT = hTs.next()
            for i in range(4):
                t0 = g * 512 + i * 128
                xt, bx = xts.next()
                S.dma(SP, xt[:], x_src[t0:t0 + 128, :], writes=[bx])
                h, bh = hs.next()
                norm_rows_to_bf16(S, C, xt[:], bx, gbc[:], bg, h[:], bh)
                transpose8(S, C, h, bh, hT, bhT, i * 128)
            for i in range(4):
                t0 = g * 512 + i * 128
                p, bp = ptm.next()
                for (n0, n1) in ((0, 512), (512, NTM)):
                    for c in range(8):
                        S.op(PE, lambda e, c=c, n0=n0, n1=n1, p=p, i=i, hT=hT: e.matmul(
                            p[:, n0:n1], lhsT=hT[:, c, i * 128:(i + 1) * 128], rhs=wtm[:, c, n0:n1],
                            start=(c == 0), stop=(c == 7)), reads=[bhT, bwtm], writes=[bp], inc=(c == 7))
                o, bo = otm.next()
                S.op(ACT, lambda e, o=o, p=p: e.copy(out=o[:], in_=p[:, 0:NTM]), reads=[bp], writes=[bo])
                S.dma(SP, T.projTM[t0:t0 + 128, :], o[:], reads=[bo])
            for ch in range(NFM // 128):
                p, bp = pfm.next()
                for c in range(8):
                    S.op(PE, lambda e, c=c, ch=ch, p=p, hT=hT: e.matmul(
                        p[:], lhsT=wfm[:, c, ch * 128:(ch + 1) * 128], rhs=hT[:, c, :],
                        start=(c == 0), stop=(c == 7)), reads=[bhT, bwfm], writes=[bp], inc=(c == 7))
                o, bo = ofm.next()
                eng = ACT if ch % 2 == 0 else DVE
                if eng == ACT:
                    S.op(ACT, lambda e, o=o, p=p: e.copy(out=o[:], in_=p[:]), reads=[bp], writes=[bo])
                else:
                    S.op(DVE, lambda e, o=o, p=p: e.tensor_copy(out=o[:], in_=p[:]), reads=[bp], writes=[bo])
                S.dma(SP, T.projT[ch * 128:(ch + 1) * 128, g * 512:(g + 1) * 512], o[:], reads=[bo])
        S.barrier()
        S.flush(nc, sems)


def load_rows_T(S, C, nc, es, src2d, nrows, ncols, name):
    nch = ncols // 128
    rows = es.enter_context(nc.sbuf_tensor(U(name + "_rows"), [8, ncols], F32))
    br = Buf()
    S.dma(SP, rows[0:nrows, :], src2d, writes=[br])
    out = es.enter_context(nc.sbuf_tensor(U(name), [128, nch, nrows], F32))
    bo = Buf()
    for c0 in range(0, nch, 16):
        n = min(16, nch - c0)
        pt, bpt = C.pmisc.next()
        for j in range(n):
            c = c0 + j
            S.op(PE, lambda e, c=c, j=j, pt=pt: e.transpose(out=pt[:, j * nrows:(j + 1) * nrows],
                                                            in_=rows[0:nrows, c * 128:(c + 1) * 128],
                                                            identity=C.identf[0:nrows, 0:nrows]),
                 reads=[br, C.bidentf], writes=[bpt], inc=(j == n - 1))
        S.op(DVE, lambda e, c0=c0, n=n, pt=pt: e.tensor_copy(
            out=out[:, c0:c0 + n, :], in_=pt[:, 0:n * nrows].rearrange("p (c k) -> p c k", k=nrows)),
            reads=[bpt], writes=[bo])
    return out, bo


def phase_C1(S, nc, sems, T, l, x_src):
    SL = T.SL
    with ExitStack() as es:
        def sb(name, shape, dt):
            return es.enter_context(nc.sbuf_tensor(U(name), shape, dt))

        def ps(name, shape, dt):
            return es.enter_context(nc.psum_tensor(U(name), shape, dt))
        C = Ctx()
        C.ident, C.bident = make_ident(S, es, nc, BF16)
        identf = sb("identf2", [128, 128], F32); bidf = Buf()
        S.op(POOL, lambda e: e.memset(identf[:], 1.0), writes=[bidf])
        S.op(POOL, lambda e: e.affine_select(out=identf[:], in_=identf[:], pattern=[[-1, 128]],
                                             compare_op=ALU.is_equal, fill=0.0, base=0, channel_multiplier=1),
             reads=[bidf], writes=[bidf])
        C.identf, C.bidentf = identf, bidf
        C.pmisc = Ring([ps("pmisc", [128, 512], F32)])
        cw = sb("cw", [128, 44, 4], F32); bcw = Buf()
        with nc.sbuf_tensor(U("crow"), [8, 2 * DFF], F32) as crow:
            bcr = Buf()
            S.dma(SP, crow[0:3, :], T.ffn_conv_w[l], writes=[bcr])
            S.dma(SP, crow[3:4, :], T.ffn_conv_b[l:l + 1, :], writes=[bcr])
            for c0 in range(0, 44, 22):
                pt, bpt = C.pmisc.next()
                for j in range(22):
                    c = c0 + j
                    S.op(PE, lambda e, c=c, j=j, pt=pt: e.transpose(out=pt[:, j * 4:(j + 1) * 4], in_=crow[0:4, c * 128:(c + 1) * 128],
                                                                    identity=identf[0:4, 0:4]),
                         reads=[bcr, bidf], writes=[bpt], inc=(j == 21))
                S.op(DVE, lambda e, c0=c0, pt=pt: e.tensor_copy(out=cw[:, c0:c0 + 22, :],
                                                                in_=pt[:, 0:88].rearrange("p (c k) -> p c k", k=4)),
                     reads=[bpt], writes=[bcw])
            S.barrier()
            S.flush(nc, sems)
        wout = sb("wout", [128, 8, D], BF16); bwout = Buf()
        wup = sb("wup", [128, 8, 2 * DFF], BF16); bwup = Buf()
        stg = Ring([sb("stg%d" % i, [128, STGW], F32) for i in range(2)])
        gpost = sb("gpost", [128, D], F32); bgp = Buf()
        gpre = sb("gpre", [128, D], F32); bgq = Buf()
        C.junk = Ring([sb("junk", [128, D], BF16)])
        C.ss = Ring([sb("ss%d" % i, [128, 1], F32) for i in range(4)])
        C.pT = Ring([ps("pT%d" % i, [128, 8, 128], BF16) for i in range(2)])
        py = Ring([ps("py", [128, D], F32)])
        pup = Ring([ps("pup%d" % i, [128, 512], F32) for i in range(3)])
        mts = Ring([sb("mt%d" % i, [128, D], BF16) for i in range(2)])
        mTs = Ring([sb("mT%d" % i, [128, 8, 128], BF16) for i in range(2)])
        xts = Ring([sb("xt%d" % i, [128, D], F32) for i in range(2)])
        x1s = Ring([sb("x1%d" % i, [128, D], F32) for i in range(2)])
        hs = Ring([sb("h%d" % i, [128, D], BF16) for i in range(2)])
        hTs = Ring([sb("hT%d" % i, [128, 8, 512], BF16) for i in range(2)])
        ubs = Ring([sb("ub%d" % i, [128, 514], F32) for i in range(3)])
        accs = Ring([sb("acc%d" % i, [128, 512], F32) for i in range(3)])
        ggs = Ring([sb("gg%d" % i, [128, 512], F32) for i in range(2)])
        gos = Ring([sb("go%d" % i, [128, 512], BF16) for i in range(3)])
        halo = sb("halo", [128, 44, 2], F32); bhalo = [Buf() for _ in range(44)]

        S.dma(SP, gpost[:], T.mix_post_norm[l].partition_broadcast(128), writes=[bgp])
        S.dma(SP, gpre[:], T.ffn_pre_norm[l].partition_broadcast(128), writes=[bgq])
        S.op(POOL, lambda e: e.memset(halo[:], 0.0), writes=bhalo)
        load_weight_bf16(S, T.w_out[l], wout, bwout, stg, [(0, D, 0)], 8)
        load_weight_bf16(S, T.w_up[l], wup, bwup, stg, [(0, 2 * DFF, 0)], 8)

        for g in range(SL // 512):
            hT, bhT = hTs.next()
            for i in range(4):
                t0 = g * 512 + i * 128
                mt, bmt = mts.next()
                S.dma(SP, mt[:], T.mixed[t0:t0 + 128, :], writes=[bmt])
                mT, bmT = mTs.next()
                transpose8(S, C, mt, bmt, mT, bmT, 0)
                xt, bx = xts.next()
                S.dma(SP, xt[:], x_src[t0:t0 + 128, :], writes=[bx])
                p, bp = py.next()
                for nb in range(2):
                    for c in range(8):
                        S.op(PE, lambda e, c=c, nb=nb, p=p, mT=mT: e.matmul(
                            p[:, nb * 512:(nb + 1) * 512], lhsT=mT[:, c, :], rhs=wout[:, c, nb * 512:(nb + 1) * 512],
                            start=(c == 0), stop=(c == 7)), reads=[bmT, bwout], writes=[bp], inc=(c == 7))
                junk, bj = C.junk.next()
                ss, bss = C.ss.next()
                S.op(ACT, lambda e, junk=junk, p=p, ss=ss: e.activation(out=junk[:], in_=p[:], func=AF.Square, accum_out=ss[:]),
                     reads=[bp], writes=[bj, bss])
                rstd_from_ss(S, ss[:], bss, D)
                x1, bx1 = x1s.next()
                S.op(DVE, lambda e, x1=x1, p=p, ss=ss: e.scalar_tensor_tensor(out=x1[:], in0=p[:], scalar=ss[:], in1=gpost[:],
                                                                             op0=ALU.mult, op1=ALU.mult),
                     reads=[bp, bss, bgp], writes=[bx1])
                S.op(DVE, lambda e, x1=x1, xt=xt: e.tensor_tensor(out=x1[:], in0=x1[:], in1=xt[:], op=ALU.add),
                     reads=[bx1, bx], writes=[bx1])
                S.dma(SP, T.xres1[t0:t0 + 128, :], x1[:], reads=[bx1])
                h, bh = hs.next()
                norm_rows_to_bf16(S, C, x1[:], bx1, gpre[:], bgq, h[:], bh)
                transpose8(S, C, h, bh, hT, bhT, i * 128)
            for f in range(22):
                accp = []
                for part in range(2):
                    ch = part * 22 + f
                    p, bp = pup.next()
                    for c in range(8):
                        S.op(PE, lambda e, c=c, ch=ch, p=p, hT=hT: e.matmul(
                            p[:], lhsT=wup[:, c, ch * 128:(ch + 1) * 128], rhs=hT[:, c, :],
                            start=(c == 0), stop=(c == 7)), reads=[bhT, bwup], writes=[bp], inc=(c == 7))
                    ub, bub = ubs.next()
                    S.op(ACT, lambda e, ub=ub, ch=ch: e.copy(out=ub[:, 0:2], in_=halo[:, ch, :]),
                         reads=[bhalo[ch]], writes=[bub])
                    S.op(ACT, lambda e, ub=ub, p=p: e.copy(out=ub[:, 2:514], in_=p[:]), reads=[bp], writes=[bub])
                    S.op(ACT, lambda e, ub=ub, ch=ch: e.copy(out=halo[:, ch, :], in_=ub[:, 512:514]),
                         reads=[bub], writes=[bhalo[ch]])
                    acc, bacc = accs.next()
                    S.op(DVE, lambda e, acc=acc, ub=ub, ch=ch: e.tensor_scalar(
                        out=acc[:], in0=ub[:, 2:514], scalar1=cw[:, ch, 2:3], scalar2=cw[:, ch, 3:4], op0=ALU.mult, op1=ALU.add),
                        reads=[bub, bcw], writes=[bacc])
                    S.op(DVE, lambda e, acc=acc, ub=ub, ch=ch: e.scalar_tensor_tensor(
                        out=acc[:], in0=ub[:, 1:513], scalar=cw[:, ch, 1:2], in1=acc[:], op0=ALU.mult, op1=ALU.add),
                        reads=[bub, bcw, bacc], writes=[bacc])
                    S.op(DVE, lambda e, acc=acc, ub=ub, ch=ch: e.scalar_tensor_tensor(
                        out=acc[:], in0=ub[:, 0:512], scalar=cw[:, ch, 0:1], in1=acc[:], op0=ALU.mult, op1=ALU.add),
                        reads=[bub, bcw, bacc], writes=[bacc])
                    accp.append((acc, bacc))
                gg, bgg = ggs.next()
                S.op(ACT, lambda e, gg=gg, a=accp[0][0]: e.activation(out=gg[:], in_=a[:], func=AF.Gelu_apprx_tanh),
                     reads=[accp[0][1]], writes=[bgg])
                go, bgo = gos.next()
                S.op(POOL, lambda e, go=go, gg=gg, a=accp[1][0]: e.tensor_tensor(out=go[:], in0=gg[:], in1=a[:], op=ALU.mult),
                     reads=[bgg, accp[1][1]], writes=[bgo])
                S.dma(SP, T.gsc[f * 128:(f + 1) * 128, g * 512:(g + 1) * 512], go[:], reads=[bgo])
        S.barrier()
        S.flush(nc, sems)


def phase_C2(S, nc, sems, T, l, x_dst):
    SL = T.SL
    with ExitStack() as es:
        def sb(name, shape, dt):
            return es.enter_context(nc.sbuf_tensor(U(name), shape, dt))

        def ps(name, shape, dt):
            return es.enter_context(nc.psum_tensor(U(name), shape, dt))
        C = Ctx()
        wdn = sb("wdn", [128, 22, D], BF16); bwdn = Buf()
        stg = Ring([sb("stg%d" % i, [128, STGW], F32) for i in range(2)])
        gpost = sb("gpost", [128, D], F32); bgp = Buf()
        C.junk = Ring([sb("junk", [128, D], BF16)])
        C.ss = Ring([sb("ss%d" % i, [128, 1], F32) for i in range(4)])
        py = Ring([ps("py%d" % i, [128, D], F32) for i in range(2)])
        gTs = Ring([sb("gT%d" % i, [128, 22, 512], BF16) for i in range(2)])
        xts = Ring([sb("xt%d" % i, [128, D], F32) for i in range(2)])
        x2s = Ring([sb("x2%d" % i, [128, D], F32) for i in range(2)])
        S.dma(SP, gpost[:], T.ffn_post_norm[l].partition_broadcast(128), writes=[bgp])
        load_weight_bf16(S, T.w_down[l], wdn, bwdn, stg, [(0, D, 0)], 22)
        for g in range(SL // 512):
            gT, bgT = gTs.next()
            S.dma(SP, gT[:], T.gsc[:, g * 512:(g + 1) * 512].rearrange("(c p) t -> p c t", p=128), writes=[bgT])
            for i in range(4):
                t0 = g * 512 + i * 128
                xt, bx = xts.next()
                S.dma(SP, xt[:], T.xres1[t0:t0 + 128, :], writes=[bx])
                p, bp = py.next()
                for nb in range(2):
                    for f in range(22):
                        S.op(PE, lambda e, f=f, nb=nb, p=p, gT=gT, i=i: e.matmul(
                            p[:, nb * 512:(nb + 1) * 512], lhsT=gT[:, f, i * 128:(i + 1) * 128],
                            rhs=wdn[:, f, nb * 512:(nb + 1) * 512], start=(f == 0), stop=(f == 21)),
                            reads=[bgT, bwdn], writes=[bp], inc=(f == 21))
                junk, bj = C.junk.next()
                ss, bss = C.ss.next()
                S.op(ACT, lambda e, junk=junk, p=p, ss=ss: e.activation(out=junk[:], in_=p[:], func=AF.Square, accum_out=ss[:]),
                     reads=[bp], writes=[bj, bss])
                rstd_from_ss(S, ss[:], bss, D)
                x2, bx2 = x2s.next()
                S.op(DVE, lambda e, x2=x2, p=p, ss=ss: e.scalar_tensor_tensor(out=x2[:], in0=p[:], scalar=ss[:], in1=gpost[:],
                                                                             op0=ALU.mult, op1=ALU.mult),
                     reads=[bp, bss, bgp], writes=[bx2])
                S.op(DVE, lambda e, x2=x2, xt=xt: e.tensor_tensor(out=x2[:], in0=x2[:], in1=xt[:], op=ALU.add),
                     reads=[bx2, bx], writes=[bx2])
                S.dma(SP, x_dst[t0:t0 + 128, :], x2[:], reads=[bx2])
        S.barrier()
        S.flush(nc, sems)


SLOPES_SWA = [2.0 ** -1, 2.0 ** -3, 2.0 ** -5, 2.0 ** -7]
SLOPES_MOBA = [2.0 ** -2, 2.0 ** -4, 2.0 ** -6, 2.0 ** -8]


def make_rel(S, es, nc, ncols):
    ri = es.enter_context(nc.sbuf_tensor(U("reli"), [128, ncols], I32))
    rf = es.enter_context(nc.sbuf_tensor(U("relf"), [128, ncols], F32))
    b = Buf()
    S.op(POOL, lambda e: e.iota(ri[:], pattern=[[1, ncols]], base=0, channel_multiplier=-1), writes=[b])
    S.op(POOL, lambda e: e.tensor_copy(out=rf[:], in_=ri[:]), reads=[b], writes=[b])
    return rf, b


def phase_SWA(S, nc, sems, T, l):
    SL = T.SL
    scale = 64 ** -0.5
    with ExitStack() as es:
        def sb(name, shape, dt):
            return es.enter_context(nc.sbuf_tensor(U(name), shape, dt))

        def ps(name, shape, dt):
            return es.enter_context(nc.psum_tensor(U(name), shape, dt))
        rel, brel = make_rel(S, es, nc, 128)
        bias = [sb("bias%d" % j, [128, 2, 2, 128], F32) for j in range(2)]
        bbias = [Buf(), Buf()]
        for j in range(2):
            for g in range(2):
                m = SLOPES_SWA[2 * j + g]
                S.op(POOL, lambda e, j=j, g=g, m=m: e.tensor_scalar(out=bias[j][:, 1, g, :], in0=rel[:], scalar1=-m, scalar2=None,
                                                                    op0=ALU.mult), reads=[brel], writes=[bbias[j]])
                S.op(POOL, lambda e, j=j, g=g: e.affine_select(out=bias[j][:, 1, g, :], in_=bias[j][:, 1, g, :], pattern=[[1, 128]],
                                                               compare_op=ALU.is_ge, fill=NEG, base=0, channel_multiplier=-1),
                     reads=[bbias[j]], writes=[bbias[j]])
                S.op(POOL, lambda e, j=j, g=g, m=m: e.tensor_scalar(out=bias[j][:, 0, g, :], in0=rel[:], scalar1=-m, scalar2=-128.0 * m,
                                                                    op0=ALU.mult, op1=ALU.add), reads=[brel], writes=[bbias[j]])
                S.op(POOL, lambda e, j=j, g=g: e.affine_select(out=bias[j][:, 0, g, :], in_=bias[j][:, 0, g, :], pattern=[[-1, 128]],
                                                               compare_op=ALU.is_ge, fill=NEG, base=-1, channel_multiplier=1),
                     reads=[bbias[j]], writes=[bbias[j]])
        biasz = [sb("biasz%d" % j, [128, 2, 2, 128], F32) for j in range(2)]
        for j in range(2):
            S.op(POOL, lambda e, j=j: e.tensor_copy(out=biasz[j][:, 1], in_=bias[j][:, 1]), reads=[bbias[j]], writes=[bbias[j]])
            S.op(POOL, lambda e, j=j: e.memset(biasz[j][:, 0], NEG), writes=[bbias[j]])
        esink = sb("esink", [128, 4], F32); bes = Buf()
        S.dma(SP, esink[:], T.swa_sinks[l].partition_broadcast(128), writes=[bes])
        S.op(ACT, lambda e: e.activation(out=esink[:], in_=esink[:], func=AF.Exp), reads=[bes], writes=[bes])
        qfs = Ring([sb("qf%d" % i, [64, 4, 512], F32) for i in range(2)])
        kfs = Ring([sb("kf%d" % i, [64, 2, 640], F32) for i in range(2)])
        vfs = Ring([sb("vf%d" % i, [128, 5, 128], F32) for i in range(2)])
        qbs = Ring([sb("qb%d" % i, [64, 4, 512], BF16) for i in range(2)])
        kbs = Ring([sb("kb%d" % i, [64, 2, 640], BF16) for i in range(2)])
        vbs = Ring([sb("vb%d" % i, [128, 5, 2, 65], BF16) for i in range(2)])
        scs = Ring([ps("sc%d" % i, [128, 2, 2, 128], F32) for i in range(2)])
        pos = Ring([ps("po%d" % i, [128, 2, 65], F32) for i in range(2)])
        s2s = Ring([sb("s2%d" % i, [128, 2, 2, 128], F32) for i in range(2)])
        pbs = Ring([sb("pb%d" % i, [128, 2, 2, 128], BF16) for i in range(2)])
        dens = Ring([sb("den%d" % i, [128, 2], F32) for i in range(2)])
        oms = Ring([sb("om%d" % i, [128, 256], BF16) for i in range(2)])
        for g in range(SL // 512):
            c0 = g * 512
            qf, bqf = qfs.next(); kf, bkf = kfs.next(); vf, bvf = vfs.next()
            qb, bqb = qbs.next(); kb, bkb = kbs.next(); vb, bvb = vbs.next()
            S.dma(SP, qf[:], T.projT[0:256, c0:c0 + 512].rearrange("(h d) t -> d h t", d=64), writes=[bqf])
            if g == 0:
                S.op(POOL, lambda e, kf=kf: e.memset(kf[:, :, 0:128], 0.0), writes=[bkf])
                S.op(POOL, lambda e, vf=vf: e.memset(vf[:, 0, :], 0.0), writes=[bvf])
                S.dma(SP, kf[:, :, 128:640], T.projT[256:384, c0:c0 + 512].rearrange("(h d) t -> d h t", d=64), writes=[bkf])
                S.dma(SP, vf[:, 1:5, :], T.projTM[c0:c0 + 512, 0:128].rearrange("(n p) c -> p n c", p=128), writes=[bvf])
            else:
                S.dma(SP, kf[:], T.projT[256:384, c0 - 128:c0 + 512].rearrange("(h d) t -> d h t", d=64), writes=[bkf])
                S.dma(SP, vf[:], T.projTM[c0 - 128:c0 + 512, 0:128].rearrange("(n p) c -> p n c", p=128), writes=[bvf])
            S.op(POOL, lambda e, qb=qb, qf=qf: e.tensor_copy(out=qb[:], in_=qf[:]), reads=[bqf], writes=[bqb])
            S.op(POOL, lambda e, kb=kb, kf=kf: e.tensor_copy(out=kb[:], in_=kf[:]), reads=[bkf], writes=[bkb])
            S.op(POOL, lambda e, vb=vb: e.memset(vb[:], 1.0), writes=[bvb])
            S.op(POOL, lambda e, vb=vb, vf=vf: e.tensor_copy(out=vb[:, :, :, 0:64], in_=vf[:].rearrange("p n (j d) -> p n j d", d=64)),
                 reads=[bvf], writes=[bvb])
            for i in range(4):
                t = g * 4 + i
                cks = [0, 1]
                bsel = biasz if t == 0 else bias
                om, bom = oms.next()
                for j in range(2):
                    sc, bsc = scs.next()
                    for ck in cks:
                        S.op(PE, lambda e, sc=sc, ck=ck, j=j, i=i, kb=kb, qb=qb: e.matmul(
                            sc[:, ck, :, :], lhsT=kb[:, j, (i + ck) * 128:(i + ck + 1) * 128],
                            rhs=qb[:, 2 * j:2 * j + 2, i * 128:(i + 1) * 128], start=True, stop=True),
                            reads=[bkb, bqb], writes=[bsc], inc=(ck == 1))
                    k0 = cks[0]
                    s2, bs2 = s2s.next()
                    S.op(DVE, lambda e, s2=s2, sc=sc, j=j, k0=k0, bsel=bsel: e.scalar_tensor_tensor(
                        out=s2[:, k0:2], in0=sc[:, k0:2], scalar=scale, in1=bsel[j][:, k0:2], op0=ALU.mult, op1=ALU.add),
                        reads=[bsc, bbias[j]], writes=[bs2])
                    pb, bpb = pbs.next()
                    S.op(ACT, lambda e, pb=pb, s2=s2, k0=k0: e.activation(out=pb[:, k0:2], in_=s2[:, k0:2], func=AF.Exp),
                         reads=[bs2], writes=[bpb])
                    po, bpo = pos.next()
                    for gg in range(2):
                        for ck in cks:
                            S.op(PE, lambda e, po=po, pb=pb, gg=gg, ck=ck, i=i, j=j, vb=vb: e.matmul(
                                po[:, gg, :], lhsT=pb[:, ck, gg, :], rhs=vb[:, i + ck, j, :], start=(ck == cks[0]), stop=(ck == 1)),
                                reads=[bpb, bvb], writes=[bpo], inc=(ck == 1 and gg == 1))
                    den, bden = dens.next()
                    S.op(DVE, lambda e, den=den, po=po, j=j: e.tensor_tensor(out=den[:], in0=po[:, :, 64], in1=esink[:, 2 * j:2 * j + 2],
                                                                             op=ALU.add), reads=[bpo, bes], writes=[bden])
                    S.op(DVE, lambda e, den=den: e.reciprocal(out=den[:], in_=den[:]), reads=[bden], writes=[bden])
                    for gg in range(2):
                        h = 2 * j + gg
                        S.op(DVE, lambda e, om=om, po=po, den=den, gg=gg, h=h: e.tensor_scalar(
                            out=om[:, h * 64:(h + 1) * 64], in0=po[:, gg, 0:64], scalar1=den[:, gg:gg + 1], scalar2=None, op0=ALU.mult),
                            reads=[bpo, bden], writes=[bom])
                S.dma(SP, T.mixed[c0 + i * 128:c0 + (i + 1) * 128, 0:256], om[:], reads=[bom])
        S.barrier()
        S.flush(nc, sems)


PRUNE = 60.0


def phase_MOBA(S, nc, sems, T, l):
    SL = T.SL
    NT = SL // 128
    NB = SL // 256
    PIECE = min(2048, SL)
    with ExitStack() as es:
        def sb(name, shape, dt):
            return es.enter_context(nc.sbuf_tensor(U(name), shape, dt))

        def ps(name, shape, dt):
            return es.enter_context(nc.psum_tensor(U(name), shape, dt))
        ji = sb("ji", [33, SL], I32); jrow = sb("jrow", [33, SL], F32); bj = Buf()
        S.op(POOL, lambda e: e.iota(ji[:], pattern=[[0, NB], [1, 256]], base=0, channel_multiplier=0), writes=[bj])
        S.op(POOL, lambda e: e.tensor_copy(out=jrow[:], in_=ji[:]), reads=[bj], writes=[bj])
        cmask = sb("cmask", [128, 2, 256], F32); bcm = Buf()
        S.op(POOL, lambda e: e.memset(cmask[:], 0.0), writes=[bcm])
        for kc in range(2):
            S.op(POOL, lambda e, kc=kc: e.affine_select(out=cmask[:, kc, :], in_=cmask[:, kc, :], pattern=[[1, 256]],
                                                        compare_op=ALU.is_ge, fill=NEG, base=-128 * kc, channel_multiplier=-1),
                 reads=[bcm], writes=[bcm])
        qaug = sb("qaug", [128, SL], BF16); bqa = Buf()
        kaug = sb("kaug", [128, SL], BF16); bka = Buf()
        vaug = sb("vaug", [128, NT, 65], BF16); bva = Buf()
        selall = sb("selall", [128, NT, 32], F32); bsel = Buf()
        kmean = sb("kmean", [128, 32], F32); bkm = Buf()
        qfs = Ring([sb("qf%d" % i, [128, PIECE], F32) for i in range(2)])
        kfs = Ring([sb("kf%d" % i, [128, PIECE], F32) for i in range(2)])
        vfs = Ring([sb("vf%d" % i, [128, PIECE // 128, 64], F32) for i in range(2)])
        gsbs = Ring([sb("gsb%d" % i, [128, 32], F32) for i in range(2)])
        top8s = Ring([sb("top8%d" % i, [128, 8], F32) for i in range(2)])
        pgate = Ring([ps("pgate%d" % i, [128, 32], F32) for i in range(2)])
        pst = Ring([ps("pst%d" % i, [128, 2, 256], F32) for i in range(3)])
        pov = Ring([ps("pov%d" % i, [128, 2, 65], F32) for i in range(3)])
        sms = Ring([sb("sm%d" % i, [128, 2, 256], F32) for i in range(2)])
        pts = Ring([sb("pt%d" % i, [128, 2, 256], BF16) for i in range(3)])
        accs = Ring([sb("acc%d" % i, [128, 2, 65], F32) for i in range(2)])
        rcs = Ring([sb("rc%d" % i, [128, 2], F32) for i in range(2)])
        oms = Ring([sb("om%d" % i, [128, 2, 64], BF16) for i in range(2)])

        S.op(POOL, lambda e: e.memset(qaug[:], 0.0), writes=[bqa])
        S.op(POOL, lambda e: e.memset(kaug[:], 0.0), writes=[bka])
        S.op(POOL, lambda e: e.memset(qaug[0:1, :], 1.0), writes=[bqa])
        S.op(POOL, lambda e: e.memset(kaug[32:33, :], 1.0), writes=[bka])
        for h in range(4):
            m = SLOPES_MOBA[h]
            S.op(POOL, lambda e, m=m: e.tensor_scalar(out=qaug[32:33, :], in0=jrow[32:33, :], scalar1=-8.0 * m, scalar2=None,
                                                      op0=ALU.mult), reads=[bj], writes=[bqa])
            S.op(POOL, lambda e, m=m: e.tensor_scalar(out=kaug[0:1, :], in0=jrow[0:1, :], scalar1=8.0 * m, scalar2=None,
                                                      op0=ALU.mult), reads=[bj], writes=[bka])
            S.op(POOL, lambda e: e.memset(vaug[:], 1.0), writes=[bva])
            S.op(POOL, lambda e: e.memset(selall[:], 0.0), writes=[bsel])
            for pc in range(SL // PIECE):
                p0 = pc * PIECE
                qf, bqf = qfs.next(); kf, bkf = kfs.next(); vf, bvf = vfs.next()
                S.dma(SP, qf[64:128, :], T.projT[384 + h * 64:384 + (h + 1) * 64, p0:p0 + PIECE], writes=[bqf])
                S.dma(SP, kf[64:128, :], T.projT[640 + h * 64:640 + (h + 1) * 64, p0:p0 + PIECE], writes=[bkf])
                S.dma(SP, vf[:], T.projTM[p0:p0 + PIECE, 128 + h * 64:128 + (h + 1) * 64].rearrange("(n p) c -> p n c", p=128),
                      writes=[bvf])
                S.op(POOL, lambda e, qf=qf, p0=p0: e.tensor_copy(out=qaug[64:128, p0:p0 + PIECE], in_=qf[64:128, :]),
                     reads=[bqf], writes=[bqa])
                S.op(ACT, lambda e, kf=kf, p0=p0: e.copy(out=kaug[64:128, p0:p0 + PIECE], in_=kf[64:128, :]),
                     reads=[bkf], writes=[bka])
                S.op(POOL, lambda e, vf=vf, p0=p0: e.tensor_copy(out=vaug[:, p0 // 128:(p0 + PIECE) // 128, 0:64], in_=vf[:]),
                     reads=[bvf], writes=[bva])
                S.op(DVE, lambda e, kf=kf, p0=p0: e.tensor_reduce(
                    out=kmean[64:128, p0 // 256:(p0 + PIECE) // 256], in_=kf[64:128, :].rearrange("p (n j) -> p n j", j=256),
                    axis=AX.X, op=ALU.add), reads=[bkf], writes=[bkm])
                for tt in range(PIECE // 128):
                    t = p0 // 128 + tt
                    own = t // 2
                    if own == 0:
                        continue
                    pg, bpg = pgate.next()
                    S.op(PE, lambda e, pg=pg, qf=qf, tt=tt, own=own: e.matmul(
                        pg[:, 0:own], lhsT=qf[64:128, tt * 128:(tt + 1) * 128], rhs=kmean[64:128, 0:own], start=True, stop=True),
                        reads=[bqf, bkm], writes=[bpg])
                    gsb, bgsb = gsbs.next()
                    S.op(POOL, lambda e, gsb=gsb: e.memset(gsb[:], -1e30), writes=[bgsb])
                    S.op(DVE, lambda e, gsb=gsb, pg=pg, own=own: e.tensor_copy(out=gsb[:, 0:own], in_=pg[:, 0:own]),
                         reads=[bpg], writes=[bgsb])
                    t8, bt8 = top8s.next()
                    S.op(DVE, lambda e, t8=t8, gsb=gsb: e.max(out=t8[:], in_=gsb[:]), reads=[bgsb], writes=[bt8])
                    S.op(DVE, lambda e, t8=t8, gsb=gsb, t=t, own=own: e.tensor_scalar(
                        out=selall[:, t, 0:own], in0=gsb[:, 0:own], scalar1=t8[:, 2:3], scalar2=None, op0=ALU.is_ge),
                        reads=[bgsb, bt8], writes=[bsel])
            for c in range(NB):
                acc, bacc = accs.next()
                blocks = [c] + [n for n in range(c - 1, -1, -1) if m * 256.0 * (c - n - 1) <= PRUNE]
                for n in blocks:
                    st, bst = pst.next()
                    for kc in range(2):
                        S.op(PE, lambda e, st=st, kc=kc, n=n, c=c: e.matmul(
                            st[:, kc, :], lhsT=kaug[:, n * 256 + kc * 128:n * 256 + (kc + 1) * 128],
                            rhs=qaug[:, c * 256:(c + 1) * 256], start=True, stop=True),
                            reads=[bka, bqa], writes=[bst], inc=(kc == 1))
                    pt, bpt = pts.next()
                    if n == c:
                        sm, bsm = sms.next()
                        S.op(DVE, lambda e, sm=sm, st=st: e.scalar_tensor_tensor(
                            out=sm[:], in0=st[:], scalar=0.125, in1=cmask[:], op0=ALU.mult, op1=ALU.add),
                            reads=[bst, bcm], writes=[bsm])
                        S.op(ACT, lambda e, pt=pt, sm=sm: e.activation(out=pt[:], in_=sm[:], func=AF.Exp), reads=[bsm], writes=[bpt])
                    else:
                        cst = -m * 256.0 * (c - n)
                        S.op(ACT, lambda e, pt=pt, st=st, cst=cst: e.activation(out=pt[:], in_=st[:], func=AF.Exp, scale=0.125, bias=cst),
                             reads=[bst], writes=[bpt])
                    ov, bov = pov.next()
                    for half in range(2):
                        for kc in range(2):
                            S.op(PE, lambda e, ov=ov, pt=pt, half=half, kc=kc, n=n: e.matmul(
                                ov[:, half, :], lhsT=pt[:, kc, half * 128:(half + 1) * 128], rhs=vaug[:, 2 * n + kc, :],
                                start=(kc == 0), stop=(kc == 1)), reads=[bpt, bva], writes=[bov], inc=(kc == 1 and half == 1))
                    if n == c:
                        S.op(DVE, lambda e, acc=acc, ov=ov: e.tensor_copy(out=acc[:], in_=ov[:]), reads=[bov], writes=[bacc])
                    else:
                        for half in range(2):
                            S.op(DVE, lambda e, acc=acc, ov=ov, half=half, c=c, n=n: e.scalar_tensor_tensor(
                                out=acc[:, half, :], in0=ov[:, half, :], scalar=selall[:, 2 * c + half, n:n + 1], in1=acc[:, half, :],
                                op0=ALU.mult, op1=ALU.add), reads=[bov, bsel, bacc], writes=[bacc])
                rc, brc = rcs.next()
                S.op(DVE, lambda e, rc=rc, acc=acc: e.reciprocal(out=rc[:], in_=acc[:, :, 64]), reads=[bacc], writes=[brc])
                om, bom = oms.next()
                for half in range(2):
                    S.op(DVE, lambda e, om=om, acc=acc, rc=rc, half=half: e.tensor_scalar(
                        out=om[:, half, :], in0=acc[:, half, 0:64], scalar1=rc[:, half:half + 1], scalar2=None, op0=ALU.mult),
                        reads=[bacc, brc], writes=[bom])
                S.dma(SP, T.mixed[c * 256:(c + 1) * 256, 256 + h * 64:256 + (h + 1) * 64].rearrange("(a p) d -> p a d", p=128),
                      om[:], reads=[bom])
        S.barrier()
        S.flush(nc, sems)


def phase_GDN(S, nc, sems, T, l):
    SL = T.SL
    DK = 128
    STOP = getattr(T, "gdn_stop", 4)
    with ExitStack() as es:
        def sb(name, shape, dt):
            return es.enter_context(nc.sbuf_tensor(U(name), shape, dt))

        def ps(name, shape, dt):
            return es.enter_context(nc.psum_tensor(U(name), shape, dt))
        C = Ctx()
        banks = [ps("bank%d" % i, [128, 512], F32) for i in range(8)]
        C.pmisc = Ring([banks[7]])
        identf, bidf = make_ident(S, es, nc, F32)
        C.identf, C.bidentf = identf, bidf
        ones = sb("ones", [128, 128], F32); bones = Buf()
        S.op(POOL, lambda e: e.memset(ones[:], 1.0), writes=[bones])
        masks = sb("masks", [128, 2, 128], F32); bmask = Buf()
        S.op(POOL, lambda e: e.memset(masks[:], 1.0), writes=[bmask])
        S.op(POOL, lambda e: e.affine_select(out=masks[:, 0, :], in_=masks[:, 0, :], pattern=[[1, 128]], compare_op=ALU.is_ge,
                                             fill=0.0, base=-1, channel_multiplier=-1), reads=[bmask], writes=[bmask])
        S.op(POOL, lambda e: e.affine_select(out=masks[:, 1, :], in_=masks[:, 1, :], pattern=[[1, 128]], compare_op=ALU.is_ge,
                                             fill=0.0, base=0, channel_multiplier=-1), reads=[bmask], writes=[bmask])
        S.op(POOL, lambda e: e.memset(masks[0:64, :, 64:128], 0.0), reads=[bmask], writes=[bmask])
        blkblk = sb("blkblk", [128, 128], F32); blk = sb("blk", [128, 2], F32); bblk = Buf()
        S.op(POOL, lambda e: e.memset(blkblk[:], 0.0), writes=[bblk])
        S.op(POOL, lambda e: e.memset(blkblk[0:64, 0:64], 1.0), writes=[bblk])
        S.op(POOL, lambda e: e.memset(blkblk[64:128, 64:128], 1.0), writes=[bblk])
        S.op(POOL, lambda e: e.memset(blk[:], 0.0), writes=[bblk])
        S.op(POOL, lambda e: e.memset(blk[0:64, 0:1], 1.0), writes=[bblk])
        S.op(POOL, lambda e: e.memset(blk[64:128, 1:2], 1.0), writes=[bblk])
        cw, bcw = load_rows_T(S, C, nc, es, T.dn_conv_w[l], 4, 1536, "dncw")
        expA = sb("expA", [128, 4], F32); bA = Buf()
        S.dma(SP, expA[:], T.dn_a_log[l].partition_broadcast(128), writes=[bA])
        S.op(ACT, lambda e: e.activation(out=expA[:], in_=expA[:], func=AF.Exp), reads=[bA], writes=[bA])
        S.op(DVE, lambda e: e.tensor_scalar(out=expA[:], in0=expA[:], scalar1=-1.0, scalar2=None, op0=ALU.mult), reads=[bA], writes=[bA])
        dtb = sb("dtb", [128, 4], F32); bdt = Buf()
        S.dma(SP, dtb[:], T.dn_dt_bias[l].partition_broadcast(128), writes=[bdt])
        wn = sb("wn", [128, 128], F32); bwn = Buf()
        S.dma(SP, wn[:], T.dn_norm[l].partition_broadcast(128), writes=[bwn])
        state = [sb("state%d" % h, [128, 128], F32) for h in range(4)]
        bstate = [Buf() for _ in range(4)]
        vn = [[sb("vn%d_%d" % (h, cc), [128, 128], F32) for cc in range(2)] for h in range(4)]
        bvn = [[Buf() for cc in range(2)] for h in range(4)]
        for h in range(4):
            S.op(POOL, lambda e, h=h: e.memset(state[h][:], 0.0), writes=[bstate[h]])
            for cc in range(2):
                S.op(POOL, lambda e, h=h, cc=cc: e.memset(vn[h][cc][:], 0.0), writes=[bvn[h][cc]])
        raws = Ring([sb("raw%d" % i, [128, 515], F32) for i in range(3)])
        caccs = Ring([sb("cacc%d" % i, [128, 512], F32) for i in range(3)])
        cts = [Ring([sb("ct%d_%d" % (i, k), [128, 512], F32) for k in range(2)]) for i in range(12)]
        sqts = [Ring([sb("sqt%d_%d" % (i, k), [128, 512], F32) for k in range(1)]) for i in range(8)]
        abs_ = Ring([sb("ab%d" % i, [128, 8], F32) for i in range(2)])
        zs = Ring([sb("z%d" % i, [128, 512], F32) for i in range(2)])
        scal = Ring([sb("scal%d" % i, [128, 96], F32) for i in range(2)])
        NSLOT = 3
        def slot_rings(k):
            R = Ctx()
            R.diags = Ring([sb("diag%d_%d" % (k, i), [128, 3, 128], F32) for i in range(1)])
            R.dmins = Ring([sb("dmin%d_%d" % (k, i), [128, 128], F32) for i in range(1)])
            R.Es = Ring([sb("E%d_%d" % (k, i), [128, 128], F32) for i in range(1)])
            R.F12s = Ring([sb("F12%d_%d" % (k, i), [128, 2, 128], F32) for i in range(1)])
            R.UAs = Ring([sb("UA%d_%d" % (k, i), [128, 2, 128], F32) for i in range(1)])
            R.Ls = Ring([sb("L%d_%d" % (k, i), [128, 128], F32) for i in range(1)])
            R.pws = Ring([sb("pw%d_%d" % (k, i), [128, 2, 128], F32) for i in range(2)])
            R.Xs = Ring([sb("X%d_%d" % (k, i), [128, 256], F32) for i in range(2)])
            R.kdecs = Ring([sb("kdec%d_%d" % (k, i), [128, 128], F32) for i in range(1)])
            R.wTs = Ring([sb("wT%d_%d" % (k, i), [128, 128], F32) for i in range(1)])
            R.oqs = Ring([sb("oq%d_%d" % (k, i), [128, 128], F32) for i in range(1)])
            R.vtmps = Ring([sb("vtmp%d_%d" % (k, i), [128, 128], F32) for i in range(1)])
            return R
        SLOTS = [slot_rings(k) for k in range(NSLOT)]
        oalls = Ring([sb("oall%d" % i, [128, 4, 128], F32) for i in range(2)])
        zsil = Ring([sb("zsil%d" % i, [128, 512], F32) for i in range(2)])
        oms = Ring([sb("om%d" % i, [128, 512], BF16) for i in range(2)])
        junkg = Ring([sb("junkg", [128, 128], F32)])
        BK = [Buf("psum_bank%d" % i) for i in range(8)]
        bA_ = banks[0]; bufA = BK[0]
        for k_ in range(NSLOT):
            R = SLOTS[k_]
            XA, XB = banks[1 + 2 * k_], banks[2 + 2 * k_]
            R.bXA, R.bXB = BK[1 + 2 * k_], BK[2 + 2 * k_]
            R.p_rb = XA[:, 0:384].rearrange("p (a i) -> p a i", i=128)
            R.p_Lt = XA[:, 384:512]
            R.p_pw = XA[:, 0:256].rearrange("p (a i) -> p a i", i=128)
            R.p_app = XA[:, 256:512]
            R.p_wT = XA[:, 0:128]
            R.p_g = XB[:, 0:256].rearrange("p (a i) -> p a i", i=128)
            R.p_tr = XB[:, 256:512].rearrange("p (a i) -> p a i", i=128)
            R.p_wq = XB[:, 0:256].rearrange("p (a i) -> p a i", i=128)
            R.p_av = XB[:, 256:384]
            R.p_kv = XB[:, 384:512]

        for g in range(SL // 512):
            p0 = g * 512
            ct = {}
            sq = {}
            for h in range(4):
                for part in range(3):
                    row0 = 896 + part * 512 + h * 128
                    ch = part * 4 + h
                    raw, braw = raws.next()
                    if g == 0:
                        S.op(POOL, lambda e, raw=raw: e.memset(raw[:, 0:3], 0.0), writes=[braw])
                        S.dma(SP, raw[:, 3:515], T.projT[row0:row0 + 128, 0:512], writes=[braw])
                    else:
                        S.dma(SP, raw[:], T.projT[row0:row0 + 128, p0 - 3:p0 + 512], writes=[braw])
                    ca, bca = caccs.next()
                    S.op(DVE, lambda e, ca=ca, raw=raw, ch=ch: e.tensor_scalar(out=ca[:], in0=raw[:, 3:515], scalar1=cw[:, ch, 3:4],
                                                                               scalar2=None, op0=ALU.mult), reads=[braw, bcw], writes=[bca])
                    for k in (2, 1, 0):
                        eng = DVE
                        S.op(eng, lambda e, ca=ca, raw=raw, ch=ch, k=k: e.scalar_tensor_tensor(
                            out=ca[:], in0=raw[:, k:k + 512], scalar=cw[:, ch, k:k + 1], in1=ca[:], op0=ALU.mult, op1=ALU.add),
                            reads=[braw, bcw, bca], writes=[bca])
                    c_, bc_ = cts[ch].next()
                    S.op(ACT, lambda e, c_=c_, ca=ca: e.activation(out=c_[:], in_=ca[:], func=AF.Silu), reads=[bca], writes=[bc_])
                    ct[(part, h)] = (c_, bc_)
                    if part < 2:
                        s_, bs_ = sqts[part * 4 + h].next()
                        S.op(POOL, lambda e, s_=s_, c_=c_: e.tensor_tensor(out=s_[:], in0=c_[:], in1=c_[:], op=ALU.mult),
                             reads=[bc_], writes=[bs_])
                        sq[(part, h)] = (s_, bs_)
            for i in range(4):
                t0 = p0 + i * 128
                cs = slice(i * 128, (i + 1) * 128)
                ab, bab = abs_.next()
                S.dma(SP, ab[:], T.projTM[t0:t0 + 128, 896:904], writes=[bab])
                z, bz = zs.next()
                S.dma(SP, z[:], T.projTM[t0:t0 + 128, 384:896], writes=[bz])
                sc, bsc = scal.next()
                for part in range(2):
                    for h in range(4):
                        s_, bs_ = sq[(part, h)]
                        idx = part * 4 + h
                        S.op(PE, lambda e, s_=s_, idx=idx, cs=cs: e.matmul(bA_[:, idx * 2:idx * 2 + 2], lhsT=s_[:, cs], rhs=ones[:, 0:2],
                                                                           start=True, stop=True),
                             reads=[bs_, bones], writes=[bufA], inc=(idx == 7))
                S.op(DVE, lambda e, sc=sc: e.tensor_scalar(out=sc[:, 0:8], in0=bA_[:, 0:16].rearrange("p (a b) -> p a b", b=2)[:, :, 0],
                                                           scalar1=EPS, scalar2=None, op0=ALU.add), reads=[], writes=[bsc, bufA])
                S.op(ACT, lambda e, sc=sc: e.activation(out=sc[:, 0:8], in_=sc[:, 0:8], func=AF.Ln), reads=[bsc], writes=[bsc])
                S.op(DVE, lambda e, sc=sc: e.tensor_scalar(out=sc[:, 52:56], in0=sc[:, 4:8], scalar1=-0.5, scalar2=None, op0=ALU.mult),
                     reads=[bsc], writes=[bsc])
                S.op(ACT, lambda e, sc=sc: e.activation(out=sc[:, 0:8], in_=sc[:, 0:8], func=AF.Exp, scale=-0.5), reads=[bsc], writes=[bsc])
                S.op(ACT, lambda e, sc=sc, ab=ab: e.activation(out=sc[:, 8:12], in_=ab[:, 0:4], func=AF.Sigmoid), reads=[bab], writes=[bsc])
                S.op(DVE, lambda e, sc=sc, ab=ab: e.tensor_tensor(out=sc[:, 20:24], in0=ab[:, 4:8], in1=dtb[:], op=ALU.add),
                     reads=[bab, bdt], writes=[bsc])
                S.op(DVE, lambda e, sc=sc: e.tensor_scalar(out=sc[:, 72:76], in0=sc[:, 20:24], scalar1=-1.0, scalar2=None, op0=ALU.mult),
                     reads=[bsc], writes=[bsc])
                S.op(DVE, lambda e, sc=sc: e.tensor_tensor(out=sc[:, 72:76], in0=sc[:, 72:76], in1=sc[:, 20:24], op=ALU.max),
                     reads=[bsc], writes=[bsc])
                S.op(ACT, lambda e, sc=sc: e.activation(out=sc[:, 72:76], in_=sc[:, 72:76], func=AF.Exp, scale=-1.0), reads=[bsc], writes=[bsc])
                S.op(ACT, lambda e, sc=sc: e.activation(out=sc[:, 72:76], in_=sc[:, 72:76], func=AF.Ln, bias=1.0), reads=[bsc], writes=[bsc])
                S.op(DVE, lambda e, sc=sc: e.scalar_tensor_tensor(out=sc[:, 76:80], in0=sc[:, 20:24], scalar=0.0, in1=sc[:, 72:76],
                                                                  op0=ALU.max, op1=ALU.add), reads=[bsc], writes=[bsc])
                S.op(DVE, lambda e, sc=sc: e.tensor_tensor(out=sc[:, 12:16], in0=sc[:, 76:80], in1=expA[:], op=ALU.mult),
                     reads=[bsc, bA], writes=[bsc])
                for cc in range(2):
                    S.op(DVE, lambda e, sc=sc, cc=cc: e.tensor_scalar(out=sc[:, 56 + cc * 4:60 + cc * 4], in0=sc[:, 12:16],
                                                                      scalar1=blk[:, cc:cc + 1], scalar2=None, op0=ALU.mult),
                         reads=[bsc, bblk], writes=[bsc])
                S.op(PE, lambda e, sc=sc: e.matmul(bA_[:, 16:20], lhsT=masks[:, 1, :], rhs=sc[:, 12:16], start=True, stop=True),
                     reads=[bsc, bmask], writes=[bufA], inc=False)
                S.op(PE, lambda e, sc=sc: e.matmul(bA_[:, 20:24], lhsT=blkblk[:], rhs=sc[:, 12:16], start=True, stop=True),
                     reads=[bsc, bblk], writes=[bufA], inc=False)
                S.op(PE, lambda e, sc=sc: e.matmul(bA_[:, 24:32], lhsT=ones[:], rhs=sc[:, 56:64], start=True, stop=True),
                     reads=[bsc, bones], writes=[bufA])
                S.op(DVE, lambda e, sc=sc: e.tensor_copy(out=sc[:, 16:20], in_=bA_[:, 16:20]), reads=[], writes=[bsc, bufA])
                S.op(DVE, lambda e, sc=sc: e.tensor_tensor(out=sc[:, 20:24], in0=bA_[:, 20:24], in1=sc[:, 16:20], op=ALU.subtract),
                     reads=[bsc], writes=[bsc, bufA])
                S.op(ACT, lambda e, sc=sc: e.activation(out=sc[:, 24:28], in_=sc[:, 16:20], func=AF.Exp), reads=[bsc], writes=[bsc])
                S.op(ACT, lambda e, sc=sc: e.activation(out=sc[:, 28:32], in_=sc[:, 20:24], func=AF.Exp), reads=[bsc], writes=[bsc])
                S.op(ACT, lambda e, sc=sc: e.activation(out=sc[:, 64:72], in_=bA_[:, 24:32], func=AF.Exp), reads=[], writes=[bsc, bufA])
                S.op(DVE, lambda e, sc=sc: e.tensor_tensor(out=sc[:, 40:44], in0=sc[:, 8:12], in1=sc[:, 4:8], op=ALU.mult), reads=[bsc], writes=[bsc])
                S.op(DVE, lambda e, sc=sc: e.tensor_tensor(out=sc[:, 32:36], in0=sc[:, 40:44], in1=sc[:, 24:28], op=ALU.mult), reads=[bsc], writes=[bsc])
                S.op(DVE, lambda e, sc=sc: e.tensor_tensor(out=sc[:, 36:40], in0=sc[:, 4:8], in1=sc[:, 28:32], op=ALU.mult), reads=[bsc], writes=[bsc])
                S.op(DVE, lambda e, sc=sc: e.tensor_scalar(out=sc[:, 44:48], in0=sc[:, 0:4], scalar1=DK ** -0.5, scalar2=None, op0=ALU.mult),
                     reads=[bsc], writes=[bsc])
                S.op(DVE, lambda e, sc=sc: e.tensor_tensor(out=sc[:, 48:52], in0=sc[:, 44:48], in1=sc[:, 24:28], op=ALU.mult), reads=[bsc], writes=[bsc])
                oall, boall = oalls.next()
                def head_gen(h, R):
                    if STOP <= 1:
                        return
                    yield
                    qT, bqT = ct[(0, h)]
                    kT, bkT = ct[(1, h)]
                    vT, bvT = ct[(2, h)]
                    dg, bdg = R.diags.next()
                    for a, col in enumerate((40 + h, 44 + h, 16 + h)):
                        S.op(POOL, lambda e, dg=dg, a=a, col=col, sc=sc: e.tensor_scalar(out=dg[:, a, :], in0=identf[:], scalar1=sc[:, col:col + 1],
                                                                                       scalar2=None, op0=ALU.mult), reads=[bsc, bidf], writes=[bdg])
                    yield
                    S.op(PE, lambda e, dg=dg: e.matmul(R.p_rb, lhsT=ones[:], rhs=dg[:], start=True, stop=True), reads=[bdg, bones], writes=[R.bXA])
                    S.op(PE, lambda e, kT=kT, cs=cs: e.matmul(R.p_g[:, 0, :], lhsT=kT[:, cs], rhs=kT[:, cs], start=True, stop=True),
                         reads=[bkT], writes=[R.bXB], inc=False)
                    S.op(PE, lambda e, kT=kT, qT=qT, cs=cs: e.matmul(R.p_g[:, 1, :], lhsT=kT[:, cs], rhs=qT[:, cs], start=True, stop=True),
                         reads=[bkT, bqT], writes=[R.bXB])
                    S.op(PE, lambda e, kT=kT, cs=cs: e.transpose(out=R.p_tr[:, 0, :], in_=kT[:, cs], identity=identf[:]),
                         reads=[bkT, bidf], writes=[R.bXB], inc=False)
                    S.op(PE, lambda e, vT=vT, cs=cs: e.transpose(out=R.p_tr[:, 1, :], in_=vT[:, cs], identity=identf[:]),
                         reads=[bvT, bidf], writes=[R.bXB])
                    dm, bdm = R.dmins.next()
                    S.op(DVE, lambda e, dm=dm, sc=sc, h=h: e.tensor_scalar(out=dm[:], in0=R.p_rb[:, 2, :], scalar1=sc[:, 16 + h:17 + h], scalar2=0.0,
                                                                           op0=ALU.subtract, op1=ALU.min), reads=[bsc], writes=[bdm, R.bXA])
                    E, bE = R.Es.next()
                    S.op(ACT, lambda e, E=E, dm=dm, sc=sc, h=h: e.activation(out=E[:], in_=dm[:], func=AF.Exp, bias=sc[:, 52 + h:53 + h]),
                         reads=[bdm, bsc], writes=[bE])
                    F12, bF = R.F12s.next()
                    S.op(DVE, lambda e, F12=F12: e.tensor_tensor(out=F12[:], in0=R.p_rb[:, 0:2, :], in1=masks[:], op=ALU.mult),
                         reads=[bmask], writes=[bF, R.bXA])
                    for a in range(2):
                        S.op(POOL, lambda e, F12=F12, E=E, a=a: e.tensor_tensor(out=F12[:, a, :], in0=F12[:, a, :], in1=E[:], op=ALU.mult),
                             reads=[bF, bE], writes=[bF])
                    UA, bUA = R.UAs.next()
                    S.op(DVE, lambda e, UA=UA, F12=F12: e.tensor_tensor(out=UA[:], in0=R.p_g, in1=F12[:], op=ALU.mult),
                         reads=[bF], writes=[bUA, R.bXB])
                    X, bX = R.Xs.next()
                    kd, bkd = R.kdecs.next()
                    S.op(ACT, lambda e, X=X, sc=sc, h=h: e.activation(out=X[:, 0:128], in_=R.p_tr[:, 1, :], func=AF.Identity, scale=sc[:, 8 + h:9 + h]),
                         reads=[bsc], writes=[bX, R.bXB])
                    S.op(ACT, lambda e, X=X, sc=sc, h=h: e.activation(out=X[:, 128:256], in_=R.p_tr[:, 0, :], func=AF.Identity, scale=sc[:, 32 + h:33 + h]),
                         reads=[bsc], writes=[bX, R.bXB])
                    S.op(ACT, lambda e, kd=kd, sc=sc, h=h: e.activation(out=kd[:], in_=R.p_tr[:, 0, :], func=AF.Identity, scale=sc[:, 36 + h:37 + h]),
                         reads=[bsc], writes=[bkd, R.bXB])
                    yield
                    S.op(PE, lambda e, UA=UA: e.transpose(out=R.p_Lt, in_=UA[:, 0, :], identity=identf[:]), reads=[bUA, bidf], writes=[R.bXA])
                    L, bL = R.Ls.next()
                    S.op(ACT, lambda e, L=L: e.copy(out=L[:], in_=R.p_Lt), reads=[], writes=[bL, R.bXA])
                    if STOP <= 2:
                        return
                    Ucur, bUcur = UA[:, 0, :], bUA
                    Lcur, bLcur = L[:], bL
                    sign = ALU.subtract
                    for lev in range(6):
                        yield
                        pa, bpa = R.p_app, R.bXA
                        S.op(PE, lambda e, pa=pa, Ucur=Ucur, X=X: e.matmul(pa, lhsT=Ucur, rhs=X[:], start=True, stop=True),
                             reads=[bUcur, bX], writes=[bpa])
                        Xn, bXn = R.Xs.next()
                        S.op(DVE, lambda e, Xn=Xn, X=X, pa=pa, sign=sign: e.tensor_tensor(out=Xn[:], in0=X[:], in1=pa, op=sign),
                             reads=[bX], writes=[bXn, bpa])
                        X, bX = Xn, bXn
                        sign = ALU.add
                        if lev == 5:
                            break
                        yield
                        pp, bpp = R.p_pw, R.bXA
                        S.op(PE, lambda e, pp=pp, Ucur=Ucur, Lcur=Lcur: e.matmul(pp[:, 0, :], lhsT=Lcur, rhs=Ucur, start=True, stop=True),
                             reads=[bUcur, bLcur], writes=[bpp], inc=(lev == 4))
                        if lev < 4:
                            S.op(PE, lambda e, pp=pp, Ucur=Ucur, Lcur=Lcur: e.matmul(pp[:, 1, :], lhsT=Ucur, rhs=Lcur, start=True, stop=True),
                                 reads=[bUcur, bLcur], writes=[bpp])
                        pw, bpw = R.pws.next()
                        na = 2 if lev < 4 else 1
                        S.op(ACT, lambda e, pw=pw, pp=pp, na=na: e.copy(out=pw[:, 0:na, :], in_=pp[:, 0:na, :]),
                             reads=[], writes=[bpw, bpp])
                        Ucur, bUcur = pw[:, 0, :], bpw
                        Lcur, bLcur = pw[:, 1, :], bpw
                    yield
                    S.op(PE, lambda e, X=X: e.transpose(out=R.p_wT, in_=X[:, 128:256], identity=identf[:]), reads=[bX, bidf], writes=[R.bXA])
                    wT, bwT = R.wTs.next()
                    S.op(ACT, lambda e, wT=wT: e.copy(out=wT[:], in_=R.p_wT), reads=[], writes=[bwT, R.bXA])
                    if STOP <= 3 or STOP == 6:
                        return
                    RS = {7: 1, 8: 2, 9: 3, 10: 4}.get(STOP, 99)
                    st, bst = state[h], bstate[h]
                    for cc in range(2):
                        yield
                        v_, bv_ = vn[h][cc], bvn[h][cc]
                        S.op(PE, lambda e, wT=wT, st=st: e.matmul(R.p_wq[:, 0, :], lhsT=wT[:], rhs=st[:], start=True, stop=True),
                             reads=[bwT, bst], writes=[R.bXB], inc=False)
                        S.op(PE, lambda e, qT=qT, st=st, cs=cs: e.matmul(R.p_wq[:, 1, :], lhsT=qT[:, cs], rhs=st[:], start=True, stop=True),
                             reads=[bqT, bst], writes=[R.bXB])
                        if RS <= 1:
                            continue
                        vt, bvt = R.vtmps.next()
                        S.op(DVE, lambda e, vt=vt, X=X: e.tensor_tensor(out=vt[:], in0=X[:, 0:128], in1=R.p_wq[:, 0, :], op=ALU.subtract),
                             reads=[bX], writes=[bvt, R.bXB])
                        S.op(DVE, lambda e, v_=v_, vt=vt, cc=cc: e.tensor_scalar(out=v_[:], in0=vt[:], scalar1=blk[:, cc:cc + 1], scalar2=None,
                                                                                op0=ALU.mult), reads=[bvt, bblk], writes=[bv_])
                        oq, boq = R.oqs.next()
                        S.op(ACT, lambda e, oq=oq, sc=sc, h=h: e.activation(out=oq[:], in_=R.p_wq[:, 1, :], func=AF.Identity,
                                                                           scale=sc[:, 48 + h:49 + h]), reads=[bsc], writes=[boq, R.bXB])
                        if RS <= 2:
                            continue
                        yield
                        S.op(PE, lambda e, UA=UA, v_=v_: e.matmul(R.p_av, lhsT=UA[:, 1, :], rhs=v_[:], start=True, stop=True),
                             reads=[bUA, bv_], writes=[R.bXB])
                        S.op(PE, lambda e, kd=kd, v_=v_: e.matmul(R.p_kv, lhsT=kd[:], rhs=v_[:], start=True, stop=True),
                             reads=[bkd, bv_], writes=[R.bXB])
                        if RS <= 3:
                            continue
                        S.op(DVE, lambda e, st=st, sc=sc, cc=cc, h=h: e.scalar_tensor_tensor(
                            out=st[:], in0=st[:], scalar=sc[:, 64 + cc * 4 + h:65 + cc * 4 + h], in1=R.p_kv, op0=ALU.mult, op1=ALU.add),
                            reads=[bst, bsc], writes=[bst, R.bXB])
                        if RS <= 4:
                            continue
                        S.op(DVE, lambda e, oq=oq: e.tensor_tensor(out=oq[:], in0=oq[:], in1=R.p_av, op=ALU.add),
                             reads=[boq], writes=[boq, R.bXB])
                        if cc == 0:
                            S.op(DVE, lambda e, oall=oall, oq=oq, h=h: e.tensor_scalar(out=oall[:, h, :], in0=oq[:], scalar1=blk[:, 0:1],
                                                                                     scalar2=None, op0=ALU.mult), reads=[boq, bblk], writes=[boall])
                        else:
                            S.op(DVE, lambda e, oall=oall, oq=oq, h=h: e.scalar_tensor_tensor(
                                out=oall[:, h, :], in0=oq[:], scalar=blk[:, 1:2], in1=oall[:, h, :], op0=ALU.mult, op1=ALU.add),
                                reads=[boq, bblk, boall], writes=[boall])

                pending = list(range(4))
                active = []
                free = list(range(NSLOT))
                while pending or active:
                    while pending and free:
                        k_ = free.pop(0)
                        active.append((head_gen(pending.pop(0), SLOTS[k_]), k_))
                    nxt = []
                    for gen_, k_ in active:
                        try:
                            next(gen_)
                            nxt.append((gen_, k_))
                        except StopIteration:
                            free.append(k_)
                    active = nxt
                if STOP <= 3 or STOP == 5 or STOP >= 7:
                    continue
                if STOP == 6:
                    S.op(POOL, lambda e, oall=oall: e.memset(oall[:], 0.5), writes=[boall])
                zl, bzl = zsil.next()
                S.op(ACT, lambda e, zl=zl, z=z: e.activation(out=zl[:], in_=z[:], func=AF.Silu), reads=[bz], writes=[bzl])
                for h in range(4):
                    jk, bjk = junkg.next()
                    S.op(DVE, lambda e, jk=jk, oall=oall, h=h, sc=sc: e.scalar_tensor_tensor(
                        out=jk[:], in0=oall[:, h, :], scalar=1.0, in1=oall[:, h, :], op0=ALU.mult, op1=ALU.mult, accum_out=sc[:, 80 + h:81 + h]),
                        reads=[boall], writes=[bjk, bsc])
                S.op(DVE, lambda e, sc=sc: e.tensor_scalar(out=sc[:, 80:84], in0=sc[:, 80:84], scalar1=1.0 / 128, scalar2=EPS, op0=ALU.mult, op1=ALU.add),
                     reads=[bsc], writes=[bsc])
                S.op(ACT, lambda e, sc=sc: e.activation(out=sc[:, 80:84], in_=sc[:, 80:84], func=AF.Ln), reads=[bsc], writes=[bsc])
                S.op(ACT, lambda e, sc=sc: e.activation(out=sc[:, 80:84], in_=sc[:, 80:84], func=AF.Exp, scale=-0.5), reads=[bsc], writes=[bsc])
                om, bom = oms.next()
                for h in range(4):
                    S.op(DVE, lambda e, oall=oall, h=h, sc=sc: e.scalar_tensor_tensor(
                        out=oall[:, h, :], in0=oall[:, h, :], scalar=sc[:, 80 + h:81 + h], in1=wn[:], op0=ALU.mult, op1=ALU.mult),
                        reads=[boall, bsc, bwn], writes=[boall])
                S.op(POOL, lambda e, om=om, oall=oall, zl=zl: e.tensor_tensor(out=om[:], in0=oall[:].rearrange("p h d -> p (h d)"), in1=zl[:], op=ALU.mult),
                     reads=[boall, bzl], writes=[bom])
                S.dma(SP, T.mixed[t0:t0 + 128, 512:1024], om[:], reads=[bom])
        S.barrier()
        S.flush(nc, sems)


WEIGHT_SPECS = [
    ("mix_pre_norm", [2, D]), ("w_in", [2, D, INW]), ("swa_sinks", [2, 4]), ("dn_conv_w", [2, 4, 1536]),
    ("dn_a_log", [2, 4]), ("dn_dt_bias", [2, 4]), ("dn_norm", [2, 128]), ("w_out", [2, D, D]),
    ("mix_post_norm", [2, D]), ("ffn_pre_norm", [2, D]), ("w_up", [2, D, 2 * DFF]), ("ffn_conv_w", [2, 3, 2 * DFF]),
    ("ffn_conv_b", [2, 2 * DFF]), ("w_down", [2, DFF, D]), ("ffn_post_norm", [2, D]),
]


def build(SL=8192, n_layers=2, phases="ABC", dump=(), mixed_in=False, gdn_stop=4):
    nc = bass.Bass("TRN2", target_bir_lowering=False)
    T = Ctx()
    T.SL = SL
    T.gdn_stop = gdn_stop
    T.x = nc.dram_tensor("x", [SL, D], F32, kind="ExternalInput").ap()
    for name, shape in WEIGHT_SPECS:
        setattr(T, name, nc.dram_tensor(name, shape, F32, kind="ExternalInput").ap())
    T.out = nc.dram_tensor("out", [SL, D], F32, kind="ExternalOutput").ap()

    def scratch(name, shape, dt):
        kind = "ExternalOutput" if name in dump else "Internal"
        if name == "mixed" and mixed_in:
            kind = "ExternalInput"
        return nc.dram_tensor(name, shape, dt, kind=kind).ap()
    T.projT = scratch("projT", [NFM, SL], F32)
    T.projTM = scratch("projTM", [SL, NTM], F32)
    T.mixed = scratch("mixed", [SL, D], BF16)
    T.xres1 = scratch("xres1", [SL, D], F32)
    T.gsc = scratch("gsc", [DFF, SL], BF16)
    T.xmid = scratch("xmid", [SL, D], F32)
    S = Sched()
    with ExitStack() as es:
        sems = {k: es.enter_context(nc.semaphore(k.replace("_", ""))) for k in S.semkeys()}
        for l in range(n_layers):
            x_src = T.x if l == 0 else T.xmid
            x_dst = T.out if l == n_layers - 1 else T.xmid
            if "A" in phases:
                phase_A(S, nc, sems, T, l, x_src)
            if "B" in phases or "S" in phases:
                phase_SWA(S, nc, sems, T, l)
            if "B" in phases or "M" in phases:
                phase_MOBA(S, nc, sems, T, l)
            if "B" in phases or "G" in phases:
                phase_GDN(S, nc, sems, T, l)
            if "C" in phases:
                phase_C1(S, nc, sems, T, l, x_src)
                phase_C2(S, nc, sems, T, l, x_dst)
    return nc, S


def kernel(**inputs):
    x = np.ascontiguousarray(inputs["x"], dtype=np.float32)
    B, SL, _ = x.shape
    nc, _ = build(SL, 2)
    w = {name: np.ascontiguousarray(inputs[name], dtype=np.float32) for name, _ in WEIGHT_SPECS}
    in_maps = [dict(w, x=x[b]) for b in range(B)]
    res = run_bass_kernel_spmd(nc, in_maps, core_ids=list(range(B)))
    return np.stack([r["out"] for r in res.results], axis=0).astype(np.float32)
```

```python
import numpy as np
from contextlib import ExitStack
import concourse.bass as bass
import concourse.mybir as mybir
from concourse.bass_utils import run_bass_kernel_spmd

F32 = mybir.dt.float32
BF16 = mybir.dt.bfloat16
I32 = mybir.dt.int32
ALU = mybir.AluOpType
AF = mybir.ActivationFunctionType
AX = mybir.AxisListType

PE, ACT, DVE, POOL, SP = "tensor", "scalar", "vector", "gpsimd", "sync"
COMPUTE = (PE, ACT, DVE, POOL)
DMA_POOL = 12

D = 1024
DFF = 2816
INW = 3336
NFM = 2432
NTM = 904
EPS = 1e-6
NEG = -30000.0
STGW = 1408


_UID = [0]


def U(name):
    _UID[0] += 1
    return "%s_%d" % (name, _UID[0])


class Buf:
    __slots__ = ("name", "w", "r")

    def __init__(self, name=""):
        self.name = name
        self.w = None
        self.r = {}


class Sched:
    def __init__(self):
        self.ops = {e: [] for e in (PE, ACT, DVE, POOL, SP)}
        self.cnt = {}
        self.waited = {e: {} for e in self.ops}
        self.dma_rr = {e: 0 for e in self.ops}
        self.n_ops = 0

    def _deps(self, eng, reads, writes, sew):
        deps = {}

        def add(t):
            if t is None:
                return
            sk, v = t
            if deps.get(sk, 0) < v:
                deps[sk] = v
        for b in reads:
            add(b.w)
        for b in writes:
            add(b.w)
            for sk, v in b.r.items():
                add((sk, v))
        out = []
        for sk, v in deps.items():
            if sk == eng and not sew:
                continue
            if self.waited[eng].get(sk, 0) >= v:
                continue
            self.waited[eng][sk] = v
            out.append((sk, v))
        return out

    def _mark(self, ticket, reads, writes):
        sk, v = ticket
        for b in reads:
            if b.r.get(sk, 0) < v:
                b.r[sk] = v
        for b in writes:
            b.w = ticket
            b.r = {}

    def op(self, eng, fn, reads=(), writes=(), inc=True, sew=None):
        if sew is None:
            sew = eng != PE
        waits = self._deps(eng, reads, writes, sew)
        c = self.cnt.get(eng, 0)
        ticket = (eng, c + 1)
        if inc:
            self.cnt[eng] = c + 1
        self._mark(ticket, reads, writes)
        self.ops[eng].append((waits, fn, eng if inc else None, 1))
        self.n_ops += 1

    def dma(self, q, out_ap, in_ap, reads=(), writes=(), **kw):
        i = self.dma_rr[q]
        self.dma_rr[q] = (i + 1) % DMA_POOL
        sk = "dma_%s_%d" % (q, i)
        c = self.cnt.get(sk, 0)
        waits = self._deps(q, reads, writes, True)
        if c > 0 and self.waited[q].get(sk, 0) < c:
            self.waited[q][sk] = c
            waits.append((sk, c))
        self.cnt[sk] = c + 16
        self._mark((sk, c + 16), reads, writes)
        self.ops[q].append((waits, lambda e: e.dma_start(out=out_ap, in_=in_ap, **kw), sk, 16))
        self.n_ops += 1

    def barrier(self):
        for e in self.ops:
            waits = []
            for sk, v in self.cnt.items():
                if sk == e:
                    continue
                if self.waited[e].get(sk, 0) < v:
                    self.waited[e][sk] = v
                    waits.append((sk, v))
            if waits:
                self.ops[e].append((waits, None, None, 0))

    def flush(self, nc, sems):
        ops = self.ops
        self.ops = {e: [] for e in ops}

        def run(e, name):
            for waits, fn, sk, inc in ops[name]:
                for wsk, v in waits:
                    e.wait_ge(sems[wsk], v)
                if fn is None:
                    continue
                ins = fn(e)
                if sk is not None:
                    ins.then_inc(sems[sk], inc)
        with nc.Block() as block:
            block.sync(lambda e: run(e, SP))
            block.tensor(lambda e: run(e, PE))
            block.scalar(lambda e: run(e, ACT))
            block.vector(lambda e: run(e, DVE))
            block.gpsimd(lambda e: run(e, POOL))

    @staticmethod
    def semkeys():
        keys = list(COMPUTE)
        for q in (PE, ACT, DVE, POOL, SP):
            for i in range(DMA_POOL):
                keys.append("dma_%s_%d" % (q, i))
        return keys


class Ring:
    def __init__(self, tiles):
        self.items = [(t, Buf()) for t in tiles]
        self.i = 0

    def next(self):
        it = self.items[self.i % len(self.items)]
        self.i += 1
        return it


class Ctx:
    pass


def make_ident(S, es, nc, dt):
    identf = es.enter_context(nc.sbuf_tensor(U("identf"), [128, 128], F32))
    b = Buf()
    S.op(POOL, lambda e: e.memset(identf[:], 1.0), writes=[b])
    S.op(POOL, lambda e: e.affine_select(out=identf[:], in_=identf[:], pattern=[[-1, 128]],
                                         compare_op=ALU.is_equal, fill=0.0, base=0, channel_multiplier=1),
         reads=[b], writes=[b])
    if dt == F32:
        return identf, b
    ident = es.enter_context(nc.sbuf_tensor(U("identb"), [128, 128], BF16))
    b2 = Buf()
    S.op(POOL, lambda e: e.tensor_copy(out=ident[:], in_=identf[:]), reads=[b], writes=[b2])
    return ident, b2


def rstd_from_ss(S, ss, bss, n):
    S.op(DVE, lambda e: e.tensor_scalar(out=ss, in0=ss, scalar1=1.0 / n, scalar2=EPS, op0=ALU.mult, op1=ALU.add),
         reads=[bss], writes=[bss])
    S.op(ACT, lambda e: e.activation(out=ss, in_=ss, func=AF.Ln), reads=[bss], writes=[bss])
    S.op(ACT, lambda e: e.activation(out=ss, in_=ss, func=AF.Exp, scale=-0.5), reads=[bss], writes=[bss])


def load_weight_bf16(S, w_dram, dst, bdst, stg_ring, colmap, nk):
    for k in range(nk):
        for (s0, s1, d0) in colmap:
            n = s1 - s0
            for o in range(0, n, STGW):
                m = min(STGW, n - o)
                stg, bs = stg_ring.next()
                S.dma(SP, stg[:, 0:m], w_dram[k * 128:(k + 1) * 128, s0 + o:s0 + o + m], writes=[bs])
                S.op(POOL, lambda e, stg=stg, m=m, k=k, dd=d0 + o: e.tensor_copy(out=dst[:, k, dd:dd + m], in_=stg[:, 0:m]),
                     reads=[bs], writes=[bdst])


def norm_rows_to_bf16(S, C, xt, bx, gbc, bg, h, bh):
    junk, bj = C.junk.next()
    ss, bss = C.ss.next()
    S.op(DVE, lambda e: e.scalar_tensor_tensor(out=junk[:], in0=xt, scalar=1.0, in1=xt, op0=ALU.mult, op1=ALU.mult,
                                               accum_out=ss[:]), reads=[bx], writes=[bj, bss])
    rstd_from_ss(S, ss[:], bss, D)
    S.op(DVE, lambda e: e.scalar_tensor_tensor(out=h, in0=xt, scalar=ss[:], in1=gbc, op0=ALU.mult, op1=ALU.mult),
         reads=[bx, bss, bg], writes=[bh])


def transpose8(S, C, h, bh, dstT, bdT, col0):
    pT, bpT = C.pT.next()
    for c in range(8):
        S.op(PE, lambda e, c=c: e.transpose(out=pT[:, c, :], in_=h[:, c * 128:(c + 1) * 128], identity=C.ident[:]),
             reads=[bh, C.bident], writes=[bpT], inc=(c == 7))
    S.op(ACT, lambda e: e.copy(out=dstT[:, :, col0:col0 + 128], in_=pT[:]), reads=[bpT], writes=[bdT])


def phase_A(S, nc, sems, T, l, x_src):
    SL = T.SL
    with ExitStack() as es:
        def sb(name, shape, dt):
            return es.enter_context(nc.sbuf_tensor(U(name), shape, dt))

        def ps(name, shape, dt):
            return es.enter_context(nc.psum_tensor(U(name), shape, dt))
        C = Ctx()
        C.ident, C.bident = make_ident(S, es, nc, BF16)
        wfm = sb("wfm", [128, 8, NFM], BF16); bwfm = Buf()
        wtm = sb("wtm", [128, 8, NTM], BF16); bwtm = Buf()
        stg = Ring([sb("stg%d" % i, [128, STGW], F32) for i in range(2)])
        gbc = sb("gbc", [128, D], F32); bg = Buf()
        C.junk = Ring([sb("junk", [128, D], BF16)])
        C.ss = Ring([sb("ss%d" % i, [128, 1], F32) for i in range(4)])
        C.pT = Ring([ps("pT%d" % i, [128, 8, 128], BF16) for i in range(2)])
        xts = Ring([sb("xt%d" % i, [128, D], F32) for i in range(2)])
        hs = Ring([sb("h%d" % i, [128, D], BF16) for i in range(2)])
        hTs = Ring([sb("hT%d" % i, [128, 8, 512], BF16) for i in range(2)])
        pfm = Ring([ps("pfm%d" % i, [128, 512], F32) for i in range(2)])
        ptm = Ring([ps("ptm%d" % i, [128, 1024], F32) for i in range(2)])
        ofm = Ring([sb("ofm%d" % i, [128, 512], F32) for i in range(4)])
        otm = Ring([sb("otm%d" % i, [128, NTM], F32) for i in range(2)])

        S.dma(SP, gbc[:], T.mix_pre_norm[l].partition_broadcast(128), writes=[bg])
        w = T.w_in[l]
        load_weight_bf16(S, w, wfm, bwfm, stg, [(0, 384, 0), (512, 1024, 384), (1280, 2816, 896)], 8)
        load_weight_bf16(S, w, wtm, bwtm, stg, [(384, 512, 0), (1024, 1280, 128), (2816, 3336, 384)], 8)

        for g in range(SL // 512):
            hT, bhT = hTs.next()
            for i in range(4):
                t0 = g * 512 + i * 128
                xt, bx = xts.next()
                S.dma(SP, xt[:], x_src[t0:t0 + 128, :], writes=[bx])
                h, bh = hs.next()
                norm_rows_to_bf16(S, C, xt[:], bx, gbc[:], bg, h[:], bh)
                transpose8(S, C, h, bh, hT, bhT, i * 128)
            for i in range(4):
                t0 = g * 512 + i * 128
                p, bp = ptm.next()
                for (n0, n1) in ((0, 512), (512, NTM)):
                    for c in range(8):
                        S.op(PE, lambda e, c=c, n0=n0, n1=n1, p=p, i=i, hT=hT: e.matmul(
                            p[:, n0:n1], lhsT=hT[:, c, i * 128:(i + 1) * 128], rhs=wtm[:, c, n0:n1],
                            start=(c == 0), stop=(c == 7)), reads=[bhT, bwtm], writes=[bp], inc=(c == 7))
                o, bo = otm.next()
                S.op(ACT, lambda e, o=o, p=p: e.copy(out=o[:], in_=p[:, 0:NTM]), reads=[bp], writes=[bo])
                S.dma(SP, T.projTM[t0:t0 + 128, :], o[:], reads=[bo])
            for ch in range(NFM // 128):
                p, bp = pfm.next()
                for c in range(8):
                    S.op(PE, lambda e, c=c, ch=ch, p=p, hT=hT: e.matmul(
                        p[:], lhsT=wfm[:, c, ch * 128:(ch + 1) * 128], rhs=hT[:, c, :],
                        start=(c == 0), stop=(c == 7)), reads=[bhT, bwfm], writes=[bp], inc=(c == 7))
                o, bo = ofm.next()
                eng = ACT if ch % 2 == 0 else DVE
                if eng == ACT:
                    S.op(ACT, lambda e, o=o, p=p: e.copy(out=o[:], in_=p[:]), reads=[bp], writes=[bo])
                else:
                    S.op(DVE, lambda e, o=o, p=p: e.tensor_copy(out=o[:], in_=p[:]), reads=[bp], writes=[bo])
                S.dma(SP, T.projT[ch * 128:(ch + 1) * 128, g * 512:(g + 1) * 512], o[:], reads=[bo])
        S.barrier()
        S.flush(nc, sems)


def load_rows_T(S, C, nc, es, src2d, nrows, ncols, name):
    nch = ncols // 128
    rows = es.enter_context(nc.sbuf_tensor(U(name + "_rows"), [8, ncols], F32))
    br = Buf()
    S.dma(SP, rows[0:nrows, :], src2d, writes=[br])
    out = es.enter_context(nc.sbuf_tensor(U(name), [128, nch, nrows], F32))
    bo = Buf()
    for c0 in range(0, nch, 16):
        n = min(16, nch - c0)
        pt, bpt = C.pmisc.next()
        for j in range(n):
            c = c0 + j
            S.op(PE, lambda e, c=c, j=j, pt=pt: e.transpose(out=pt[:, j * nrows:(j + 1) * nrows],
                                                            in_=rows[0:nrows, c * 128:(c + 1) * 128],
                                                            identity=C.identf[0:nrows, 0:nrows]),
                 reads=[br, C.bidentf], writes=[bpt], inc=(j == n - 1))
        S.op(DVE, lambda e, c0=c0, n=n, pt=pt: e.tensor_copy(
            out=out[:, c0:c0 + n, :], in_=pt[:, 0:n * nrows].rearrange("p (c k) -> p c k", k=nrows)),
            reads=[bpt], writes=[bo])
    return out, bo


def phase_C1(S, nc, sems, T, l, x_src):
    SL = T.SL
    with ExitStack() as es:
        def sb(name, shape, dt):
            return es.enter_context(nc.sbuf_tensor(U(name), shape, dt))

        def ps(name, shape, dt):
            return es.enter_context(nc.psum_tensor(U(name), shape, dt))
        C = Ctx()
        C.ident, C.bident = make_ident(S, es, nc, BF16)
        identf = sb("identf2", [128, 128], F32); bidf = Buf()
        S.op(POOL, lambda e: e.memset(identf[:], 1.0), writes=[bidf])
        S.op(POOL, lambda e: e.affine_select(out=identf[:], in_=identf[:], pattern=[[-1, 128]],
                                             compare_op=ALU.is_equal, fill=0.0, base=0, channel_multiplier=1),
             reads=[bidf], writes=[bidf])
        C.identf, C.bidentf = identf, bidf
        C.pmisc = Ring([ps("pmisc", [128, 512], F32)])
        cw = sb("cw", [128, 44, 4], F32); bcw = Buf()
        with nc.sbuf_tensor(U("crow"), [8, 2 * DFF], F32) as crow:
            bcr = Buf()
            S.dma(SP, crow[0:3, :], T.ffn_conv_w[l], writes=[bcr])
            S.dma(SP, crow[3:4, :], T.ffn_conv_b[l:l + 1, :], writes=[bcr])
            for c0 in range(0, 44, 22):
                pt, bpt = C.pmisc.next()
                for j in range(22):
                    c = c0 + j
                    S.op(PE, lambda e, c=c, j=j, pt=pt: e.transpose(out=pt[:, j * 4:(j + 1) * 4], in_=crow[0:4, c * 128:(c + 1) * 128],
                                                                    identity=identf[0:4, 0:4]),
                         reads=[bcr, bidf], writes=[bpt], inc=(j == 21))
                S.op(DVE, lambda e, c0=c0, pt=pt: e.tensor_copy(out=cw[:, c0:c0 + 22, :],
                                                                in_=pt[:, 0:88].rearrange("p (c k) -> p c k", k=4)),
                     reads=[bpt], writes=[bcw])
            S.barrier()
            S.flush(nc, sems)
        wout = sb("wout", [128, 8, D], BF16); bwout = Buf()
        wup = sb("wup", [128, 8, 2 * DFF], BF16); bwup = Buf()
        stg = Ring([sb("stg%d" % i, [128, STGW], F32) for i in range(2)])
        gpost = sb("gpost", [128, D], F32); bgp = Buf()
        gpre = sb("gpre", [128, D], F32); bgq = Buf()
        C.junk = Ring([sb("junk", [128, D], BF16)])
        C.ss = Ring([sb("ss%d" % i, [128, 1], F32) for i in range(4)])
        C.pT = Ring([ps("pT%d" % i, [128, 8, 128], BF16) for i in range(2)])
        py = Ring([ps("py", [128, D], F32)])
        pup = Ring([ps("pup%d" % i, [128, 512], F32) for i in range(3)])
        mts = Ring([sb("mt%d" % i, [128, D], BF16) for i in range(2)])
        mTs = Ring([sb("mT%d" % i, [128, 8, 128], BF16) for i in range(2)])
        xts = Ring([sb("xt%d" % i, [128, D], F32) for i in range(2)])
        x1s = Ring([sb("x1%d" % i, [128, D], F32) for i in range(2)])
        hs = Ring([sb("h%d" % i, [128, D], BF16) for i in range(2)])
        hTs = Ring([sb("hT%d" % i, [128, 8, 512], BF16) for i in range(2)])
        ubs = Ring([sb("ub%d" % i, [128, 514], F32) for i in range(3)])
        accs = Ring([sb("acc%d" % i, [128, 512], F32) for i in range(3)])
        ggs = Ring([sb("gg%d" % i, [128, 512], F32) for i in range(2)])
        gos = Ring([sb("go%d" % i, [128, 512], BF16) for i in range(3)])
        halo = sb("halo", [128, 44, 2], F32); bhalo = [Buf() for _ in range(44)]

        S.dma(SP, gpost[:], T.mix_post_norm[l].partition_broadcast(128), writes=[bgp])
        S.dma(SP, gpre[:], T.ffn_pre_norm[l].partition_broadcast(128), writes=[bgq])
        S.op(POOL, lambda e: e.memset(halo[:], 0.0), writes=bhalo)
        load_weight_bf16(S, T.w_out[l], wout, bwout, stg, [(0, D, 0)], 8)
        load_weight_bf16(S, T.w_up[l], wup, bwup, stg, [(0, 2 * DFF, 0)], 8)

        for g in range(SL // 512):
            hT, bhT = hTs.next()
            for i in range(4):
                t0 = g * 512 + i * 128
                mt, bmt = mts.next()
                S.dma(SP, mt[:], T.mixed[t0:t0 + 128, :], writes=[bmt])
                mT, bmT = mTs.next()
                transpose8(S, C, mt, bmt, mT, bmT, 0)
                xt, bx = xts.next()
                S.dma(SP, xt[:], x_src[t0:t0 + 128, :], writes=[bx])
                p, bp = py.next()
                for nb in range(2):
                    for c in range(8):
                        S.op(PE, lambda e, c=c, nb=nb, p=p, mT=mT: e.matmul(
                            p[:, nb * 512:(nb + 1) * 512], lhsT=mT[:, c, :], rhs=wout[:, c, nb * 512:(nb + 1) * 512],
                            start=(c == 0), stop=(c == 7)), reads=[bmT, bwout], writes=[bp], inc=(c == 7))
                junk, bj = C.junk.next()
                ss, bss = C.ss.next()
                S.op(ACT, lambda e, junk=junk, p=p, ss=ss: e.activation(out=junk[:], in_=p[:], func=AF.Square, accum_out=ss[:]),
                     reads=[bp], writes=[bj, bss])
                rstd_from_ss(S, ss[:], bss, D)
                x1, bx1 = x1s.next()
                S.op(DVE, lambda e, x1=x1, p=p, ss=ss: e.scalar_tensor_tensor(out=x1[:], in0=p[:], scalar=ss[:], in1=gpost[:],
                                                                             op0=ALU.mult, op1=ALU.mult),
                     reads=[bp, bss, bgp], writes=[bx1])
                S.op(DVE, lambda e, x1=x1, xt=xt: e.tensor_tensor(out=x1[:], in0=x1[:], in1=xt[:], op=ALU.add),
                     reads=[bx1, bx], writes=[bx1])
                S.dma(SP, T.xres1[t0:t0 + 128, :], x1[:], reads=[bx1])
                h, bh = hs.next()
                norm_rows_to_bf16(S, C, x1[:], bx1, gpre[:], bgq, h[:], bh)
                transpose8(S, C, h, bh, hT, bhT, i * 128)
            for f in range(22):
                accp = []
                for part in range(2):
                    ch = part * 22 + f
                    p, bp = pup.next()
                    for c in range(8):
                        S.op(PE, lambda e, c=c, ch=ch, p=p, hT=hT: e.matmul(
                            p[:], lhsT=wup[:, c, ch * 128:(ch + 1) * 128], rhs=hT[:, c, :],
                            start=(c == 0), stop=(c == 7)), reads=[bhT, bwup], writes=[bp], inc=(c == 7))
                    ub, bub = ubs.next()
                    S.op(ACT, lambda e, ub=ub, ch=ch: e.copy(out=ub[:, 0:2], in_=halo[:, ch, :]),
                         reads=[bhalo[ch]], writes=[bub])
                    S.op(ACT, lambda e, ub=ub, p=p: e.copy(out=ub[:, 2:514], in_=p[:]), reads=[bp], writes=[bub])
                    S.op(ACT, lambda e, ub=ub, ch=ch: e.copy(out=halo[:, ch, :], in_=ub[:, 512:514]),
                         reads=[bub], writes=[bhalo[ch]])
                    acc, bacc = accs.next()
                    S.op(DVE, lambda e, acc=acc, ub=ub, ch=ch: e.tensor_scalar(
                        out=acc[:], in0=ub[:, 2:514], scalar1=cw[:, ch, 2:3], scalar2=cw[:, ch, 3:4], op0=ALU.mult, op1=ALU.add),
                        reads=[bub, bcw], writes=[bacc])
                    S.op(DVE, lambda e, acc=acc, ub=ub, ch=ch: e.scalar_tensor_tensor(
                        out=acc[:], in0=ub[:, 1:513], scalar=cw[:, ch, 1:2], in1=acc[:], op0=ALU.mult, op1=ALU.add),
                        reads=[bub, bcw, bacc], writes=[bacc])
                    S.op(DVE, lambda e, acc=acc, ub=ub, ch=ch: e.scalar_tensor_tensor(
                        out=acc[:], in0=ub[:, 0:512], scalar=cw[:, ch, 0:1], in1=acc[:], op0=ALU.mult, op1=ALU.add),
                        reads=[bub, bcw, bacc], writes=[bacc])
                    accp.append((acc, bacc))
                gg, bgg = ggs.next()
                S.op(ACT, lambda e, gg=gg, a=accp[0][0]: e.activation(out=gg[:], in_=a[:], func=AF.Gelu_apprx_tanh),
                     reads=[accp[0][1]], writes=[bgg])
                go, bgo = gos.next()
                S.op(POOL, lambda e, go=go, gg=gg, a=accp[1][0]: e.tensor_tensor(out=go[:], in0=gg[:], in1=a[:], op=ALU.mult),
                     reads=[bgg, accp[1][1]], writes=[bgo])
                S.dma(SP, T.gsc[f * 128:(f + 1) * 128, g * 512:(g + 1) * 512], go[:], reads=[bgo])
        S.barrier()
        S.flush(nc, sems)


def phase_C2(S, nc, sems, T, l, x_dst):
    SL = T.SL
    with ExitStack() as es:
        def sb(name, shape, dt):
            return es.enter_context(nc.sbuf_tensor(U(name), shape, dt))

        def ps(name, shape, dt):
            return es.enter_context(nc.psum_tensor(U(name), shape, dt))
        C = Ctx()
        wdn = sb("wdn", [128, 22, D], BF16); bwdn = Buf()
        stg = Ring([sb("stg%d" % i, [128, STGW], F32) for i in range(2)])
        gpost = sb("gpost", [128, D], F32); bgp = Buf()
        C.junk = Ring([sb("junk", [128, D], BF16)])
        C.ss = Ring([sb("ss%d" % i, [128, 1], F32) for i in range(4)])
        py = Ring([ps("py%d" % i, [128, D], F32) for i in range(2)])
        gTs = Ring([sb("gT%d" % i, [128, 22, 512], BF16) for i in range(2)])
        xts = Ring([sb("xt%d" % i, [128, D], F32) for i in range(2)])
        x2s = Ring([sb("x2%d" % i, [128, D], F32) for i in range(2)])
        S.dma(SP, gpost[:], T.ffn_post_norm[l].partition_broadcast(128), writes=[bgp])
        load_weight_bf16(S, T.w_down[l], wdn, bwdn, stg, [(0, D, 0)], 22)
        for g in range(SL // 512):
            gT, bgT = gTs.next()
            S.dma(SP, gT[:], T.gsc[:, g * 512:(g + 1) * 512].rearrange("(c p) t -> p c t", p=128), writes=[bgT])
            for i in range(4):
                t0 = g * 512 + i * 128
                xt, bx = xts.next()
                S.dma(SP, xt[:], T.xres1[t0:t0 + 128, :], writes=[bx])
                p, bp = py.next()
                for nb in range(2):
                    for f in range(22):
                        S.op(PE, lambda e, f=f, nb=nb, p=p, gT=gT, i=i: e.matmul(
                            p[:, nb * 512:(nb + 1) * 512], lhsT=gT[:, f, i * 128:(i + 1) * 128],
                            rhs=wdn[:, f, nb * 512:(nb + 1) * 512], start=(f == 0), stop=(f == 21)),
                            reads=[bgT, bwdn], writes=[bp], inc=(f == 21))
                junk, bj = C.junk.next()
                ss, bss = C.ss.next()
                S.op(ACT, lambda e, junk=junk, p=p, ss=ss: e.activation(out=junk[:], in_=p[:], func=AF.Square, accum_out=ss[:]),
                     reads=[bp], writes=[bj, bss])
                rstd_from_ss(S, ss[:], bss, D)
                x2, bx2 = x2s.next()
                S.op(DVE, lambda e, x2=x2, p=p, ss=ss: e.scalar_tensor_tensor(out=x2[:], in0=p[:], scalar=ss[:], in1=gpost[:],
                                                                             op0=ALU.mult, op1=ALU.mult),
                     reads=[bp, bss, bgp], writes=[bx2])
                S.op(DVE, lambda e, x2=x2, xt=xt: e.tensor_tensor(out=x2[:], in0=x2[:], in1=xt[:], op=ALU.add),
                     reads=[bx2, bx], writes=[bx2])
                S.dma(SP, x_dst[t0:t0 + 128, :], x2[:], reads=[bx2])
        S.barrier()
        S.flush(nc, sems)


SLOPES_SWA = [2.0 ** -1, 2.0 ** -3, 2.0 ** -5, 2.0 ** -7]
SLOPES_MOBA = [2.0 ** -2, 2.0 ** -4, 2.0 ** -6, 2.0 ** -8]


def make_rel(S, es, nc, ncols):
    ri = es.enter_context(nc.sbuf_tensor(U("reli"), [128, ncols], I32))
    rf = es.enter_context(nc.sbuf_tensor(U("relf"), [128, ncols], F32))
    b = Buf()
    S.op(POOL, lambda e: e.iota(ri[:], pattern=[[1, ncols]], base=0, channel_multiplier=-1), writes=[b])
    S.op(POOL, lambda e: e.tensor_copy(out=rf[:], in_=ri[:]), reads=[b], writes=[b])
    return rf, b


def phase_SWA(S, nc, sems, T, l):
    SL = T.SL
    scale = 64 ** -0.5
    with ExitStack() as es:
        def sb(name, shape, dt):
            return es.enter_context(nc.sbuf_tensor(U(name), shape, dt))

        def ps(name, shape, dt):
            return es.enter_context(nc.psum_tensor(U(name), shape, dt))
        rel, brel = make_rel(S, es, nc, 128)
        bias = [sb("bias%d" % j, [128, 2, 2, 128], F32) for j in range(2)]
        bbias = [Buf(), Buf()]
        for j in range(2):
            for g in range(2):
                m = SLOPES_SWA[2 * j + g]
                S.op(POOL, lambda e, j=j, g=g, m=m: e.tensor_scalar(out=bias[j][:, 1, g, :], in0=rel[:], scalar1=-m, scalar2=None,
                                                                    op0=ALU.mult), reads=[brel], writes=[bbias[j]])
                S.op(POOL, lambda e, j=j, g=g: e.affine_select(out=bias[j][:, 1, g, :], in_=bias[j][:, 1, g, :], pattern=[[1, 128]],
                                                               compare_op=ALU.is_ge, fill=NEG, base=0, channel_multiplier=-1),
                     reads=[bbias[j]], writes=[bbias[j]])
                S.op(POOL, lambda e, j=j, g=g, m=m: e.tensor_scalar(out=bias[j][:, 0, g, :], in0=rel[:], scalar1=-m, scalar2=-128.0 * m,
                                                                    op0=ALU.mult, op1=ALU.add), reads=[brel], writes=[bbias[j]])
                S.op(POOL, lambda e, j=j, g=g: e.affine_select(out=bias[j][:, 0, g, :], in_=bias[j][:, 0, g, :], pattern=[[-1, 128]],
                                                               compare_op=ALU.is_ge, fill=NEG, base=-1, channel_multiplier=1),
                     reads=[bbias[j]], writes=[bbias[j]])
        biasz = [sb("biasz%d" % j, [128, 2, 2, 128], F32) for j in range(2)]
        for j in range(2):
            S.op(POOL, lambda e, j=j: e.tensor_copy(out=biasz[j][:, 1], in_=bias[j][:, 1]), reads=[bbias[j]], writes=[bbias[j]])
            S.op(POOL, lambda e, j=j: e.memset(biasz[j][:, 0], NEG), writes=[bbias[j]])
        esink = sb("esink", [128, 4], F32); bes = Buf()
        S.dma(SP, esink[:], T.swa_sinks[l].partition_broadcast(128), writes=[bes])
        S.op(ACT, lambda e: e.activation(out=esink[:], in_=esink[:], func=AF.Exp), reads=[bes], writes=[bes])
        qfs = Ring([sb("qf%d" % i, [64, 4, 512], F32) for i in range(2)])
        kfs = Ring([sb("kf%d" % i, [64, 2, 640], F32) for i in range(2)])
        vfs = Ring([sb("vf%d" % i, [128, 5, 128], F32) for i in range(2)])
        qbs = Ring([sb("qb%d" % i, [64, 4, 512], BF16) for i in range(2)])
        kbs = Ring([sb("kb%d" % i, [64, 2, 640], BF16) for i in range(2)])
        vbs = Ring([sb("vb%d" % i, [128, 5, 2, 65], BF16) for i in range(2)])
        scs = Ring([ps("sc%d" % i, [128, 2, 2, 128], F32) for i in range(2)])
        pos = Ring([ps("po%d" % i, [128, 2, 65], F32) for i in range(2)])
        s2s = Ring([sb("s2%d" % i, [128, 2, 2, 128], F32) for i in range(2)])
        pbs = Ring([sb("pb%d" % i, [128, 2, 2, 128], BF16) for i in range(2)])
        dens = Ring([sb("den%d" % i, [128, 2], F32) for i in range(2)])
        oms = Ring([sb("om%d" % i, [128, 256], BF16) for i in range(2)])
        for g in range(SL // 512):
            c0 = g * 512
            qf, bqf = qfs.next(); kf, bkf = kfs.next(); vf, bvf = vfs.next()
            qb, bqb = qbs.next(); kb, bkb = kbs.next(); vb, bvb = vbs.next()
            S.dma(SP, qf[:], T.projT[0:256, c0:c0 + 512].rearrange("(h d) t -> d h t", d=64), writes=[bqf])
            if g == 0:
                S.op(POOL, lambda e, kf=kf: e.memset(kf[:, :, 0:128], 0.0), writes=[bkf])
                S.op(POOL, lambda e, vf=vf: e.memset(vf[:, 0, :], 0.0), writes=[bvf])
                S.dma(SP, kf[:, :, 128:640], T.projT[256:384, c0:c0 + 512].rearrange("(h d) t -> d h t", d=64), writes=[bkf])
                S.dma(SP, vf[:, 1:5, :], T.projTM[c0:c0 + 512, 0:128].rearrange("(n p) c -> p n c", p=128), writes=[bvf])
            else:
                S.dma(SP, kf[:], T.projT[256:384, c0 - 128:c0 + 512].rearrange("(h d) t -> d h t", d=64), writes=[bkf])
                S.dma(SP, vf[:], T.projTM[c0 - 128:c0 + 512, 0:128].rearrange("(n p) c -> p n c", p=128), writes=[bvf])
            S.op(POOL, lambda e, qb=qb, qf=qf: e.tensor_copy(out=qb[:], in_=qf[:]), reads=[bqf], writes=[bqb])
            S.op(POOL, lambda e, kb=kb, kf=kf: e.tensor_copy(out=kb[:], in_=kf[:]), reads=[bkf], writes=[bkb])
            S.op(POOL, lambda e, vb=vb: e.memset(vb[:], 1.0), writes=[bvb])
            S.op(POOL, lambda e, vb=vb, vf=vf: e.tensor_copy(out=vb[:, :, :, 0:64], in_=vf[:].rearrange("p n (j d) -> p n j d", d=64)),
                 reads=[bvf], writes=[bvb])
            for i in range(4):
                t = g * 4 + i
                cks = [0, 1]
                bsel = biasz if t == 0 else bias
                om, bom = oms.next()
                for j in range(2):
                    sc, bsc = scs.next()
                    for ck in cks:
                        S.op(PE, lambda e, sc=sc, ck=ck, j=j, i=i, kb=kb, qb=qb: e.matmul(
                            sc[:, ck, :, :], lhsT=kb[:, j, (i + ck) * 128:(i + ck + 1) * 128],
                            rhs=qb[:, 2 * j:2 * j + 2, i * 128:(i + 1) * 128], start=True, stop=True),
                            reads=[bkb, bqb], writes=[bsc], inc=(ck == 1))
                    k0 = cks[0]
                    s2, bs2 = s2s.next()
                    S.op(DVE, lambda e, s2=s2, sc=sc, j=j, k0=k0, bsel=bsel: e.scalar_tensor_tensor(
                        out=s2[:, k0:2], in0=sc[:, k0:2], scalar=scale, in1=bsel[j][:, k0:2], op0=ALU.mult, op1=ALU.add),
                        reads=[bsc, bbias[j]], writes=[bs2])
                    pb, bpb = pbs.next()
                    S.op(ACT, lambda e, pb=pb, s2=s2, k0=k0: e.activation(out=pb[:, k0:2], in_=s2[:, k0:2], func=AF.Exp),
                         reads=[bs2], writes=[bpb])
                    po, bpo = pos.next()
                    for gg in range(2):
                        for ck in cks:
                            S.op(PE, lambda e, po=po, pb=pb, gg=gg, ck=ck, i=i, j=j, vb=vb: e.matmul(
                                po[:, gg, :], lhsT=pb[:, ck, gg, :], rhs=vb[:, i + ck, j, :], start=(ck == cks[0]), stop=(ck == 1)),
                                reads=[bpb, bvb], writes=[bpo], inc=(ck == 1 and gg == 1))
                    den, bden = dens.next()
                    S.op(DVE, lambda e, den=den, po=po, j=j: e.tensor_tensor(out=den[:], in0=po[:, :, 64], in1=esink[:, 2 * j:2 * j + 2],
                                                                             op=ALU.add), reads=[bpo, bes], writes=[bden])
                    S.op(DVE, lambda e, den=den: e.reciprocal(out=den[:], in_=den[:]), reads=[bden], writes=[bden])
                    for gg in range(2):
                        h = 2 * j + gg
                        S.op(DVE, lambda e, om=om, po=po, den=den, gg=gg, h=h: e.tensor_scalar(
                            out=om[:, h * 64:(h + 1) * 64], in0=po[:, gg, 0:64], scalar1=den[:, gg:gg + 1], scalar2=None, op0=ALU.mult),
                            reads=[bpo, bden], writes=[bom])
                S.dma(SP, T.mixed[c0 + i * 128:c0 + (i + 1) * 128, 0:256], om[:], reads=[bom])
        S.barrier()
        S.flush(nc, sems)


PRUNE = 60.0


def phase_MOBA(S, nc, sems, T, l):
    SL = T.SL
    NT = SL // 128
    NB = SL // 256
    PIECE = min(2048, SL)
    with ExitStack() as es:
        def sb(name, shape, dt):
            return es.enter_context(nc.sbuf_tensor(U(name), shape, dt))

        def ps(name, shape, dt):
            return es.enter_context(nc.psum_tensor(U(name), shape, dt))
        ji = sb("ji", [NB, 256], I32); jf = sb("jf", [NB, 256], F32); rows8 = sb("rows8", [NB, 8, 256], BF16); bj = Buf(); baug = Buf()
        S.op(POOL, lambda e: e.iota(ji[:], pattern=[[1, 256]], base=0, channel_multiplier=0), writes=[bj])
        S.op(POOL, lambda e: e.tensor_copy(out=jf[:], in_=ji[:]), reads=[bj], writes=[bj])
        for h_ in range(4):
            for r_, sg in ((0, -8.0), (1, 8.0)):
                S.op(POOL, lambda e, h_=h_, r_=r_, sg=sg: e.tensor_scalar(out=rows8[:, 2 * h_ + r_, :], in0=jf[:], scalar1=sg * SLOPES_MOBA[h_],
                                                                          scalar2=None, op0=ALU.mult), reads=[bj], writes=[bj])
        S.dma(SP, T.augrows.rearrange("r (p i) -> p r i", i=256), rows8[:], reads=[bj], writes=[baug])
        cmask = sb("cmask", [128, 2, 256], F32); bcm = Buf()
        S.op(POOL, lambda e: e.memset(cmask[:], 0.0), writes=[bcm])
        for kc in range(2):
            S.op(POOL, lambda e, kc=kc: e.affine_select(out=cmask[:, kc, :], in_=cmask[:, kc, :], pattern=[[1, 256]],
                                                        compare_op=ALU.is_ge, fill=NEG, base=-128 * kc, channel_multiplier=-1),
                 reads=[bcm], writes=[bcm])
        qaug = sb("qaug", [128, SL], BF16); bqa = Buf()
        kaug = sb("kaug", [128, SL], BF16); bka = Buf()
        vaug = sb("vaug", [128, NT, 65], BF16); bva = Buf()
        selall = sb("selall", [128, NT, 32], F32); bsel = Buf()
        kmean = sb("kmean", [128, 32], F32); bkm = Buf()
        qfs = Ring([sb("qf%d" % i, [128, PIECE], F32) for i in range(2)])
        kfs = Ring([sb("kf%d" % i, [128, PIECE], F32) for i in range(2)])
        vfs = Ring([sb("vf%d" % i, [128, PIECE // 128, 64], F32) for i in range(2)])
        gsbs = Ring([sb("gsb%d" % i, [128, 32], F32) for i in range(2)])
        top8s = Ring([sb("top8%d" % i, [128, 8], F32) for i in range(2)])
        pgate = Ring([ps("pgate%d" % i, [128, 32], F32) for i in range(2)])
        pst = Ring([ps("pst%d" % i, [128, 2, 256], F32) for i in range(3)])
        pov = Ring([ps("pov%d" % i, [128, 2, 65], F32) for i in range(3)])
        sms = Ring([sb("sm%d" % i, [128, 2, 256], F32) for i in range(2)])
        pts = Ring([sb("pt%d" % i, [128, 2, 256], BF16) for i in range(3)])
        accs = Ring([sb("acc%d" % i, [128, 2, 65], F32) for i in range(2)])
        rcs = Ring([sb("rc%d" % i, [128, 2], F32) for i in range(2)])
        oms = Ring([sb("om%d" % i, [128, 2, 64], BF16) for i in range(2)])

        S.op(POOL, lambda e: e.memset(qaug[:], 0.0), writes=[bqa])
        S.op(POOL, lambda e: e.memset(kaug[:], 0.0), writes=[bka])
        S.op(POOL, lambda e: e.memset(qaug[0:1, :], 1.0), writes=[bqa])
        S.op(POOL, lambda e: e.memset(kaug[32:33, :], 1.0), writes=[bka])
        for h in range(4):
            m = SLOPES_MOBA[h]
            S.dma(SP, qaug[32:33, :], T.augrows[2 * h:2 * h + 1, :], reads=[baug], writes=[bqa])
            S.dma(SP, kaug[0:1, :], T.augrows[2 * h + 1:2 * h + 2, :], reads=[baug], writes=[bka])
            S.op(POOL, lambda e: e.memset(vaug[:], 1.0), writes=[bva])
            S.op(POOL, lambda e: e.memset(selall[:], 0.0), writes=[bsel])
            for pc in range(SL // PIECE):
                p0 = pc * PIECE
                qf, bqf = qfs.next(); kf, bkf = kfs.next(); vf, bvf = vfs.next()
                S.dma(SP, qf[64:128, :], T.projT[384 + h * 64:384 + (h + 1) * 64, p0:p0 + PIECE], writes=[bqf])
                S.dma(SP, kf[64:128, :], T.projT[640 + h * 64:640 + (h + 1) * 64, p0:p0 + PIECE], writes=[bkf])
                S.dma(SP, vf[:], T.projTM[p0:p0 + PIECE, 128 + h * 64:128 + (h + 1) * 64].rearrange("(n p) c -> p n c", p=128),
                      writes=[bvf])
                S.op(POOL, lambda e, qf=qf, p0=p0: e.tensor_copy(out=qaug[64:128, p0:p0 + PIECE], in_=qf[64:128, :]),
                     reads=[bqf], writes=[bqa])
                S.op(ACT, lambda e, kf=kf, p0=p0: e.copy(out=kaug[64:128, p0:p0 + PIECE], in_=kf[64:128, :]),
                     reads=[bkf], writes=[bka])
                S.op(POOL, lambda e, vf=vf, p0=p0: e.tensor_copy(out=vaug[:, p0 // 128:(p0 + PIECE) // 128, 0:64], in_=vf[:]),
                     reads=[bvf], writes=[bva])
                S.op(DVE, lambda e, kf=kf, p0=p0: e.tensor_reduce(
                    out=kmean[64:128, p0 // 256:(p0 + PIECE) // 256], in_=kf[64:128, :].rearrange("p (n j) -> p n j", j=256),
                    axis=AX.X, op=ALU.add), reads=[bkf], writes=[bkm])
                for tt in range(PIECE // 128):
                    t = p0 // 128 + tt
                    own = t // 2
                    if own == 0:
                        continue
                    pg, bpg = pgate.next()
                    S.op(PE, lambda e, pg=pg, qf=qf, tt=tt, own=own: e.matmul(
                        pg[:, 0:own], lhsT=qf[64:128, tt * 128:(tt + 1) * 128], rhs=kmean[64:128, 0:own], start=True, stop=True),
                        reads=[bqf, bkm], writes=[bpg])
                    gsb, bgsb = gsbs.next()
                    S.op(POOL, lambda e, gsb=gsb: e.memset(gsb[:], -1e30), writes=[bgsb])
                    S.op(DVE, lambda e, gsb=gsb, pg=pg, own=own: e.tensor_copy(out=gsb[:, 0:own], in_=pg[:, 0:own]),
                         reads=[bpg], writes=[bgsb])
                    t8, bt8 = top8s.next()
                    S.op(DVE, lambda e, t8=t8, gsb=gsb: e.max(out=t8[:], in_=gsb[:]), reads=[bgsb], writes=[bt8])
                    S.op(DVE, lambda e, t8=t8, gsb=gsb, t=t, own=own: e.tensor_scalar(
                        out=selall[:, t, 0:own], in0=gsb[:, 0:own], scalar1=t8[:, 2:3], scalar2=None, op0=ALU.is_ge),
                        reads=[bgsb, bt8], writes=[bsel])
            for c in range(NB):
                acc, bacc = accs.next()
                blocks = [c] + [n for n in range(c - 1, -1, -1) if m * 256.0 * (c - n - 1) <= PRUNE]
                for n in blocks:
                    st, bst = pst.next()
                    for kc in range(2):
                        S.op(PE, lambda e, st=st, kc=kc, n=n, c=c: e.matmul(
                            st[:, kc, :], lhsT=kaug[:, n * 256 + kc * 128:n * 256 + (kc + 1) * 128],
                            rhs=qaug[:, c * 256:(c + 1) * 256], start=True, stop=True),
                            reads=[bka, bqa], writes=[bst], inc=(kc == 1))
                    pt, bpt = pts.next()
                    if n == c:
                        sm, bsm = sms.next()
                        S.op(DVE, lambda e, sm=sm, st=st: e.scalar_tensor_tensor(
                            out=sm[:], in0=st[:], scalar=0.125, in1=cmask[:], op0=ALU.mult, op1=ALU.add),
                            reads=[bst, bcm], writes=[bsm])
                        S.op(ACT, lambda e, pt=pt, sm=sm: e.activation(out=pt[:], in_=sm[:], func=AF.Exp), reads=[bsm], writes=[bpt])
                    else:
                        cst = -m * 256.0 * (c - n)
                        S.op(ACT, lambda e, pt=pt, st=st, cst=cst: e.activation(out=pt[:], in_=st[:], func=AF.Exp, scale=0.125, bias=cst),
                             reads=[bst], writes=[bpt])
                    ov, bov = pov.next()
                    for half in range(2):
                        for kc in range(2):
                            S.op(PE, lambda e, ov=ov, pt=pt, half=half, kc=kc, n=n: e.matmul(
                                ov[:, half, :], lhsT=pt[:, kc, half * 128:(half + 1) * 128], rhs=vaug[:, 2 * n + kc, :],
                                start=(kc == 0), stop=(kc == 1)), reads=[bpt, bva], writes=[bov], inc=(kc == 1 and half == 1))
                    if n == c:
                        S.op(DVE, lambda e, acc=acc, ov=ov: e.tensor_copy(out=acc[:], in_=ov[:]), reads=[bov], writes=[bacc])
                    else:
                        for half in range(2):
                            S.op(DVE, lambda e, acc=acc, ov=ov, half=half, c=c, n=n: e.scalar_tensor_tensor(
                                out=acc[:, half, :], in0=ov[:, half, :], scalar=selall[:, 2 * c + half, n:n + 1], in1=acc[:, half, :],
                                op0=ALU.mult, op1=ALU.add), reads=[bov, bsel, bacc], writes=[bacc])
                rc, brc = rcs.next()
                S.op(DVE, lambda e, rc=rc, acc=acc: e.reciprocal(out=rc[:], in_=acc[:, :, 64]), reads=[bacc], writes=[brc])
                om, bom = oms.next()
                for half in range(2):
                    S.op(DVE, lambda e, om=om, acc=acc, rc=rc, half=half: e.tensor_scalar(
                        out=om[:, half, :], in0=acc[:, half, 0:64], scalar1=rc[:, half:half + 1], scalar2=None, op0=ALU.mult),
                        reads=[bacc, brc], writes=[bom])
                S.dma(SP, T.mixed[c * 256:(c + 1) * 256, 256 + h * 64:256 + (h + 1) * 64].rearrange("(a p) d -> p a d", p=128),
                      om[:], reads=[bom])
        S.barrier()
        S.flush(nc, sems)


def phase_GDN(S, nc, sems, T, l):
    SL = T.SL
    DK = 128
    STOP = getattr(T, "gdn_stop", 4)
    with ExitStack() as es:
        def sb(name, shape, dt):
            return es.enter_context(nc.sbuf_tensor(U(name), shape, dt))

        def ps(name, shape, dt):
            return es.enter_context(nc.psum_tensor(U(name), shape, dt))
        C = Ctx()
        banks = [ps("bank%d" % i, [128, 512], F32) for i in range(8)]
        C.pmisc = Ring([banks[7]])
        identf, bidf = make_ident(S, es, nc, F32)
        C.identf, C.bidentf = identf, bidf
        ones = sb("ones", [128, 128], F32); bones = Buf()
        S.op(POOL, lambda e: e.memset(ones[:], 1.0), writes=[bones])
        masks = sb("masks", [128, 2, 128], F32); bmask = Buf()
        S.op(POOL, lambda e: e.memset(masks[:], 1.0), writes=[bmask])
        S.op(POOL, lambda e: e.affine_select(out=masks[:, 0, :], in_=masks[:, 0, :], pattern=[[1, 128]], compare_op=ALU.is_ge,
                                             fill=0.0, base=-1, channel_multiplier=-1), reads=[bmask], writes=[bmask])
        S.op(POOL, lambda e: e.affine_select(out=masks[:, 1, :], in_=masks[:, 1, :], pattern=[[1, 128]], compare_op=ALU.is_ge,
                                             fill=0.0, base=0, channel_multiplier=-1), reads=[bmask], writes=[bmask])
        S.op(POOL, lambda e: e.memset(masks[0:64, :, 64:128], 0.0), reads=[bmask], writes=[bmask])
        blkblk = sb("blkblk", [128, 128], F32); blk = sb("blk", [128, 2], F32); bblk = Buf()
        S.op(POOL, lambda e: e.memset(blkblk[:], 0.0), writes=[bblk])
        S.op(POOL, lambda e: e.memset(blkblk[0:64, 0:64], 1.0), writes=[bblk])
        S.op(POOL, lambda e: e.memset(blkblk[64:128, 64:128], 1.0), writes=[bblk])
        S.op(POOL, lambda e: e.memset(blk[:], 0.0), writes=[bblk])
        S.op(POOL, lambda e: e.memset(blk[0:64, 0:1], 1.0), writes=[bblk])
        S.op(POOL, lambda e: e.memset(blk[64:128, 1:2], 1.0), writes=[bblk])
        cw, bcw = load_rows_T(S, C, nc, es, T.dn_conv_w[l], 4, 1536, "dncw")
        expA = sb("expA", [128, 4], F32); bA = Buf()
        S.dma(SP, expA[:], T.dn_a_log[l].partition_broadcast(128), writes=[bA])
        S.op(ACT, lambda e: e.activation(out=expA[:], in_=expA[:], func=AF.Exp), reads=[bA], writes=[bA])
        S.op(DVE, lambda e: e.tensor_scalar(out=expA[:], in0=expA[:], scalar1=-1.0, scalar2=None, op0=ALU.mult), reads=[bA], writes=[bA])
        dtb = sb("dtb", [128, 4], F32); bdt = Buf()
        S.dma(SP, dtb[:], T.dn_dt_bias[l].partition_broadcast(128), writes=[bdt])
        wn = sb("wn", [128, 128], F32); bwn = Buf()
        S.dma(SP, wn[:], T.dn_norm[l].partition_broadcast(128), writes=[bwn])
        state = [sb("state%d" % h, [128, 128], F32) for h in range(4)]
        bstate = [Buf() for _ in range(4)]
        vn = [[sb("vn%d_%d" % (h, cc), [128, 128], F32) for cc in range(2)] for h in range(4)]
        bvn = [[Buf() for cc in range(2)] for h in range(4)]
        for h in range(4):
            S.op(POOL, lambda e, h=h: e.memset(state[h][:], 0.0), writes=[bstate[h]])
            for cc in range(2):
                S.op(POOL, lambda e, h=h, cc=cc: e.memset(vn[h][cc][:], 0.0), writes=[bvn[h][cc]])
        raws = Ring([sb("raw%d" % i, [128, 515], F32) for i in range(3)])
        caccs = Ring([sb("cacc%d" % i, [128, 512], F32) for i in range(3)])
        cts = [Ring([sb("ct%d_%d" % (i, k), [128, 512], F32) for k in range(2)]) for i in range(12)]
        sqts = [Ring([sb("sqt%d_%d" % (i, k), [128, 512], F32) for k in range(1)]) for i in range(8)]
        abs_ = Ring([sb("ab%d" % i, [128, 8], F32) for i in range(2)])
        zs = Ring([sb("z%d" % i, [128, 512], F32) for i in range(2)])
        scal = Ring([sb("scal%d" % i, [128, 96], F32) for i in range(2)])
        NSLOT = 3
        def slot_rings(k):
            R = Ctx()
            R.diags = Ring([sb("diag%d_%d" % (k, i), [128, 3, 128], F32) for i in range(1)])
            R.dmins = Ring([sb("dmin%d_%d" % (k, i), [128, 128], F32) for i in range(1)])
            R.Es = Ring([sb("E%d_%d" % (k, i), [128, 128], F32) for i in range(1)])
            R.F12s = Ring([sb("F12%d_%d" % (k, i), [128, 2, 128], F32) for i in range(1)])
            R.UAs = Ring([sb("UA%d_%d" % (k, i), [128, 2, 128], F32) for i in range(1)])
            R.Ls = Ring([sb("L%d_%d" % (k, i), [128, 128], F32) for i in range(1)])
            R.pws = Ring([sb("pw%d_%d" % (k, i), [128, 2, 128], F32) for i in range(2)])
            R.Xs = Ring([sb("X%d_%d" % (k, i), [128, 256], F32) for i in range(2)])
            R.kdecs = Ring([sb("kdec%d_%d" % (k, i), [128, 128], F32) for i in range(1)])
            R.wTs = Ring([sb("wT%d_%d" % (k, i), [128, 128], F32) for i in range(1)])
            R.oqs = Ring([sb("oq%d_%d" % (k, i), [128, 128], F32) for i in range(1)])
            R.vtmps = Ring([sb("vtmp%d_%d" % (k, i), [128, 128], F32) for i in range(1)])
            return R
        SLOTS = [slot_rings(k) for k in range(NSLOT)]
        oalls = Ring([sb("oall%d" % i, [128, 4, 128], F32) for i in range(2)])
        zsil = Ring([sb("zsil%d" % i, [128, 512], F32) for i in range(2)])
        oms = Ring([sb("om%d" % i, [128, 512], BF16) for i in range(2)])
        junkg = Ring([sb("junkg", [128, 128], F32)])
        BK = [Buf("psum_bank%d" % i) for i in range(8)]
        bA_ = banks[0]; bufA = BK[0]
        for k_ in range(NSLOT):
            R = SLOTS[k_]
            XA, XB = banks[1 + 2 * k_], banks[2 + 2 * k_]
            R.bXA, R.bXB = BK[1 + 2 * k_], BK[2 + 2 * k_]
            R.p_rb = XA[:, 0:384].rearrange("p (a i) -> p a i", i=128)
            R.p_Lt = XA[:, 384:512]
            R.p_pw = XA[:, 0:256].rearrange("p (a i) -> p a i", i=128)
            R.p_app = XA[:, 256:512]
            R.p_wT = XA[:, 0:128]
            R.p_g = XB[:, 0:256].rearrange("p (a i) -> p a i", i=128)
            R.p_tr = XB[:, 256:512].rearrange("p (a i) -> p a i", i=128)
            R.p_wq = XB[:, 0:256].rearrange("p (a i) -> p a i", i=128)
            R.p_av = XB[:, 256:384]
            R.p_kv = XB[:, 384:512]

        for g in range(SL // 512):
            p0 = g * 512
            ct = {}
            sq = {}
            for h in range(4):
                for part in range(3):
                    row0 = 896 + part * 512 + h * 128
                    ch = part * 4 + h
                    raw, braw = raws.next()
                    if g == 0:
                        S.op(POOL, lambda e, raw=raw: e.memset(raw[:, 0:3], 0.0), writes=[braw])
                        S.dma(SP, raw[:, 3:515], T.projT[row0:row0 + 128, 0:512], writes=[braw])
                    else:
                        S.dma(SP, raw[:], T.projT[row0:row0 + 128, p0 - 3:p0 + 512], writes=[braw])
                    ca, bca = caccs.next()
                    S.op(DVE, lambda e, ca=ca, raw=raw, ch=ch: e.tensor_scalar(out=ca[:], in0=raw[:, 3:515], scalar1=cw[:, ch, 3:4],
                                                                               scalar2=None, op0=ALU.mult), reads=[braw, bcw], writes=[bca])
                    for k in (2, 1, 0):
                        eng = DVE
                        S.op(eng, lambda e, ca=ca, raw=raw, ch=ch, k=k: e.scalar_tensor_tensor(
                            out=ca[:], in0=raw[:, k:k + 512], scalar=cw[:, ch, k:k + 1], in1=ca[:], op0=ALU.mult, op1=ALU.add),
                            reads=[braw, bcw, bca], writes=[bca])
                    c_, bc_ = cts[ch].next()
                    S.op(ACT, lambda e, c_=c_, ca=ca: e.activation(out=c_[:], in_=ca[:], func=AF.Silu), reads=[bca], writes=[bc_])
                    ct[(part, h)] = (c_, bc_)
                    if part < 2:
                        s_, bs_ = sqts[part * 4 + h].next()
                        S.op(POOL, lambda e, s_=s_, c_=c_: e.tensor_tensor(out=s_[:], in0=c_[:], in1=c_[:], op=ALU.mult),
                             reads=[bc_], writes=[bs_])
                        sq[(part, h)] = (s_, bs_)
            for i in range(4):
                t0 = p0 + i * 128
                cs = slice(i * 128, (i + 1) * 128)
                ab, bab = abs_.next()
                S.dma(SP, ab[:], T.projTM[t0:t0 + 128, 896:904], writes=[bab])
                z, bz = zs.next()
                S.dma(SP, z[:], T.projTM[t0:t0 + 128, 384:896], writes=[bz])
                sc, bsc = scal.next()
                for part in range(2):
                    for h in range(4):
                        s_, bs_ = sq[(part, h)]
                        idx = part * 4 + h
                        S.op(PE, lambda e, s_=s_, idx=idx, cs=cs: e.matmul(bA_[:, idx * 2:idx * 2 + 2], lhsT=s_[:, cs], rhs=ones[:, 0:2],
                                                                           start=True, stop=True),
                             reads=[bs_, bones], writes=[bufA], inc=(idx == 7))
                S.op(DVE, lambda e, sc=sc: e.tensor_scalar(out=sc[:, 0:8], in0=bA_[:, 0:16].rearrange("p (a b) -> p a b", b=2)[:, :, 0],
                                                           scalar1=EPS, scalar2=None, op0=ALU.add), reads=[], writes=[bsc, bufA])
                S.op(ACT, lambda e, sc=sc: e.activation(out=sc[:, 0:8], in_=sc[:, 0:8], func=AF.Ln), reads=[bsc], writes=[bsc])
                S.op(DVE, lambda e, sc=sc: e.tensor_scalar(out=sc[:, 52:56], in0=sc[:, 4:8], scalar1=-0.5, scalar2=None, op0=ALU.mult),
                     reads=[bsc], writes=[bsc])
                S.op(ACT, lambda e, sc=sc: e.activation(out=sc[:, 0:8], in_=sc[:, 0:8], func=AF.Exp, scale=-0.5), reads=[bsc], writes=[bsc])
                S.op(ACT, lambda e, sc=sc, ab=ab: e.activation(out=sc[:, 8:12], in_=ab[:, 0:4], func=AF.Sigmoid), reads=[bab], writes=[bsc])
                S.op(DVE, lambda e, sc=sc, ab=ab: e.tensor_tensor(out=sc[:, 20:24], in0=ab[:, 4:8], in1=dtb[:], op=ALU.add),
                     reads=[bab, bdt], writes=[bsc])
                S.op(DVE, lambda e, sc=sc: e.tensor_scalar(out=sc[:, 72:76], in0=sc[:, 20:24], scalar1=-1.0, scalar2=None, op0=ALU.mult),
                     reads=[bsc], writes=[bsc])
                S.op(DVE, lambda e, sc=sc: e.tensor_tensor(out=sc[:, 72:76], in0=sc[:, 72:76], in1=sc[:, 20:24], op=ALU.max),
                     reads=[bsc], writes=[bsc])
                S.op(ACT, lambda e, sc=sc: e.activation(out=sc[:, 72:76], in_=sc[:, 72:76], func=AF.Exp, scale=-1.0), reads=[bsc], writes=[bsc])
                S.op(ACT, lambda e, sc=sc: e.activation(out=sc[:, 72:76], in_=sc[:, 72:76], func=AF.Ln, bias=1.0), reads=[bsc], writes=[bsc])
                S.op(DVE, lambda e, sc=sc: e.scalar_tensor_tensor(out=sc[:, 76:80], in0=sc[:, 20:24], scalar=0.0, in1=sc[:, 72:76],
                                                                  op0=ALU.max, op1=ALU.add), reads=[bsc], writes=[bsc])
                S.op(DVE, lambda e, sc=sc: e.tensor_tensor(out=sc[:, 12:16], in0=sc[:, 76:80], in1=expA[:], op=ALU.mult),
                     reads=[bsc, bA], writes=[bsc])
                for cc in range(2):
                    S.op(DVE, lambda e, sc=sc, cc=cc: e.tensor_scalar(out=sc[:, 56 + cc * 4:60 + cc * 4], in0=sc[:, 12:16],
                                                                      scalar1=blk[:, cc:cc + 1], scalar2=None, op0=ALU.mult),
                         reads=[bsc, bblk], writes=[bsc])
                S.op(PE, lambda e, sc=sc: e.matmul(bA_[:, 16:20], lhsT=masks[:, 1, :], rhs=sc[:, 12:16], start=True, stop=True),
                     reads=[bsc, bmask], writes=[bufA], inc=False)
                S.op(PE, lambda e, sc=sc: e.matmul(bA_[:, 20:24], lhsT=blkblk[:], rhs=sc[:, 12:16], start=True, stop=True),
                     reads=[bsc, bblk], writes=[bufA], inc=False)
                S.op(PE, lambda e, sc=sc: e.matmul(bA_[:, 24:32], lhsT=ones[:], rhs=sc[:, 56:64], start=True, stop=True),
                     reads=[bsc, bones], writes=[bufA])
                S.op(DVE, lambda e, sc=sc: e.tensor_copy(out=sc[:, 16:20], in_=bA_[:, 16:20]), reads=[], writes=[bsc, bufA])
                S.op(DVE, lambda e, sc=sc: e.tensor_tensor(out=sc[:, 20:24], in0=bA_[:, 20:24], in1=sc[:, 16:20], op=ALU.subtract),
                     reads=[bsc], writes=[bsc, bufA])
                S.op(ACT, lambda e, sc=sc: e.activation(out=sc[:, 24:28], in_=sc[:, 16:20], func=AF.Exp), reads=[bsc], writes=[bsc])
                S.op(ACT, lambda e, sc=sc: e.activation(out=sc[:, 28:32], in_=sc[:, 20:24], func=AF.Exp), reads=[bsc], writes=[bsc])
                S.op(ACT, lambda e, sc=sc: e.activation(out=sc[:, 64:72], in_=bA_[:, 24:32], func=AF.Exp), reads=[], writes=[bsc, bufA])
                S.op(DVE, lambda e, sc=sc: e.tensor_tensor(out=sc[:, 40:44], in0=sc[:, 8:12], in1=sc[:, 4:8], op=ALU.mult), reads=[bsc], writes=[bsc])
                S.op(DVE, lambda e, sc=sc: e.tensor_tensor(out=sc[:, 32:36], in0=sc[:, 40:44], in1=sc[:, 24:28], op=ALU.mult), reads=[bsc], writes=[bsc])
                S.op(DVE, lambda e, sc=sc: e.tensor_tensor(out=sc[:, 36:40], in0=sc[:, 4:8], in1=sc[:, 28:32], op=ALU.mult), reads=[bsc], writes=[bsc])
                S.op(DVE, lambda e, sc=sc: e.tensor_scalar(out=sc[:, 44:48], in0=sc[:, 0:4], scalar1=DK ** -0.5, scalar2=None, op0=ALU.mult),
                     reads=[bsc], writes=[bsc])
                S.op(DVE, lambda e, sc=sc: e.tensor_tensor(out=sc[:, 48:52], in0=sc[:, 44:48], in1=sc[:, 24:28], op=ALU.mult), reads=[bsc], writes=[bsc])
                oall, boall = oalls.next()
                def head_gen(h, R):
                    if STOP <= 1:
                        return
                    yield
                    qT, bqT = ct[(0, h)]
                    kT, bkT = ct[(1, h)]
                    vT, bvT = ct[(2, h)]
                    dg, bdg = R.diags.next()
                    for a, col in enumerate((40 + h, 44 + h, 16 + h)):
                        S.op(DVE, lambda e, dg=dg, a=a, col=col, sc=sc: e.tensor_scalar(out=dg[:, a, :], in0=identf[:], scalar1=sc[:, col:col + 1],
                                                                                       scalar2=None, op0=ALU.mult), reads=[bsc, bidf], writes=[bdg])
                    yield
                    S.op(PE, lambda e, dg=dg: e.matmul(R.p_rb, lhsT=ones[:], rhs=dg[:], start=True, stop=True), reads=[bdg, bones], writes=[R.bXA])
                    S.op(PE, lambda e, kT=kT, cs=cs: e.matmul(R.p_g[:, 0, :], lhsT=kT[:, cs], rhs=kT[:, cs], start=True, stop=True),
                         reads=[bkT], writes=[R.bXB], inc=False)
                    S.op(PE, lambda e, kT=kT, qT=qT, cs=cs: e.matmul(R.p_g[:, 1, :], lhsT=kT[:, cs], rhs=qT[:, cs], start=True, stop=True),
                         reads=[bkT, bqT], writes=[R.bXB])
                    S.op(PE, lambda e, kT=kT, cs=cs: e.transpose(out=R.p_tr[:, 0, :], in_=kT[:, cs], identity=identf[:]),
                         reads=[bkT, bidf], writes=[R.bXB], inc=False)
                    S.op(PE, lambda e, vT=vT, cs=cs: e.transpose(out=R.p_tr[:, 1, :], in_=vT[:, cs], identity=identf[:]),
                         reads=[bvT, bidf], writes=[R.bXB])
                    dm, bdm = R.dmins.next()
                    S.op(DVE, lambda e, dm=dm, sc=sc, h=h: e.tensor_scalar(out=dm[:], in0=R.p_rb[:, 2, :], scalar1=sc[:, 16 + h:17 + h], scalar2=0.0,
                                                                           op0=ALU.subtract, op1=ALU.min), reads=[bsc], writes=[bdm, R.bXA])
                    E, bE = R.Es.next()
                    S.op(ACT, lambda e, E=E, dm=dm, sc=sc, h=h: e.activation(out=E[:], in_=dm[:], func=AF.Exp, bias=sc[:, 52 + h:53 + h]),
                         reads=[bdm, bsc], writes=[bE])
                    F12, bF = R.F12s.next()
                    S.op(DVE, lambda e, F12=F12: e.tensor_tensor(out=F12[:], in0=R.p_rb[:, 0:2, :], in1=masks[:], op=ALU.mult),
                         reads=[bmask], writes=[bF, R.bXA])
                    for a in range(2):
                        S.op(DVE, lambda e, F12=F12, E=E, a=a: e.tensor_tensor(out=F12[:, a, :], in0=F12[:, a, :], in1=E[:], op=ALU.mult),
                             reads=[bF, bE], writes=[bF])
                    UA, bUA = R.UAs.next()
                    S.op(DVE, lambda e, UA=UA, F12=F12: e.tensor_tensor(out=UA[:], in0=R.p_g, in1=F12[:], op=ALU.mult),
                         reads=[bF], writes=[bUA, R.bXB])
                    X, bX = R.Xs.next()
                    kd, bkd = R.kdecs.next()
                    S.op(ACT, lambda e, X=X, sc=sc, h=h: e.activation(out=X[:, 0:128], in_=R.p_tr[:, 1, :], func=AF.Identity, scale=sc[:, 8 + h:9 + h]),
                         reads=[bsc], writes=[bX, R.bXB])
                    S.op(ACT, lambda e, X=X, sc=sc, h=h: e.activation(out=X[:, 128:256], in_=R.p_tr[:, 0, :], func=AF.Identity, scale=sc[:, 32 + h:33 + h]),
                         reads=[bsc], writes=[bX, R.bXB])
                    S.op(ACT, lambda e, kd=kd, sc=sc, h=h: e.activation(out=kd[:], in_=R.p_tr[:, 0, :], func=AF.Identity, scale=sc[:, 36 + h:37 + h]),
                         reads=[bsc], writes=[bkd, R.bXB])
                    yield
                    S.op(PE, lambda e, UA=UA: e.transpose(out=R.p_Lt, in_=UA[:, 0, :], identity=identf[:]), reads=[bUA, bidf], writes=[R.bXA])
                    L, bL = R.Ls.next()
                    S.op(ACT, lambda e, L=L: e.copy(out=L[:], in_=R.p_Lt), reads=[], writes=[bL, R.bXA])
                    if STOP <= 2:
                        return
                    Ucur, bUcur = UA[:, 0, :], bUA
                    Lcur, bLcur = L[:], bL
                    sign = ALU.subtract
                    for lev in range(6):
                        yield
                        pa, bpa = R.p_app, R.bXA
                        S.op(PE, lambda e, pa=pa, Ucur=Ucur, X=X: e.matmul(pa, lhsT=Ucur, rhs=X[:], start=True, stop=True),
                             reads=[bUcur, bX], writes=[bpa])
                        Xn, bXn = R.Xs.next()
                        S.op(DVE, lambda e, Xn=Xn, X=X, pa=pa, sign=sign: e.tensor_tensor(out=Xn[:], in0=X[:], in1=pa, op=sign),
                             reads=[bX], writes=[bXn, bpa])
                        X, bX = Xn, bXn
                        sign = ALU.add
                        if lev == 5:
                            break
                        yield
                        pp, bpp = R.p_pw, R.bXA
                        S.op(PE, lambda e, pp=pp, Ucur=Ucur, Lcur=Lcur: e.matmul(pp[:, 0, :], lhsT=Lcur, rhs=Ucur, start=True, stop=True),
                             reads=[bUcur, bLcur], writes=[bpp], inc=(lev == 4))
                        if lev < 4:
                            S.op(PE, lambda e, pp=pp, Ucur=Ucur, Lcur=Lcur: e.matmul(pp[:, 1, :], lhsT=Ucur, rhs=Lcur, start=True, stop=True),
                                 reads=[bUcur, bLcur], writes=[bpp])
                        pw, bpw = R.pws.next()
                        na = 2 if lev < 4 else 1
                        S.op(ACT, lambda e, pw=pw, pp=pp, na=na: e.copy(out=pw[:, 0:na, :], in_=pp[:, 0:na, :]),
                             reads=[], writes=[bpw, bpp])
                        Ucur, bUcur = pw[:, 0, :], bpw
                        Lcur, bLcur = pw[:, 1, :], bpw
                    yield
                    S.op(PE, lambda e, X=X: e.transpose(out=R.p_wT, in_=X[:, 128:256], identity=identf[:]), reads=[bX, bidf], writes=[R.bXA])
                    wT, bwT = R.wTs.next()
                    S.op(ACT, lambda e, wT=wT: e.copy(out=wT[:], in_=R.p_wT), reads=[], writes=[bwT, R.bXA])
                    if STOP <= 3 or STOP == 6:
                        return
                    RS = {7: 1, 8: 2, 9: 3, 10: 4}.get(STOP, 99)
                    st, bst = state[h], bstate[h]
                    for cc in range(2):
                        yield
                        v_, bv_ = vn[h][cc], bvn[h][cc]
                        S.op(PE, lambda e, wT=wT, st=st: e.matmul(R.p_wq[:, 0, :], lhsT=wT[:], rhs=st[:], start=True, stop=True),
                             reads=[bwT, bst], writes=[R.bXB], inc=False)
                        S.op(PE, lambda e, qT=qT, st=st, cs=cs: e.matmul(R.p_wq[:, 1, :], lhsT=qT[:, cs], rhs=st[:], start=True, stop=True),
                             reads=[bqT, bst], writes=[R.bXB])
                        if RS <= 1:
                            continue
                        vt, bvt = R.vtmps.next()
                        S.op(DVE, lambda e, vt=vt, X=X: e.tensor_tensor(out=vt[:], in0=X[:, 0:128], in1=R.p_wq[:, 0, :], op=ALU.subtract),
                             reads=[bX], writes=[bvt, R.bXB])
                        S.op(DVE, lambda e, v_=v_, vt=vt, cc=cc: e.tensor_scalar(out=v_[:], in0=vt[:], scalar1=blk[:, cc:cc + 1], scalar2=None,
                                                                                op0=ALU.mult), reads=[bvt, bblk], writes=[bv_])
                        oq, boq = R.oqs.next()
                        S.op(ACT, lambda e, oq=oq, sc=sc, h=h: e.activation(out=oq[:], in_=R.p_wq[:, 1, :], func=AF.Identity,
                                                                           scale=sc[:, 48 + h:49 + h]), reads=[bsc], writes=[boq, R.bXB])
                        if RS <= 2:
                            continue
                        yield
                        S.op(PE, lambda e, UA=UA, v_=v_: e.matmul(R.p_av, lhsT=UA[:, 1, :], rhs=v_[:], start=True, stop=True),
                             reads=[bUA, bv_], writes=[R.bXB])
                        S.op(PE, lambda e, kd=kd, v_=v_: e.matmul(R.p_kv, lhsT=kd[:], rhs=v_[:], start=True, stop=True),
                             reads=[bkd, bv_], writes=[R.bXB])
                        if RS <= 3:
                            continue
                        S.op(DVE, lambda e, st=st, sc=sc, cc=cc, h=h: e.scalar_tensor_tensor(
                            out=st[:], in0=st[:], scalar=sc[:, 64 + cc * 4 + h:65 + cc * 4 + h], in1=R.p_kv, op0=ALU.mult, op1=ALU.add),
                            reads=[bst, bsc], writes=[bst, R.bXB])
                        if RS <= 4:
                            continue
                        S.op(DVE, lambda e, oq=oq: e.tensor_tensor(out=oq[:], in0=oq[:], in1=R.p_av, op=ALU.add),
                             reads=[boq], writes=[boq, R.bXB])
                        if cc == 0:
                            S.op(DVE, lambda e, oall=oall, oq=oq, h=h: e.tensor_scalar(out=oall[:, h, :], in0=oq[:], scalar1=blk[:, 0:1],
                                                                                     scalar2=None, op0=ALU.mult), reads=[boq, bblk], writes=[boall])
                        else:
                            S.op(DVE, lambda e, oall=oall, oq=oq, h=h: e.scalar_tensor_tensor(
                                out=oall[:, h, :], in0=oq[:], scalar=blk[:, 1:2], in1=oall[:, h, :], op0=ALU.mult, op1=ALU.add),
                                reads=[boq, bblk, boall], writes=[boall])

                pending = list(range(4))
                active = []
                free = list(range(NSLOT))
                while pending or active:
                    while pending and free:
                        k_ = free.pop(0)
                        active.append((head_gen(pending.pop(0), SLOTS[k_]), k_))
                    nxt = []
                    for gen_, k_ in active:
                        try:
                            next(gen_)
                            nxt.append((gen_, k_))
                        except StopIteration:
                            free.append(k_)
                    active = nxt
                if STOP <= 3 or STOP == 5 or STOP >= 7:
                    continue
                if STOP == 6:
                    S.op(POOL, lambda e, oall=oall: e.memset(oall[:], 0.5), writes=[boall])
                zl, bzl = zsil.next()
                S.op(ACT, lambda e, zl=zl, z=z: e.activation(out=zl[:], in_=z[:], func=AF.Silu), reads=[bz], writes=[bzl])
                for h in range(4):
                    jk, bjk = junkg.next()
                    S.op(DVE, lambda e, jk=jk, oall=oall, h=h, sc=sc: e.scalar_tensor_tensor(
                        out=jk[:], in0=oall[:, h, :], scalar=1.0, in1=oall[:, h, :], op0=ALU.mult, op1=ALU.mult, accum_out=sc[:, 80 + h:81 + h]),
                        reads=[boall], writes=[bjk, bsc])
                S.op(DVE, lambda e, sc=sc: e.tensor_scalar(out=sc[:, 80:84], in0=sc[:, 80:84], scalar1=1.0 / 128, scalar2=EPS, op0=ALU.mult, op1=ALU.add),
                     reads=[bsc], writes=[bsc])
                S.op(ACT, lambda e, sc=sc: e.activation(out=sc[:, 80:84], in_=sc[:, 80:84], func=AF.Ln), reads=[bsc], writes=[bsc])
                S.op(ACT, lambda e, sc=sc: e.activation(out=sc[:, 80:84], in_=sc[:, 80:84], func=AF.Exp, scale=-0.5), reads=[bsc], writes=[bsc])
                om, bom = oms.next()
                for h in range(4):
                    S.op(DVE, lambda e, oall=oall, h=h, sc=sc: e.scalar_tensor_tensor(
                        out=oall[:, h, :], in0=oall[:, h, :], scalar=sc[:, 80 + h:81 + h], in1=wn[:], op0=ALU.mult, op1=ALU.mult),
                        reads=[boall, bsc, bwn], writes=[boall])
                S.op(POOL, lambda e, om=om, oall=oall, zl=zl: e.tensor_tensor(out=om[:], in0=oall[:].rearrange("p h d -> p (h d)"), in1=zl[:], op=ALU.mult),
                     reads=[boall, bzl], writes=[bom])
                S.dma(SP, T.mixed[t0:t0 + 128, 512:1024], om[:], reads=[bom])
        S.barrier()
        S.flush(nc, sems)


WEIGHT_SPECS = [
    ("mix_pre_norm", [2, D]), ("w_in", [2, D, INW]), ("swa_sinks", [2, 4]), ("dn_conv_w", [2, 4, 1536]),
    ("dn_a_log", [2, 4]), ("dn_dt_bias", [2, 4]), ("dn_norm", [2, 128]), ("w_out", [2, D, D]),
    ("mix_post_norm", [2, D]), ("ffn_pre_norm", [2, D]), ("w_up", [2, D, 2 * DFF]), ("ffn_conv_w", [2, 3, 2 * DFF]),
    ("ffn_conv_b", [2, 2 * DFF]), ("w_down", [2, DFF, D]), ("ffn_post_norm", [2, D]),
]


def build(SL=8192, n_layers=2, phases="ABC", dump=(), mixed_in=False, gdn_stop=4):
    nc = bass.Bass("TRN2", target_bir_lowering=False)
    T = Ctx()
    T.SL = SL
    T.gdn_stop = gdn_stop
    T.x = nc.dram_tensor("x", [SL, D], F32, kind="ExternalInput").ap()
    for name, shape in WEIGHT_SPECS:
        setattr(T, name, nc.dram_tensor(name, shape, F32, kind="ExternalInput").ap())
    T.out = nc.dram_tensor("out", [SL, D], F32, kind="ExternalOutput").ap()

    def scratch(name, shape, dt):
        kind = "ExternalOutput" if name in dump else "Internal"
        if name == "mixed" and mixed_in:
            kind = "ExternalInput"
        return nc.dram_tensor(name, shape, dt, kind=kind).ap()
    T.projT = scratch("projT", [NFM, SL], F32)
    T.projTM = scratch("projTM", [SL, NTM], F32)
    T.mixed = scratch("mixed", [SL, D], BF16)
    T.xres1 = scratch("xres1", [SL, D], F32)
    T.gsc = scratch("gsc", [DFF, SL], BF16)
    T.xmid = scratch("xmid", [SL, D], F32)
    T.augrows = scratch("augrows", [8, SL], BF16)
    S = Sched()
    with ExitStack() as es:
        sems = {k: es.enter_context(nc.semaphore(k.replace("_", ""))) for k in S.semkeys()}
        for l in range(n_layers):
            x_src = T.x if l == 0 else T.xmid
            x_dst = T.out if l == n_layers - 1 else T.xmid
            if "A" in phases:
                phase_A(S, nc, sems, T, l, x_src)
            if "B" in phases or "S" in phases:
                phase_SWA(S, nc, sems, T, l)
            if "B" in phases or "M" in phases:
                phase_MOBA(S, nc, sems, T, l)
            if "B" in phases or "G" in phases:
                phase_GDN(S, nc, sems, T, l)
            if "C" in phases:
                phase_C1(S, nc, sems, T, l, x_src)
                phase_C2(S, nc, sems, T, l, x_dst)
    return nc, S


def kernel(**inputs):
    x = np.ascontiguousarray(inputs["x"], dtype=np.float32)
    B, SL, _ = x.shape
    nc, _ = build(SL, 2)
    w = {name: np.ascontiguousarray(inputs[name], dtype=np.float32) for name, _ in WEIGHT_SPECS}
    in_maps = [dict(w, x=x[b]) for b in range(B)]
    res = run_bass_kernel_spmd(nc, in_maps, core_ids=list(range(B)))
    return np.stack([r["out"] for r in res.results], axis=0).astype(np.float32)
```
